# Optimizing a Trainium2 kernel written in Bass

```python
import math
import jax, jax.numpy as jnp
from jax import lax
import numpy as np

D_MODEL = 1024
BATCH = 16
SEQ = 2048
DEPTH = 2

D_CONV = 512
CONV_WIDTH = 31
N_HEADS = 8
D_HEAD_OUT = 64
D_ATTN = N_HEADS * D_HEAD_OUT
D_MIX = D_CONV + D_ATTN
Q_LORA = 256
KV_LORA = 128
N_IDX_HEADS = 8
D_IDX = 64
TOPK_MAX = 256
N_BUCKETS = 32
MAX_DISTANCE = 128
Q_BLOCK = 128
EPS = 1e-6
SPLIT_SIZES = (D_CONV, D_CONV, D_CONV, Q_LORA, KV_LORA, D_IDX, N_IDX_HEADS, D_ATTN)
D_IN_PROJ = sum(SPLIT_SIZES)

kernel_name = "hymba_conformer_dsa_hybrid"


def rmsnorm(x, g):
    xf = x.astype(jnp.float32)
    y = xf * lax.rsqrt(jnp.mean(xf * xf, axis=-1, keepdims=True) + EPS)
    return (y * g.astype(jnp.float32)).astype(x.dtype)


def layernorm(x, g, b):
    xf = x.astype(jnp.float32)
    mu = jnp.mean(xf, axis=-1, keepdims=True)
    var = jnp.mean(jnp.square(xf - mu), axis=-1, keepdims=True)
    y = (xf - mu) * lax.rsqrt(var + EPS)
    return (y * g.astype(jnp.float32) + b.astype(jnp.float32)).astype(x.dtype)


def t5_bucket(n):
    max_exact = N_BUCKETS // 2
    n = jnp.maximum(n, 0)
    nf = jnp.maximum(n, 1).astype(jnp.float32)
    large = max_exact + (jnp.log(nf / max_exact) / math.log(MAX_DISTANCE / max_exact)
                         * (N_BUCKETS - max_exact)).astype(jnp.int32)
    large = jnp.minimum(large, N_BUCKETS - 1)
    return jnp.where(n < max_exact, n, large)


def conformer_conv(u_val, u_gate, conv_w, conv_b, ln_g, ln_b, w_pw2):
    a = u_val * jax.nn.sigmoid(u_gate)
    y = lax.conv_general_dilated(
        a, conv_w[:, None, :].astype(a.dtype), window_strides=(1,),
        padding=[(CONV_WIDTH - 1, 0)], dimension_numbers=("NWC", "WIO", "NWC"),
        feature_group_count=D_CONV) + conv_b
    y = jax.nn.silu(layernorm(y, ln_g, ln_b))
    return y @ w_pw2


def dsa_attention(c_q, c_kv, k_idx, w_idx, q_norm_g, w_uq, w_qidx, kv_norm_g, w_uv, rel_bias):
    B, L, _ = c_q.shape
    k_top = min(TOPK_MAX, L // 4)
    cq = rmsnorm(c_q, q_norm_g)
    q = (cq @ w_uq).reshape(B, L, N_HEADS, KV_LORA)
    q_idx = (cq @ w_qidx).reshape(B, L, N_IDX_HEADS, D_IDX)
    kv = rmsnorm(c_kv, kv_norm_g)
    w = w_idx.astype(jnp.float32) * (N_IDX_HEADS ** -0.5 * D_IDX ** -0.5)
    k_idx_f = k_idx.astype(jnp.float32)
    nb = L // Q_BLOCK
    key_pos = jnp.arange(L)

    def to_blocks(a):
        return a.reshape(B, nb, Q_BLOCK, *a.shape[2:]).swapaxes(0, 1)

    def block_fn(args):
        blk, q_b, qi_b, w_b = args
        t_pos = blk * Q_BLOCK + jnp.arange(Q_BLOCK)
        s_raw = jnp.einsum("bthd,bsd->bths", qi_b.astype(jnp.float32), k_idx_f)
        score = jnp.einsum("bth,bths->bts", w_b, jax.nn.relu(s_raw))
        causal = key_pos[None, :] <= t_pos[:, None]
        score = jnp.where(causal[None], score, -jnp.inf)
        _, sel = lax.top_k(score, k_top)
        kv_sel = jax.vmap(lambda kv_b, i_b: kv_b[i_b])(kv, sel)
        logits = jnp.einsum("bthc,btkc->bthk", q_b, kv_sel).astype(jnp.float32) * (KV_LORA ** -0.5)
        dist = t_pos[None, :, None] - sel
        bias = jnp.moveaxis(rel_bias[t5_bucket(dist)], -1, 2)
        logits = logits + bias.astype(jnp.float32)
        valid = (dist >= 0)[:, :, None, :]
        probs = jax.nn.softmax(jnp.where(valid, logits, -jnp.inf), axis=-1)
        return jnp.einsum("bthk,btkc->bthc", probs.astype(kv.dtype), kv_sel)

    o = lax.map(block_fn, (jnp.arange(nb), to_blocks(q), to_blocks(q_idx), to_blocks(w)))
    o = o.swapaxes(0, 1).reshape(B, L, N_HEADS, KV_LORA)
    o = jnp.einsum("bshc,hcd->bshd", o, w_uv)
    return o.reshape(B, L, D_ATTN)


def setup_inputs(seed: int = 0) -> dict:
    key = jax.random.key(seed)
    ks = jax.random.split(key, 24)
    f32 = jnp.float32
    nrm = lambda k, shape, s: jax.random.normal(k, shape, f32) * s
    return {
        "x": nrm(ks[0], (BATCH, SEQ, D_MODEL), 1.0),
        "c": nrm(ks[1], (BATCH, D_MODEL), 1.0),
        "w_ada": nrm(ks[2], (DEPTH, D_MODEL, 3 * D_MODEL), D_MODEL ** -0.5),
        "b_ada": nrm(ks[3], (DEPTH, 3 * D_MODEL), 0.02),
        "g_pre": 1.0 + nrm(ks[4], (DEPTH, D_MODEL), 0.05),
        "w_in": nrm(ks[5], (DEPTH, D_MODEL, D_IN_PROJ), D_MODEL ** -0.5),
        "conv_w": nrm(ks[6], (DEPTH, CONV_WIDTH, D_CONV), CONV_WIDTH ** -0.5),
        "conv_b": nrm(ks[7], (DEPTH, D_CONV), 0.02),
        "conv_ln_g": 1.0 + nrm(ks[8], (DEPTH, D_CONV), 0.05),
        "conv_ln_b": nrm(ks[9], (DEPTH, D_CONV), 0.02),
        "w_pw2": nrm(ks[10], (DEPTH, D_CONV, D_CONV), D_CONV ** -0.5),
        "q_norm_g": 1.0 + nrm(ks[11], (DEPTH, Q_LORA), 0.05),
        "w_uq": nrm(ks[12], (DEPTH, Q_LORA, N_HEADS * KV_LORA), Q_LORA ** -0.5),
        "w_qidx": nrm(ks[13], (DEPTH, Q_LORA, N_IDX_HEADS * D_IDX), Q_LORA ** -0.5),
        "kv_norm_g": 1.0 + nrm(ks[14], (DEPTH, KV_LORA), 0.05),
        "w_uv": nrm(ks[15], (DEPTH, N_HEADS, KV_LORA, D_HEAD_OUT), KV_LORA ** -0.5),
        "rel_bias": nrm(ks[16], (N_BUCKETS, N_HEADS), 0.5),
        "w_out": nrm(ks[17], (DEPTH, D_MIX, D_MODEL), D_MIX ** -0.5),
        "g_post": 1.0 + nrm(ks[18], (DEPTH, D_MODEL), 0.05),
    }


def reference(x, c, w_ada, b_ada, g_pre, w_in, conv_w, conv_b, conv_ln_g, conv_ln_b, w_pw2,
              q_norm_g, w_uq, w_qidx, kv_norm_g, w_uv, rel_bias, w_out, g_post):
    offsets = [int(v) for v in np.cumsum(SPLIT_SIZES)[:-1]]
    c_act = jax.nn.silu(c)
    for l in range(DEPTH):
        mod = c_act @ w_ada[l] + b_ada[l]
        shift, scale, gate = jnp.split(mod, 3, axis=-1)
        h = rmsnorm(x, g_pre[l]) * (1.0 + scale[:, None, :]) + shift[:, None, :]
        p = h @ w_in[l]
        u_val, u_gate, g_conv, c_q, c_kv, k_idx, w_idx, g_attn = jnp.split(p, offsets, axis=-1)
        y_conv = conformer_conv(u_val, u_gate, conv_w[l], conv_b[l], conv_ln_g[l], conv_ln_b[l],
                                w_pw2[l]) * jax.nn.silu(g_conv)
        y_attn = dsa_attention(c_q, c_kv, k_idx, w_idx, q_norm_g[l], w_uq[l], w_qidx[l],
                               kv_norm_g[l], w_uv[l], rel_bias) * jax.nn.silu(g_attn)
        y = jnp.concatenate([y_conv, y_attn], axis=-1) @ w_out[l]
        x = x + gate[:, None, :] * rmsnorm(y, g_post[l])
    return x
```

```python
import math
from contextlib import ExitStack

import numpy as np
import concourse.bass as bass
import concourse.mybir as mybir
from concourse.bass_utils import run_bass_kernel_spmd

F32 = mybir.dt.float32
BF16 = mybir.dt.bfloat16
AF = mybir.ActivationFunctionType
ALU = mybir.AluOpType
AX = mybir.AxisListType

D_MODEL = 1024
SEQ = 2048
NT = SEQ // 128
DEPTH = 2
D_CONV = 512
CONV_WIDTH = 31
N_HEADS = 8
KV_LORA = 128
Q_LORA = 256
D_IDX = 64
TOPK = 256
N_BUCKETS = 32
MAX_DISTANCE = 128
EPS = 1e-6
D_IN_PROJ = 2504
NB = 14
NCORES = 8
SEQ_PER_CORE = 2
NG = 383


class Src:
    def __init__(self, name, sem, inc):
        self.name, self.sem, self.inc, self.count = name, sem, inc, 0


class Buf:
    __slots__ = ("name", "last_w", "readers")

    def __init__(self, name=""):
        self.name, self.last_w, self.readers = name, None, []


class _Rec:
    def __init__(self):
        self.call = None

    def __getattr__(self, name):
        def f(*a, **k):
            self.call = (name, a, k)
            return self
        return f


class Sched:
    ENG = ("pe", "act", "dve", "pool", "sp")

    def __init__(self, nc, stack, ndma=32):
        self.nc = nc
        self.src = {}
        self.prog = {e: [] for e in self.ENG}
        self.waited = {e: {} for e in self.ENG}
        for e in self.ENG:
            if e == "sp":
                continue
            self.src[e] = Src(e, stack.enter_context(nc.semaphore("sem_" + e)), 1)
        self.ring = [Src("dma%d" % i, stack.enter_context(nc.semaphore("semd%d" % i)), 16) for i in range(ndma)]
        self.ring_pos = 0
        self.nins = 0

    def _deps(self, eng, reads, writes):
        need = {}

        def add(tok):
            if tok is None:
                return
            s, c = tok
            if need.get(s, 0) < c:
                need[s] = c

        for b in reads:
            add(b.last_w)
        for b in writes:
            add(b.last_w)
            for r in b.readers:
                add(r)
        w = self.waited[eng]
        for s, c in need.items():
            if s.name == eng and eng == "pe":
                continue
            if w.get(s, 0) >= c:
                continue
            w[s] = c
            self.prog[eng].append(("w", s, c))

    def op(self, eng, fn, reads=(), writes=()):
        self._deps(eng, reads, writes)
        if eng == "sp":
            s = self.ring[self.ring_pos]
            self.ring_pos = (self.ring_pos + 1) % len(self.ring)
            if s.count and self.waited["sp"].get(s, 0) < s.count:
                self.waited["sp"][s] = s.count
                self.prog["sp"].append(("w", s, s.count))
        else:
            s = self.src[eng]
        s.count += s.inc
        rec = _Rec()
        fn(rec)
        assert rec.call is not None
        self.prog[eng].append(("i", rec.call, s, s.count))
        tok = (s, s.count)
        for b in writes:
            b.last_w = tok
            b.readers = []
        for b in reads:
            if len(b.readers) > 64:
                best = {}
                for (ss, cc) in b.readers:
                    if best.get(ss, 0) < cc:
                        best[ss] = cc
                b.readers = list(best.items())
            b.readers.append(tok)
        self.nins += 1
        return tok

    def finish(self):
        for s in self.ring:
            if s.count:
                self.prog["sp"].append(("w", s, s.count))

    def emit(self):
        nc = self.nc
        engmap = {"pe": "tensor", "act": "scalar", "dve": "vector", "pool": "gpsimd", "sp": "sync"}
        miles = {}
        for e in self.ENG:
            for it in self.prog[e]:
                if it[0] == "w" and it[1].inc == 1:
                    miles.setdefault(it[1], set()).add(it[2])
        rank = {}
        for src, st in miles.items():
            for r, idx in enumerate(sorted(st)):
                rank[(src, idx)] = r + 1
        self.n_inc = sum(len(v) for v in miles.values())
        with nc.Block() as block:
            for e in self.ENG:
                prog = self.prog[e]

                def body(engine, prog=prog):
                    for it in prog:
                        src = it[1] if it[0] == "w" else it[2]
                        if it[0] == "w":
                            if src.inc == 1:
                                engine.wait_ge(src.sem, rank[(src, it[2])])
                            else:
                                engine.wait_ge(src.sem, it[2])
                        else:
                            name, a, k = it[1]
                            ins = getattr(engine, name)(*a, **k)
                            if src.inc != 1:
                                ins.then_inc(src.sem, src.inc)
                            elif (src, it[3]) in rank:
                                ins.then_inc(src.sem, 1)

                getattr(block, engmap[e])(body)


def t5_bucket_np(n):
    max_exact = N_BUCKETS // 2
    n = np.maximum(n, 0)
    nf = np.maximum(n, 1).astype(np.float32)
    large = max_exact + (np.log(nf / np.float32(max_exact)) / np.float32(math.log(MAX_DISTANCE / max_exact))
                         * np.float32(N_BUCKETS - max_exact)).astype(np.int32)
    large = np.minimum(large, N_BUCKETS - 1)
    return np.where(n < max_exact, n, large)


def build_program(layers=(0, 1), nseq=SEQ_PER_CORE, debug=None):
    nc = bass.Bass("TRN2", target_bir_lowering=False)
    L = DEPTH
    dram = {}

    def din(name, shape):
        dram[name] = nc.dram_tensor(name, list(shape), F32, kind="ExternalInput").ap()
        return dram[name]

    x_d = din("x", [nseq, SEQ, D_MODEL])
    cT_d = din("cT", [128, 8, nseq])
    wada_d = din("w_adaP", [L, 8, 128, 8, 384])
    NJOB = 17
    wpack_d = din("wpack", [L, NJOB, 128, 2048])
    badaT_d = din("b_adaT", [128, L, 24])
    gpreT_d = din("g_preT", [128, L, 8])
    gpostT_d = din("g_postT", [128, L, 8])
    convw_d = din("conv_wT", [128, L, 4, CONV_WIDTH])
    convb_d = din("conv_bT", [128, L, 4])
    lng_d = din("ln_gT", [128, L, 4])
    lnb_d = din("ln_bT", [128, L, 4])
    qng_d = din("q_norm_gT", [128, L, 2])
    kvng_d = din("kv_norm_gT", [128, L, 1])
    relb_d = din("rel_bias", [N_BUCKETS, N_HEADS])
    relbT_d = din("rel_biasT", [N_HEADS, N_BUCKETS])
    oh_d = din("oh", [N_BUCKETS, NG])
    out_d = nc.dram_tensor("out", [nseq, SEQ, D_MODEL], F32, kind="ExternalOutput").ap()
    scr_d = nc.dram_tensor("scr_g", [N_HEADS, NG], F32, kind="Internal").ap()
    dbg_d = {}
    if debug:
        for name, shape in debug.items():
            dbg_d[name] = nc.dram_tensor("dbg_" + name, list(shape), F32, kind="ExternalOutput").ap()

    with ExitStack() as st:
        S = Sched(nc, st)
        off = [16640]

        def sb(name, shape, dt, at=None):
            nbytes = int(np.prod(shape[1:])) * (4 if dt == F32 else 2)
            nbytes = (nbytes + 31) // 32 * 32
            if at is None:
                at = off[0]
                off[0] += nbytes
            t = nc.alloc_sbuf_tensor_at(name, list(shape), dt, offset=at)
            return t

        psum = nc.alloc_psum_tensor("psum", [128, 8, 512], F32)
        pbuf = [Buf("ps%d" % i) for i in range(8)]

        def pbank(i):
            return psum[:, i, :]

        ident_f = sb("ident_f", [128, 128], F32); b_identf = Buf()
        ident_b = sb("ident_b", [128, 128], BF16); b_identb = Buf()
        ones_f = sb("ones_f", [128, 128], F32); b_onesf = Buf()
        ones_b = sb("ones_b", [128, 128], BF16); b_onesb = Buf()
        jf = sb("jf", [128, 128], F32); b_jf = Buf()
        causal = sb("causal", [128, 128], F32); b_causal = Buf()
        pow2 = sb("pow2", [128, NB + 2], F32); b_pow2 = Buf()
        negbig = sb("negbig", [128, 1], F32); b_negbig = Buf()
        cT = sb("cT", [128, 8, nseq], F32); b_cT = Buf()
        cact = sb("cact", [128, 8, nseq], F32); b_cact = Buf()
        modT = sb("modT", [128, L, 24, nseq], F32); b_modT = Buf()
        badaT = sb("badaT", [128, L, 24], F32); b_small = Buf()
        gpreT = sb("gpreT", [128, L, 8], F32)
        gpostT = sb("gpostT", [128, L, 8], F32)
        convw = sb("convw", [128, L, 4, CONV_WIDTH], F32)
        convb = sb("convb", [128, L, 4], F32)
        lng = sb("lng", [128, L, 4], F32)
        lnb = sb("lnb", [128, L, 4], F32)
        qng = sb("qng", [128, L, 2], F32)
        kvng = sb("kvng", [128, L, 1], F32)
        b31bc = sb("b31bc", [128, N_HEADS], F32); b_b31 = Buf()
        tbc = sb("tbc", [128, N_HEADS, 256], BF16); b_tbc = Buf()
        xs_res = sb("xres", [128, NT, D_MODEL], F32)
        b_x = [Buf("x%d" % i) for i in range(NT)]
        dummy = sb("dummy", [128, 8], F32)
        PB = off[0]
        LIMIT = 229376 - 64
        PX1 = PB
        PX2 = PX1 + 16384
        PX3 = PX2 + 16384
        PX4 = PX3 + 40704
        PX5 = PX4 + 57344

        S.op("pool", lambda e: e.memset(ident_f[:], 1.0), writes=[b_identf])
        S.op("pool", lambda e: e.affine_select(out=ident_f[:], in_=ident_f[:], pattern=[[-1, 128]], compare_op=ALU.is_equal,
                                               fill=0.0, base=0, channel_multiplier=1), reads=[b_identf], writes=[b_identf])
        S.op("pool", lambda e: e.tensor_copy(out=ident_b[:], in_=ident_f[:]), reads=[b_identf], writes=[b_identb])
        S.op("pool", lambda e: e.memset(ones_f[:], 1.0), writes=[b_onesf])
        S.op("pool", lambda e: e.memset(ones_b[:], 1.0), writes=[b_onesb])
        S.op("pool", lambda e: e.memset(jf[:], 1.0), writes=[b_jf])
        S.op("pool", lambda e: e.affine_select(out=jf[:], in_=jf[:], pattern=[[1, 128]], compare_op=ALU.is_equal,
                                               fill=0.0, base=-127, channel_multiplier=1), reads=[b_jf], writes=[b_jf])
        S.op("pool", lambda e: e.memset(causal[:], 0.0), writes=[b_causal])
        S.op("pool", lambda e: e.affine_select(out=causal[:], in_=causal[:], pattern=[[-1, 128]], compare_op=ALU.is_ge,
                                               fill=-1e30, base=0, channel_multiplier=1), reads=[b_causal], writes=[b_causal])
        for j in range(NB + 2):
            v = 2.0 ** (-min(j + 1, NB))
            S.op("pool", lambda e, j=j, v=v: e.memset(pow2[:, j:j + 1], v), writes=[b_pow2])
        S.op("pool", lambda e: e.memset(negbig[:], -1e29), writes=[b_negbig])

        for (t, d) in ((cT, cT_d), (badaT, badaT_d), (gpreT, gpreT_d), (gpostT, gpostT_d), (convw, convw_d), (convb, convb_d),
                       (lng, lng_d), (lnb, lnb_d), (qng, qng_d), (kvng, kvng_d)):
            S.op("sp", lambda e, t=t, d=d: e.dma_start(out=t[:], in_=d), writes=[b_small if t is not cT else b_cT])
        S.op("sp", lambda e: e.dma_start(out=b31bc[:], in_=bass.AP(tensor=relb_d.tensor, offset=31 * N_HEADS,
                                                                    ap=[[0, 128], [1, N_HEADS]])), writes=[b_b31])

        po = PX3
        relb_s = sb("relb_s", [128, N_HEADS], F32, at=po); po += 32
        relbT_s = sb("relbT_s", [128, N_BUCKETS], F32, at=po); po += 128
        nb31 = sb("nb31", [128, 1], F32, at=po); po += 32
        oh_s = sb("oh_s", [128, NG], F32, at=po); po += 1536
        gc_s = sb("gc_s", [128, NG], F32, at=po); po += 1536
        tbrev = sb("tbrev", [128, N_HEADS, 256], F32, at=po); po += 8192
        wada_st = [sb("wada_st%d" % i, [128, 8, 384], F32, at=po + i * 12288) for i in range(2)]
        po += 2 * 12288
        assert po <= PX4
        b_pro = Buf(); b_gc = Buf(); b_scr = Buf(); b_tbrev = Buf()
        b_wada = [Buf(), Buf()]

        S.op("sp", lambda e: e.dma_start(out=relb_s[0:N_BUCKETS, :], in_=relb_d), writes=[b_pro])
        S.op("sp", lambda e: e.dma_start(out=relbT_s[0:N_HEADS, :], in_=relbT_d), writes=[b_pro])
        S.op("sp", lambda e: e.dma_start(out=oh_s[0:N_BUCKETS, :], in_=oh_d), writes=[b_pro])
        S.op("dve", lambda e: e.tensor_scalar(out=nb31[0:N_HEADS, :], in0=relbT_s[0:N_HEADS, 31:32], scalar1=-1.0, scalar2=None,
                                              op0=ALU.mult), reads=[b_pro], writes=[b_pro])
        S.op("pe", lambda e: e.matmul(psum[0:N_HEADS, 0, 0:NG], lhsT=relb_s[0:N_BUCKETS, :], rhs=oh_s[0:N_BUCKETS, :],
                                      start=True, stop=True), reads=[b_pro], writes=[pbuf[0]])
        S.op("act", lambda e: e.activation(out=gc_s[0:N_HEADS, :], in_=psum[0:N_HEADS, 0, 0:NG], func=AF.Exp,
                                           bias=nb31[0:N_HEADS, 0:1], scale=1.0), reads=[pbuf[0], b_pro], writes=[b_gc])
        S.op("sp", lambda e: e.dma_start(out=scr_d, in_=gc_s[0:N_HEADS, :]), reads=[b_gc], writes=[b_scr])
        S.op("sp", lambda e: e.dma_start(out=tbrev[:], in_=bass.AP(tensor=scr_d.tensor, offset=0,
                                                                    ap=[[1, 128], [NG, N_HEADS], [1, 256]])),
             reads=[b_scr], writes=[b_tbrev])
        for h in range(N_HEADS):
            bk = 1 + (h % 2)
            S.op("pe", lambda e, h=h, bk=bk: e.matmul(psum[:, bk, 0:256], lhsT=jf[:], rhs=tbrev[:, h, :], start=True, stop=True),
                 reads=[b_jf, b_tbrev], writes=[pbuf[bk]])
            S.op("act", lambda e, h=h, bk=bk: e.activation(out=tbc[:, h, :], in_=psum[:, bk, 0:256], func=AF.Copy),
                 reads=[pbuf[bk]], writes=[b_tbc])

        S.op("act", lambda e: e.activation(out=cact[:], in_=cT[:], func=AF.Silu), reads=[b_cT], writes=[b_cact])
        gi = 0
        for l in layers:
            for g in range(8):
                slot = gi % 2
                gi += 1
                S.op("sp", lambda e, slot=slot, g=g, l=l: e.dma_start(out=wada_st[slot][:], in_=wada_d[l, g]),
                     writes=[b_wada[slot]])
                for jj in range(3):
                    j = g * 3 + jj
                    for k in range(8):
                        S.op("pe", lambda e, slot=slot, jj=jj, j=j, k=k: e.matmul(
                            psum[:, 3, j * nseq:(j + 1) * nseq], lhsT=wada_st[slot][:, k, jj * 128:(jj + 1) * 128],
                            rhs=cact[:, k, :], start=(k == 0), stop=(k == 7)),
                            reads=[b_wada[slot], b_cact], writes=[pbuf[3]])
            S.op("dve", lambda e, l=l: e.tensor_tensor(
                out=modT[:, l, :, :], in0=psum[:, 3, 0:24 * nseq].rearrange("p (j b) -> p j b", b=nseq),
                in1=badaT[:, l, :].unsqueeze(2).to_broadcast([128, 24, nseq]), op=ALU.add),
                reads=[pbuf[3], b_small], writes=[b_modT])

        yconvT = sb("yconvT", [128, 4, SEQ], BF16, at=PX1)
        sgT = sb("sgT", [128, 4, SEQ], BF16, at=PX2)
        yc = sb("yc", [128, 4, SEQ], BF16, at=PX2)
        o = PX4
        hT = sb("hT", [128, 8, SEQ], BF16, at=o); o += 32768
        wst = [sb("wst%d" % i, [128, 2048], F32, at=o + i * 8192) for i in range(2)]; o += 16384
        wbf = [sb("wbf%d" % i, [128, 2048], BF16, at=o + i * 4096) for i in range(2)]; o += 8192
        assert o == PX5
        b_hT = [Buf("hT%d" % i) for i in range(4)]
        b_yconv = [Buf() for _ in range(4)]
        b_sg = [Buf() for _ in range(8)]
        b_yc = [Buf() for _ in range(4)]
        b_wst = [Buf(), Buf()]
        b_wbf = [Buf(), Buf()]

        o = PX3
        abc = sb("abc", [128, D_MODEL], F32, at=o); o += 4096
        shbc = sb("shbc", [128, D_MODEL], F32, at=o); o += 4096
        xs = [sb("xs%d" % i, [128, D_MODEL], F32, at=o + i * 4096) for i in range(2)]; o += 8192
        dg = [sb("dg%d" % i, [128, 128], F32, at=o + i * 512) for i in range(2)]; o += 1024
        vecT = sb("vecT", [128, 3, 8], F32, at=o); o += 96
        ssq = sb("ssq", [128, NT], F32, at=o); o += 64
        rs16 = sb("rs16", [128, NT], F32, at=o); o += 64
        rstd16 = sb("rstd16", [128, NT], F32, at=o); o += 64
        assert o <= PX4
        b_abc = Buf(); b_shbc = Buf(); b_xs = [Buf(), Buf()]; b_dg = [Buf(), Buf()]; b_vecT = Buf()
        b_ssq = Buf(); b_rs16 = Buf(); b_rstd16 = Buf()

        o = PX3
        apad = [sb("apad%d" % i, [128, 30 + SEQ], BF16, at=o + i * 4160) for i in range(2)]; o += 8320
        sig = [sb("sig%d" % i, [128, 512], BF16, at=o + i * 1024) for i in range(2)]; o += 2048
        cdiag = sb("cdiag", [128, CONV_WIDTH, 128], BF16, at=o); o += 7936
        ycsq = [sb("ycsq0", [128, 512], BF16, at=o), sb("ycsq1", [128, 512], BF16, at=PX3 + 16384)]; o += 1024
        zt1 = sb("zt1", [128, 4, 512], BF16, at=o); o += 4096
        m2b = sb("m2b", [128, 512], F32, at=o); o += 2048
        varb = sb("varb", [128, 512], F32, at=o); o += 2048
        dtmp = [sb("dtmp0", [128, 512], F32, at=o)] * 2; o += 2048
        zt0 = sb("zt0", [128, 4, 512], BF16, at=o); o += 4096
        ztb = [zt0, zt1]
        sgc = [sb("sgc%d" % i, [128, 512], BF16, at=o + i * 1024) for i in range(2)]; o += 2048
        wpw2b = sb("wpw2b", [128, 4, 512], BF16, at=o); o += 4096
        assert o <= PX4, o
        meanb4 = [sb("meanb4_%d" % i, [128, 512], F32, at=PX3 + i * 2048) for i in range(4)]
        rstdb4 = [sb("rstdb4_%d" % i, [128, 512], F32, at=PX3 + 8192 + i * 2048) for i in range(4)]
        assert 16384 + 1024 <= 8320 + 2048 + 7936
        b_apad = [Buf(), Buf()]; b_sig = [Buf(), Buf()]; b_cdiag = Buf(); b_ycsq = [Buf(), Buf()]
        b_mean4 = [Buf() for _ in range(4)]; b_m2b = Buf(); b_varb = Buf(); b_rstd4 = [Buf() for _ in range(4)]; b_dtmp = [Buf()] * 2
        b_ztb = [[Buf() for _ in range(4)] for _ in range(2)]; b_sgc = [Buf(), Buf()]; b_wpw2b = Buf()

        o = PX3
        cqnT = sb("cqnT", [128, 2, SEQ], BF16, at=o); o += 8192
        kvnT = sb("kvnT", [128, SEQ], BF16, at=o); o += 4096
        kidx2 = sb("kidx2", [128, SEQ], BF16, at=o); o += 4096
        vaug = sb("vaug", [128, NT, N_HEADS, 65], BF16, at=o); o += 16640
        wtok = sb("wtok", [128, NT, N_HEADS], F32, at=o); o += 512
        wuqb = sb("wuqb", [128, 2, 1024], BF16, at=o); o += 4096
        wqib = sb("wqib", [128, 2, 512], BF16, at=o); o += 2048
        wuvb = sb("wuvb", [128, 512], BF16, at=o); o += 1024
        assert o <= PX4, o
        o = PX5
        sqc = [sb("sqc0", [128, 512], BF16, at=o)] * 2; o += 1024
        rstdc = [sb("rstdc0", [128, 512], F32, at=o)] * 2; o += 2048
        kdw = sb("kdw", [128, 8, 128], BF16, at=o); o += 2048
        assert o <= LIMIT, o
        b_cqn = [Buf() for _ in range(8)]
        b_kvn = Buf(); b_kidx = Buf(); b_vaug = Buf(); b_wtok = Buf()
        b_wuqb = Buf(); b_wqib = Buf(); b_wuvb = Buf()
        b_sqc = [Buf()] * 2; b_rstdc = [Buf()] * 2; b_kdw = Buf()

        o = PX4
        qblk = [sb("qblk%d" % i, [128, N_HEADS, 256], BF16, at=o + i * 4096) for i in range(2)]; o += 8192
        qiblk = [sb("qiblk0", [128, 4, 256], BF16, at=o)] * 2; o += 2048
        score = [sb("score%d" % i, [128, SEQ], F32, at=o + i * 8192) for i in range(2)]; o += 16384
        maskts = [sb("maskts%d" % i, [128, SEQ], BF16, at=o + i * 4096) for i in range(2)]; o += 8192
        maskT = sb("maskT", [128, NT, 256], BF16, at=o); o += 8192
        rt = [sb("rt%d" % i, [128, 512], BF16, at=o + i * 1024) for i in range(4)]; o += 4096
        et = [sb("et%d" % i, [128, 256], BF16, at=o + i * 512) for i in range(4)]; o += 2048
        pm = [sb("pm%d" % i, [128, 256], BF16, at=o + i * 512) for i in range(4)]; o += 2048
        dgw = [sb("dgw%d" % i, [128, N_HEADS, 128], BF16, at=o + i * 2048) for i in range(2)]; o += 4096
        ytok = sb("ytok", [128, 2, 512], F32, at=o); o += 4096
        bis = sb("bis", [128, 8], F32, at=o); o += 32
        steps = sb("steps", [128, NB + 2], F32, at=o); o += 96
        cands = sb("cands", [128, NB + 2], F32, at=o); o += 96
        cnts = sb("cnts", [128, NB + 2], F32, at=o); o += 96
        sels = sb("sels", [128, NB + 2], F32, at=o); o += 96
        rcp = sb("rcp", [128, 2, N_HEADS], F32, at=o); o += 64
        osb = sb("osb", [128, 2, N_HEADS, 65], F32, at=o); o += 4160
        assert o <= LIMIT, (o, LIMIT)
        b_osb = Buf()
        b_qblk = [Buf(), Buf()]; b_qiblk = [Buf()] * 2; b_score = [Buf(), Buf()]; b_maskts = [Buf(), Buf()]
        b_maskT = [Buf(), Buf()]
        b_rt = [Buf() for _ in range(4)]; b_et = [Buf() for _ in range(4)]; b_pm = [Buf() for _ in range(4)]
        b_dgw = [Buf(), Buf()]; b_ytok = [Buf(), Buf()]; b_bis = Buf(); b_steps = Buf()
        b_cands = [Buf() for _ in range(NB + 2)]; b_cnts = [Buf() for _ in range(NB + 2)]; b_sels = [Buf() for _ in range(NB + 2)]
        b_rcp = Buf()

        o = PX3
        woutb = sb("woutb", [128, 8, 1024], BF16, at=o); o += 16384
        gbc = sb("gbc", [128, D_MODEL], F32, at=o); o += 4096
        etmp = [sb("etmp%d" % i, [128, 512], F32, at=o + i * 2048) for i in range(2)]; o += 4096
        ejunk = sb("ejunk", [128, 512], F32, at=o); o += 2048
        ess = sb("ess", [128, 4], F32, at=o); o += 32
        dgE = [sb("dgE%d" % i, [128, 128], F32, at=o + i * 512) for i in range(2)]; o += 1024
        gvT = sb("gvT", [128, 8], F32, at=o); o += 32
        assert o <= PX4, o
        b_woutb = [Buf() for _ in range(4)]; b_gbc = Buf(); b_etmp = [Buf(), Buf()]; b_ejunk = Buf(); b_ess = Buf()
        b_dgE = [Buf(), Buf()]; b_gvT = Buf()

        b_region = Buf("region")

        def phase_barrier(extra=()):
            S.op("pool", lambda e: e.memset(dummy[:, 0:1], 0.0), writes=[b_region] + list(extra))

        RG = [b_region]

        wcnt = [0]
        wjob = [0, 0]

        def load_w(parts, dst_writes, casts):
            slot = wcnt[0] % 2
            wcnt[0] += 1
            stg = wst[slot]
            jn = wjob[1]
            wjob[1] += 1
            assert jn < NJOB
            S.op("sp", lambda e, stg=stg, jn=jn: e.dma_start(out=stg[:], in_=wpack_d[wjob[0], jn]), reads=RG, writes=[b_wst[slot]])
            for (oap, ifn, obufs) in casts:
                S.op("dve", lambda e, oap=oap, ifn=ifn, stg=stg: e.tensor_copy(out=oap, in_=ifn(stg)),
                     reads=[b_wst[slot]] + RG, writes=obufs)

        def win_cols(l, c0, c1):
            return None

        def stage3(stg, n):
            return stg[:, 0:8 * n].rearrange("p (k n) -> p k n", k=8)

        def bcast_rows(vec_col_fn, dst, dst_buf, dgs, b_dgs, extra_reads):
            for half in range(2):
                bk = 4 + half
                for kk in range(4):
                    k = half * 4 + kk
                    sl = k % 2
                    S.op("dve", lambda e, k=k, sl=sl: e.tensor_scalar(out=dgs[sl][:], in0=ident_f[:], scalar1=vec_col_fn(k),
                                                                      scalar2=None, op0=ALU.mult),
                         reads=[b_identf] + extra_reads + RG, writes=[b_dgs[sl]])
                    S.op("pe", lambda e, kk=kk, sl=sl, bk=bk: e.matmul(psum[:, bk, kk * 128:(kk + 1) * 128], lhsT=ones_f[:],
                                                                      rhs=dgs[sl][:], start=True, stop=True),
                         reads=[b_onesf, b_dgs[sl]], writes=[pbuf[bk]])
                S.op("act", lambda e, half=half, bk=bk: e.activation(out=dst[:, half * 512:(half + 1) * 512], in_=psum[:, bk, :],
                                                                     func=AF.Copy), reads=[pbuf[bk]] + RG, writes=[dst_buf])

        def block(b, l, last_layer):
            wjob[0] = l
            wjob[1] = 0
            phase_barrier()
            S.op("dve", lambda e: e.scalar_tensor_tensor(out=vecT[:, 0, :], in0=modT[:, l, 8:16, b], scalar=1.0, in1=gpreT[:, l, :],
                                                         op0=ALU.add, op1=ALU.mult), reads=[b_modT, b_small] + RG, writes=[b_vecT])
            S.op("dve", lambda e: e.tensor_copy(out=vecT[:, 1, :], in_=modT[:, l, 0:8, b]), reads=[b_modT] + RG, writes=[b_vecT])
            bcast_rows(lambda k: vecT[:, 0, k:k + 1], abc, b_abc, dg, b_dg, [b_vecT])
            bcast_rows(lambda k: vecT[:, 1, k:k + 1], shbc, b_shbc, dg, b_dg, [b_vecT])
            for tt in range(NT):
                S.op("act", lambda e, tt=tt: e.activation(out=xs[tt % 2][:], in_=xs_res[:, tt, :], func=AF.Square,
                                                          accum_out=ssq[:, tt:tt + 1]),
                     reads=[b_x[tt]] + RG, writes=[b_xs[tt % 2], b_ssq])
            S.op("act", lambda e: e.activation(out=rs16[:], in_=ssq[:], func=AF.Sqrt, bias=EPS, scale=1.0 / D_MODEL),
                 reads=[b_ssq] + RG, writes=[b_rs16])
            S.op("dve", lambda e: e.reciprocal(out=rstd16[:], in_=rs16[:]), reads=[b_rs16] + RG, writes=[b_rstd16])
            for tt in range(NT):
                sl = tt % 2
                S.op("dve", lambda e, tt=tt, sl=sl: e.scalar_tensor_tensor(out=xs[sl][:], in0=xs_res[:, tt, :], scalar=rstd16[:, tt:tt + 1],
                                                                           in1=abc[:], op0=ALU.mult, op1=ALU.mult),
                     reads=[b_x[tt], b_rstd16, b_abc] + RG, writes=[b_xs[sl]])
                S.op("dve", lambda e, sl=sl: e.tensor_tensor(out=xs[sl][:], in0=xs[sl][:], in1=shbc[:], op=ALU.add),
                     reads=[b_xs[sl], b_shbc] + RG, writes=[b_xs[sl]])
                for half in range(2):
                    bk = (tt * 2 + half) % 4
                    for kk in range(4):
                        k = half * 4 + kk
                        S.op("pe", lambda e, sl=sl, k=k, kk=kk, bk=bk: e.transpose(out=psum[:, bk, kk * 128:(kk + 1) * 128],
                                                                                  in_=xs[sl][:, k * 128:(k + 1) * 128], identity=ident_f[:]),
                             reads=[b_xs[sl], b_identf], writes=[pbuf[bk]])
                    S.op("act", lambda e, tt=tt, half=half, bk=bk: e.activation(
                        out=hT[:, half * 4:(half + 1) * 4, tt * 128:(tt + 1) * 128],
                        in_=psum[:, bk, :].rearrange("p (k t) -> p k t", k=4), func=AF.Copy),
                        reads=[pbuf[bk]] + RG, writes=[b_hT[tt // 4]])

            phase_barrier()

            def inproj(wtile_fn, bk, tb, n=512, k_list=range(8)):
                for k in k_list:
                    S.op("pe", lambda e, k=k: e.matmul(psum[:, bk, 0:n], lhsT=wtile_fn(k), rhs=hT[:, k, tb * 512:tb * 512 + n],
                                                       start=(k == 0), stop=(k == 7)),
                         reads=[b_hT[tb]] + wt_reads[0], writes=[pbuf[bk]])

            wt_reads = [[]]
            load_w([(lambda stg: stg[:].rearrange("p (k n) -> p k n", k=4), None)], None,
                   [(wpw2b[:], lambda stg: stg[:].rearrange("p (k n) -> p k n", k=4), [b_wpw2b])])
            for c in range(4):
                slot = wcnt[0] % 2
                wb = wbf[slot]
                wb3 = wb[:].rearrange("p (k n) -> p k n", k=8)
                load_w([(lambda stg: stage3(stg, 256)[:, :, 0:128], win_cols(l, c * 128, (c + 1) * 128)),
                        (lambda stg: stage3(stg, 256)[:, :, 128:256], win_cols(l, 512 + c * 128, 512 + (c + 1) * 128))], None,
                       [(wb3, lambda stg: stage3(stg, 256), [b_wbf[slot]])])
                ap_ = apad[c % 2]
                bap = b_apad[c % 2]
                S.op("pool", lambda e, ap_=ap_: e.memset(ap_[:, 0:30], 0.0), reads=RG, writes=[bap])
                for tb in range(4):
                    wt_reads[0] = [b_wbf[slot]]
                    inproj(lambda k: wb3[:, k, 128:256], 0, tb)
                    inproj(lambda k: wb3[:, k, 0:128], 1, tb)
                    sl = tb % 2
                    S.op("act", lambda e, sl=sl: e.activation(out=sig[sl][:], in_=psum[:, 0, :], func=AF.Sigmoid),
                         reads=[pbuf[0]] + RG, writes=[b_sig[sl]])
                    S.op("dve", lambda e, sl=sl, tb=tb, ap_=ap_: e.tensor_tensor(out=ap_[:, 30 + tb * 512:30 + (tb + 1) * 512], in0=psum[:, 1, :],
                                                                                in1=sig[sl][:], op=ALU.mult),
                         reads=[pbuf[1], b_sig[sl]] + RG, writes=[bap])
                    for k in range(tb * 8, min(CONV_WIDTH, tb * 8 + 8)):
                        S.op("dve", lambda e, k=k, c=c: e.tensor_scalar(out=cdiag[:, k, :], in0=ident_b[:], scalar1=convw[:, l, c, k:k + 1],
                                                                        scalar2=None, op0=ALU.mult),
                             reads=[b_identb, b_small] + RG, writes=[b_cdiag])
                for tb in range(4):
                    bk = 2 + (tb % 2)
                    for k in range(CONV_WIDTH):
                        S.op("pe", lambda e, k=k, tb=tb, bk=bk, ap_=ap_: e.matmul(psum[:, bk, :], lhsT=cdiag[:, k, :],
                                                                                 rhs=ap_[:, k + tb * 512:k + tb * 512 + 512],
                                                                                 start=(k == 0), stop=(k == CONV_WIDTH - 1)),
                             reads=[b_cdiag, bap], writes=[pbuf[bk]])
                    S.op("act", lambda e, tb=tb, bk=bk, c=c: e.activation(out=yc[:, c, tb * 512:(tb + 1) * 512], in_=psum[:, bk, :],
                                                                          func=AF.Identity, bias=convb[:, l, c:c + 1], scale=1.0),
                         reads=[pbuf[bk], b_small] + RG, writes=[b_yc[c]])
            gslot = []
            for g in range(2):
                sl_ = wcnt[0] % 2
                gslot.append(sl_)
                load_w([(lambda stg: stage3(stg, 256), win_cols(l, 1024 + g * 256, 1024 + (g + 1) * 256))], None,
                       [(wbf[sl_][:].rearrange("p (k n) -> p k n", k=8), lambda stg: stage3(stg, 256), [b_wbf[sl_]])])
            alias_w = [b_apad[0], b_apad[1], b_sig[0], b_sig[1], b_cdiag]
            for tb in range(4):
                tsl = slice(tb * 512, (tb + 1) * 512)
                b4, b5 = 4 + 2 * (tb % 2), 5 + 2 * (tb % 2)
                for c in range(4):
                    S.op("pe", lambda e, c=c: e.matmul(psum[:, b4, :], lhsT=ones_b[:], rhs=yc[:, c, tsl], start=(c == 0), stop=(c == 3)),
                         reads=[b_onesb, b_yc[c]], writes=[pbuf[b4]])
                for c in range(4):
                    sl = c % 2
                    S.op("act", lambda e, c=c, sl=sl: e.activation(out=ycsq[sl][:], in_=yc[:, c, tsl], func=AF.Square),
                         reads=[b_yc[c]] + RG, writes=[b_ycsq[sl]] + (alias_w if sl == 1 else []))
                    S.op("pe", lambda e, c=c, sl=sl: e.matmul(psum[:, b5, :], lhsT=ones_b[:], rhs=ycsq[sl][:], start=(c == 0), stop=(c == 3)),
                         reads=[b_onesb, b_ycsq[sl]], writes=[pbuf[b5]])
                S.op("act", lambda e: e.activation(out=meanb4[tb][:], in_=psum[:, b4, :], func=AF.Copy, scale=1.0 / D_CONV),
                     reads=[pbuf[b4]] + RG, writes=[b_mean4[tb]] + alias_w)
                S.op("dve", lambda e: e.tensor_tensor(out=m2b[:], in0=meanb4[tb][:], in1=meanb4[tb][:], op=ALU.mult),
                     reads=[b_mean4[tb]] + RG, writes=[b_m2b])
                S.op("dve", lambda e: e.scalar_tensor_tensor(out=varb[:], in0=psum[:, b5, :], scalar=1.0 / D_CONV, in1=m2b[:],
                                                             op0=ALU.mult, op1=ALU.subtract),
                     reads=[pbuf[b5], b_m2b] + RG, writes=[b_varb])
                S.op("dve", lambda e: e.tensor_scalar(out=varb[:], in0=varb[:], scalar1=0.0, scalar2=None, op0=ALU.max),
                     reads=[b_varb] + RG, writes=[b_varb])
                S.op("act", lambda e: e.activation(out=rstdb4[tb][:], in_=varb[:], func=AF.Sqrt, bias=EPS, scale=1.0),
                     reads=[b_varb] + RG, writes=[b_rstd4[tb]] + alias_w)
                S.op("dve", lambda e: e.reciprocal(out=rstdb4[tb][:], in_=rstdb4[tb][:]), reads=[b_rstd4[tb]] + RG, writes=[b_rstd4[tb]])
            def ln_apply(tb, c):
                tsl = slice(tb * 512, (tb + 1) * 512)
                zt = ztb[tb % 2]; b_zt = b_ztb[tb % 2]
                sl = c % 2
                S.op("dve", lambda e: e.tensor_tensor(out=dtmp[sl][:], in0=yc[:, c, tsl], in1=meanb4[tb][:], op=ALU.subtract),
                     reads=[b_yc[c], b_mean4[tb]] + RG, writes=[b_dtmp[sl]])
                S.op("dve", lambda e: e.tensor_tensor(out=dtmp[sl][:], in0=dtmp[sl][:], in1=rstdb4[tb][:], op=ALU.mult),
                     reads=[b_dtmp[sl], b_rstd4[tb]] + RG, writes=[b_dtmp[sl]])
                S.op("act", lambda e: e.activation(out=zt[:, c, :], in_=dtmp[sl][:], func=AF.Silu,
                                                   bias=lnb[:, l, c:c + 1], scale=lng[:, l, c:c + 1]),
                     reads=[b_dtmp[sl], b_small] + RG, writes=[b_zt[c]])

            for c in range(4):
                ln_apply(0, c)
            for tb in range(4):
                tsl = slice(tb * 512, (tb + 1) * 512)
                zt = ztb[tb % 2]; b_zt = b_ztb[tb % 2]
                for c2 in range(4):
                    bk = 0 + (c2 % 2)
                    bg = 2 + (c2 % 2)
                    for c in range(4):
                        S.op("pe", lambda e, c=c, c2=c2, bk=bk: e.matmul(psum[:, bk, :], lhsT=wpw2b[:, c, c2 * 128:(c2 + 1) * 128],
                                                                        rhs=zt[:, c, :], start=(c == 0), stop=(c == 3)),
                             reads=[b_wpw2b, b_zt[c]], writes=[pbuf[bk]])
                    gs_ = gslot[c2 // 2]
                    wt_reads[0] = [b_wbf[gs_]]
                    wg3 = wbf[gs_][:].rearrange("p (k n) -> p k n", k=8)
                    inproj(lambda k: wg3[:, k, (c2 % 2) * 128:(c2 % 2 + 1) * 128], bg, tb)
                    if tb + 1 < 4:
                        ln_apply(tb + 1, c2)
                    sl = c2 % 2
                    S.op("act", lambda e, sl=sl, bg=bg: e.activation(out=sgc[sl][:], in_=psum[:, bg, :], func=AF.Silu),
                         reads=[pbuf[bg]] + RG, writes=[b_sgc[sl]])
                    S.op("dve", lambda e, sl=sl, c2=c2, bk=bk: e.tensor_tensor(out=yconvT[:, c2, tsl], in0=psum[:, bk, :], in1=sgc[sl][:],
                                                                              op=ALU.mult),
                         reads=[pbuf[bk], b_sgc[sl]] + RG, writes=[b_yconv[tb]])

            phase_barrier()
            attn_scale = (8 ** -0.5) * (D_IDX ** -0.5)
            S.op("pool", lambda e: e.memset(vaug[:, :, :, 64:65], 1.0), reads=RG, writes=[b_vaug])
            load_w([(lambda stg: stg[:].rearrange("p (k n) -> p k n", k=2), None)], None,
                   [(wuqb[:], lambda stg: stg[:].rearrange("p (k n) -> p k n", k=2), [b_wuqb])])
            load_w([(lambda stg: stg[:, 0:1024].rearrange("p (k n) -> p k n", k=2), None),
                    (lambda stg: stg[:, 1024:1536], None)], None,
                   [(wqib[:], lambda stg: stg[:, 0:1024].rearrange("p (k n) -> p k n", k=2), [b_wqib]),
                    (wuvb[:], lambda stg: stg[:, 1024:1536], [b_wuvb])])
            s1 = wcnt[0] % 2
            w1 = wbf[s1][:].rearrange("p (k n) -> p k n", k=8)
            load_w([(lambda stg: stage3(stg, 256), win_cols(l, 1536, 1792))], None, [(w1, lambda stg: stage3(stg, 256), [b_wbf[s1]])])
            s2 = wcnt[0] % 2
            w2 = wbf[s2][:].rearrange("p (k n) -> p k n", k=8)
            load_w([(lambda stg: stage3(stg, 256)[:, :, 0:200], win_cols(l, 1792, 1992))], None,
                   [(w2[:, :, 0:200], lambda stg: stage3(stg, 256)[:, :, 0:200], [b_wbf[s2]]),
                    (kdw[:, :, 0:64], lambda stg: stage3(stg, 256)[:, :, 128:192], [b_kdw]),
                    (kdw[:, :, 64:128], lambda stg: stage3(stg, 256)[:, :, 128:192], [b_kdw])])
            for tb in range(4):
                tsl = slice(tb * 512, (tb + 1) * 512)
                wt_reads[0] = [b_wbf[s1]]
                inproj(lambda k: w1[:, k, 0:128], 0, tb)
                inproj(lambda k: w1[:, k, 128:256], 1, tb)
                for ch in range(2):
                    S.op("act", lambda e, ch=ch: e.activation(out=sqc[ch][:], in_=psum[:, ch, :], func=AF.Square),
                         reads=[pbuf[ch]] + RG, writes=[b_sqc[ch]])
                    S.op("pe", lambda e, ch=ch: e.matmul(psum[:, 2, :], lhsT=ones_b[:], rhs=sqc[ch][:], start=(ch == 0), stop=(ch == 1)),
                         reads=[b_onesb, b_sqc[ch]], writes=[pbuf[2]])
                S.op("act", lambda e: e.activation(out=rstdc[0][:], in_=psum[:, 2, :], func=AF.Sqrt, bias=EPS, scale=1.0 / Q_LORA),
                     reads=[pbuf[2]] + RG, writes=[b_rstdc[0]])
                S.op("dve", lambda e: e.reciprocal(out=rstdc[0][:], in_=rstdc[0][:]), reads=[b_rstdc[0]] + RG, writes=[b_rstdc[0]])
                for ch in range(2):
                    S.op("dve", lambda e, ch=ch: e.scalar_tensor_tensor(out=cqnT[:, ch, tsl], in0=psum[:, ch, :], scalar=qng[:, l, ch:ch + 1],
                                                                        in1=rstdc[0][:], op0=ALU.mult, op1=ALU.mult),
                         reads=[pbuf[ch], b_small, b_rstdc[0]] + RG, writes=[b_cqn[2 * tb], b_cqn[2 * tb + 1]])
                wt_reads[0] = [b_wbf[s2]]
                inproj(lambda k: w2[:, k, 0:128], 3, tb)
                S.op("act", lambda e: e.activation(out=sqc[0][:], in_=psum[:, 3, :], func=AF.Square), reads=[pbuf[3]] + RG, writes=[b_sqc[0]])
                S.op("pe", lambda e: e.matmul(psum[:, 4, :], lhsT=ones_b[:], rhs=sqc[0][:], start=True, stop=True),
                     reads=[b_onesb, b_sqc[0]], writes=[pbuf[4]])
                S.op("act", lambda e: e.activation(out=rstdc[1][:], in_=psum[:, 4, :], func=AF.Sqrt, bias=EPS, scale=1.0 / KV_LORA),
                     reads=[pbuf[4]] + RG, writes=[b_rstdc[1]])
                S.op("dve", lambda e: e.reciprocal(out=rstdc[1][:], in_=rstdc[1][:]), reads=[b_rstdc[1]] + RG, writes=[b_rstdc[1]])
                S.op("dve", lambda e: e.scalar_tensor_tensor(out=kvnT[:, tsl], in0=psum[:, 3, :], scalar=kvng[:, l, 0:1], in1=rstdc[1][:],
                                                             op0=ALU.mult, op1=ALU.mult),
                     reads=[pbuf[3], b_small, b_rstdc[1]] + RG, writes=[b_kvn])
                wt_reads[0] = [b_kdw]
                inproj(lambda k: kdw[:, k, :], 5, tb)
                S.op("act", lambda e: e.activation(out=kidx2[:, tsl], in_=psum[:, 5, :], func=AF.Copy), reads=[pbuf[5]] + RG, writes=[b_kidx])
                for t4 in range(4):
                    tt = tb * 4 + t4
                    for k in range(8):
                        S.op("pe", lambda e, k=k, tt=tt, t4=t4: e.matmul(psum[:, 6, t4 * 8:(t4 + 1) * 8], lhsT=hT[:, k, tt * 128:(tt + 1) * 128],
                                                                        rhs=w2[:, k, 192:200], start=(k == 0), stop=(k == 7)),
                             reads=[b_hT[tb], b_wbf[s2]], writes=[pbuf[6]])
                S.op("act", lambda e, tb=tb: e.activation(out=wtok[:, tb * 4:(tb + 1) * 4, :],
                                                          in_=psum[:, 6, 0:32].rearrange("p (t h) -> p t h", h=8), func=AF.Copy, scale=attn_scale),
                     reads=[pbuf[6]] + RG, writes=[b_wtok])
            for g in range(2):
                sg_ = wcnt[0] % 2
                wg = wbf[sg_][:].rearrange("p (k n) -> p k n", k=8)
                load_w([(lambda stg: stage3(stg, 256), win_cols(l, 1992 + g * 256, 1992 + (g + 1) * 256))], None,
                       [(wg, lambda stg: stage3(stg, 256), [b_wbf[sg_]])])
                for cc in range(2):
                    ch = g * 2 + cc
                    for tb in range(4):
                        bk = (cc * 4 + tb) % 2
                        wt_reads[0] = [b_wbf[sg_]]
                        inproj(lambda k: wg[:, k, cc * 128:(cc + 1) * 128], bk, tb)
                        S.op("act", lambda e, ch=ch, tb=tb, bk=bk: e.activation(out=sgT[:, ch, tb * 512:(tb + 1) * 512], in_=psum[:, bk, :],
                                                                               func=AF.Silu),
                             reads=[pbuf[bk]] + RG, writes=[b_sg[2 * tb], b_sg[2 * tb + 1]])
            for stl in range(NT):
                bk = 2 + (stl % 2)
                S.op("pe", lambda e, stl=stl, bk=bk: e.matmul(psum[:, bk, :], lhsT=kvnT[:, stl * 128:(stl + 1) * 128], rhs=wuvb[:],
                                                             start=True, stop=True), reads=[b_kvn, b_wuvb], writes=[pbuf[bk]])
                S.op("act", lambda e, stl=stl, bk=bk: e.activation(out=vaug[:, stl, :, 0:64],
                                                                   in_=psum[:, bk, :].rearrange("p (h d) -> p h d", h=N_HEADS), func=AF.Copy),
                     reads=[pbuf[bk]] + RG, writes=[b_vaug])

            phase_barrier()
            sm_scale = KV_LORA ** -0.5
            cnt_r = [0]; cnt_e = [0]
            DSK = 2
            psb = psum[:, 7, :].bitcast(BF16).rearrange("p (j t) -> p j t", t=128)

            def gen_Qq(B):
                qs = B % 2
                tcols = slice(B * 256, (B + 1) * 256)
                for hp in range(4):
                    for hh in range(2):
                        h = hp * 2 + hh
                        for ch in range(2):
                            S.op("pe", lambda e, h=h, hh=hh, ch=ch: e.matmul(psum[:, 7, hh * 256:(hh + 1) * 256],
                                                                             lhsT=wuqb[:, ch, h * 128:(h + 1) * 128], rhs=cqnT[:, ch, tcols],
                                                                             start=(ch == 0), stop=(ch == 1)),
                                 reads=[b_wuqb, b_cqn[B]], writes=[pbuf[7]])
                    S.op("act", lambda e, hp=hp: e.activation(out=qblk[qs][:, hp * 2:hp * 2 + 2, :],
                                                              in_=psum[:, 7, :].rearrange("p (h t) -> p h t", h=2), func=AF.Copy),
                         reads=[pbuf[7]] + RG, writes=[b_qblk[qs]])
                    yield 1.0

            def gen_Qi(B):
                qs = B % 2
                tcols = slice(B * 256, (B + 1) * 256)
                for pp in range(2):
                    for pq in range(2):
                        pr = pp * 2 + pq
                        for ch in range(2):
                            S.op("pe", lambda e, pr=pr, pq=pq, ch=ch: e.matmul(psum[:, 7, pq * 256:(pq + 1) * 256],
                                                                               lhsT=wqib[:, ch, pr * 128:(pr + 1) * 128], rhs=cqnT[:, ch, tcols],
                                                                               start=(ch == 0), stop=(ch == 1)),
                                 reads=[b_wqib, b_cqn[B]], writes=[pbuf[7]])
                    S.op("act", lambda e, pp=pp: e.activation(out=qiblk[qs][:, pp * 2:pp * 2 + 2, :],
                                                              in_=psum[:, 7, :].rearrange("p (h t) -> p h t", h=2), func=AF.Copy),
                         reads=[pbuf[7]] + RG, writes=[b_qiblk[qs]])
                    yield 1.0

            def gen_D(i):
                dw = dgw[i % 2]; bdw = b_dgw[i % 2]
                for h in range(N_HEADS):
                    S.op("dve", lambda e, h=h: e.tensor_scalar(out=dw[:, h, :], in0=ident_b[:], scalar1=wtok[:, i, h:h + 1],
                                                               scalar2=None, op0=ALU.mult),
                         reads=[b_identb, b_wtok] + RG, writes=[bdw])
                yield 1.0

            def gen_X(i):
                B, tl = i // 2, i % 2
                qs = B % 2
                Lk = 128 * (i + 1)
                sc = score[i % 2]; bsc = b_score[i % 2]
                dw = dgw[i % 2]; bdw = b_dgw[i % 2]
                nsb = (Lk + 511) // 512

                def emit_dm(item):
                    sbk, h, rs_, c0, w = item
                    S.op("pe", lambda e: e.matmul(psum[:, 6, 0:w], lhsT=dw[:, h, :], rhs=rt[rs_][:, 0:w],
                                                  start=(h == 0), stop=(h == N_HEADS - 1)),
                         reads=[bdw, b_rt[rs_]], writes=[pbuf[6]])
                    if h == N_HEADS - 1:
                        last = (sbk == nsb - 1)
                        wc = w - 128 if last else w
                        if wc > 0:
                            S.op("act", lambda e: e.activation(out=sc[:, c0:c0 + wc], in_=psum[:, 6, 0:wc], func=AF.Copy),
                                 reads=[pbuf[6]] + RG, writes=[bsc])
                        if last:
                            S.op("dve", lambda e: e.tensor_tensor(out=sc[:, c0 + wc:c0 + wc + 128], in0=psum[:, 6, wc:wc + 128],
                                                                  in1=causal[:], op=ALU.add),
                                 reads=[pbuf[6], b_causal] + RG, writes=[bsc])

                pend = []
                for sbk in range(nsb):
                    c0 = sbk * 512
                    w = min(Lk, c0 + 512) - c0
                    for pr in range(4):
                        items = []
                        for hf in range(2):
                            h = pr * 2 + hf
                            rb = 4 + hf
                            rs_ = cnt_r[0] % 4
                            cnt_r[0] += 1
                            S.op("pe", lambda e, hf=hf, rb=rb: e.matmul(
                                psum[:, rb, 0:w], lhsT=qiblk[qs][hf * 64:(hf + 1) * 64, pr, tl * 128:(tl + 1) * 128],
                                rhs=kidx2[hf * 64:(hf + 1) * 64, c0:c0 + w], start=True, stop=True),
                                reads=[b_qiblk[qs], b_kidx], writes=[pbuf[rb]])
                            items.append((sbk, h, rs_, c0, w, rb))
                        for (sbk_, h, rs_, c0_, w_, rb) in items:
                            S.op("act", lambda e, rb=rb, rs_=rs_: e.activation(out=rt[rs_][:, 0:w], in_=psum[:, rb, 0:w], func=AF.Relu),
                                 reads=[pbuf[rb]] + RG, writes=[b_rt[rs_]])
                        for it in pend:
                            emit_dm(it)
                        pend = [it[:5] for it in items]
                        yield 2.0
                for it in pend:
                    emit_dm(it)
                yield 1.0

            dve_counting = [False]

            def gen_Y(i):
                Lk = 128 * (i + 1)
                sc = score[i % 2]; bsc = b_score[i % 2]
                mk = maskts[i % 2]; bmk = b_maskts[i % 2]
                if i < 2:
                    thr_ap = negbig[:, 0:1]
                    thr_reads = [b_negbig]
                else:
                    dve_counting[0] = True
                    S.op("dve", lambda e: e.tensor_reduce(out=bis[:, 0:1], in_=sc[:, 0:Lk], axis=AX.X, op=ALU.max),
                         reads=[bsc] + RG, writes=[b_bis])
                    yield 2.0
                    S.op("dve", lambda e: e.tensor_reduce(out=bis[:, 1:2], in_=sc[:, 0:128 * i], axis=AX.X, op=ALU.min),
                         reads=[bsc] + RG, writes=[b_bis])
                    yield 2.0
                    S.op("dve", lambda e: e.tensor_tensor(out=bis[:, 2:3], in0=bis[:, 0:1], in1=bis[:, 1:2], op=ALU.subtract),
                         reads=[b_bis] + RG, writes=[b_bis])
                    S.op("dve", lambda e: e.tensor_scalar(out=steps[:], in0=pow2[:], scalar1=bis[:, 2:3], scalar2=None, op0=ALU.mult),
                         reads=[b_pow2, b_bis] + RG, writes=[b_steps])
                    S.op("dve", lambda e: e.tensor_tensor(out=cands[:, 0:1], in0=bis[:, 1:2], in1=steps[:, 0:1], op=ALU.add),
                         reads=[b_bis, b_steps] + RG, writes=[b_cands[0]])
                    yield 1.0
                    for j in range(NB):
                        S.op("dve", lambda e, j=j: e.tensor_scalar(out=mk[:, 0:Lk], in0=sc[:, 0:Lk], scalar1=cands[:, j:j + 1],
                                                                   scalar2=None, op0=ALU.is_ge, op1=ALU.add,
                                                                   accum_out=cnts[:, j:j + 1]),
                             reads=[bsc, b_cands[j]] + RG, writes=[bmk, b_cnts[j]])
                        S.op("dve", lambda e, j=j: e.tensor_scalar(out=sels[:, j:j + 1], in0=cnts[:, j:j + 1], scalar1=TOPK - 0.5,
                                                                   scalar2=steps[:, j:j + 1], op0=ALU.is_ge, op1=ALU.mult),
                             reads=[b_cnts[j], b_steps] + RG, writes=[b_sels[j]])
                        S.op("dve", lambda e, j=j: e.scalar_tensor_tensor(out=cands[:, j + 1:j + 2], in0=sels[:, j:j + 1],
                                                                          scalar=cands[:, j:j + 1], in1=steps[:, j + 1:j + 2],
                                                                          op0=ALU.add, op1=ALU.subtract),
                             reads=[b_sels[j], b_cands[j], b_steps] + RG, writes=[b_cands[j + 1]])
                        yield 2.5
                    thr_ap = cands[:, NB:NB + 1]
                    thr_reads = [b_cands[NB]]
                S.op("dve", lambda e: e.tensor_scalar(out=mk[:, 0:Lk], in0=sc[:, 0:Lk], scalar1=thr_ap, scalar2=None, op0=ALU.is_ge),
                     reads=[bsc] + thr_reads + RG, writes=[bmk])
                dve_counting[0] = False
                yield 1.0

            def gen_Z(i):
                tl = i % 2
                mk = maskts[i % 2]; bmk = b_maskts[i % 2]
                for j0 in range(0, i + 1, 8):
                    n = min(8, i + 1 - j0)
                    for jj in range(n):
                        j = j0 + jj
                        S.op("pe", lambda e, j=j, jj=jj: e.transpose(out=psb[:, jj, :], in_=mk[:, j * 128:(j + 1) * 128], identity=ident_b[:]),
                             reads=[bmk, b_identb], writes=[pbuf[7]])
                    S.op("act", lambda e, j0=j0, n=n: e.activation(out=maskT[:, j0:j0 + n, tl * 128:(tl + 1) * 128], in_=psb[:, 0:n, :],
                                                                   func=AF.Copy),
                         reads=[pbuf[7]] + RG, writes=[b_maskT[tl]])
                    yield 1.0

            def gen_W(B):
                i0, i1 = B * 2, B * 2 + 1
                qs = B % 2
                tcols = slice(B * 256, (B + 1) * 256)
                ob = [0, 1]

                def emit_pv(item):
                    h, j, es, c0 = item
                    for tl in range(2):
                        if tl * 128 < c0:
                            continue
                        lastj = i0 if tl == 0 else i1
                        S.op("pe", lambda e, tl=tl, lastj=lastj: e.matmul(
                            psum[:, ob[tl], 0:65], lhsT=pm[es][:, tl * 128:(tl + 1) * 128], rhs=vaug[:, j, h, :],
                            start=(j == 0), stop=(j == lastj)),
                            reads=[b_pm[es], b_vaug], writes=[pbuf[ob[tl]]])
                    if j == i1:
                        for tl in range(2):
                            S.op("act", lambda e, tl=tl: e.activation(out=osb[:, tl, h, :], in_=psum[:, ob[tl], 0:65], func=AF.Copy),
                                 reads=[pbuf[ob[tl]]] + RG, writes=[b_osb])

                pend = []
                for h in range(N_HEADS):
                    for j in range(i1 + 1):
                        c0 = 0 if j <= i0 else 128
                        qb = 2 + (cnt_e[0] % 2)
                        es = cnt_e[0] % 4
                        cnt_e[0] += 1
                        S.op("pe", lambda e, j=j, c0=c0, qb=qb, h=h: e.matmul(psum[:, qb, c0:256], lhsT=kvnT[:, j * 128:(j + 1) * 128],
                                                                             rhs=qblk[qs][:, h, c0:256], start=True, stop=True),
                             reads=[b_kvn, b_qblk[qs]], writes=[pbuf[qb]])
                        S.op("act", lambda e, c0=c0, qb=qb, es=es, h=h: e.activation(out=et[es][:, c0:256], in_=psum[:, qb, c0:256], func=AF.Exp,
                                                                                    bias=b31bc[:, h:h + 1], scale=sm_scale),
                             reads=[pbuf[qb], b_b31] + RG, writes=[b_et[es]])
                        meng = "pool" if dve_counting[0] else "dve"
                        if j >= i0 - 1:
                            if j == i0 - 1:
                                tc0, tc1, u0 = 0, 128, 128
                            elif j == i0:
                                tc0, tc1, u0 = 0, 256, 0
                            else:
                                tc0, tc1, u0 = 128, 256, 0
                            S.op(meng, lambda e, es=es, tc0=tc0, tc1=tc1, u0=u0, h=h: e.tensor_tensor(
                                out=et[es][:, tc0:tc1], in0=et[es][:, tc0:tc1], in1=tbc[:, h, u0:u0 + (tc1 - tc0)], op=ALU.mult),
                                reads=[b_et[es], b_tbc] + RG, writes=[b_et[es]])
                        S.op(meng, lambda e, es=es, c0=c0, j=j: e.tensor_tensor(out=pm[es][:, c0:256], in0=et[es][:, c0:256], in1=maskT[:, j, c0:256],
                                                                               op=ALU.mult),
                             reads=[b_et[es], b_maskT[0], b_maskT[1]] + RG, writes=[b_pm[es]])
                        pend.append((h, j, es, c0))
                        if len(pend) > DSK:
                            emit_pv(pend.pop(0))
                        yield 1.0
                while pend:
                    emit_pv(pend.pop(0))
                yield 1.0
                S.op("dve", lambda e: e.reciprocal(out=rcp[:], in_=osb[:, :, :, 64]), reads=[b_osb] + RG, writes=[b_rcp])
                S.op("dve", lambda e: e.tensor_tensor(out=ytok[:].rearrange("p t (h d) -> p t h d", h=N_HEADS), in0=osb[:, :, :, 0:64],
                                                      in1=rcp[:].unsqueeze(3).to_broadcast([128, 2, N_HEADS, 64]), op=ALU.mult),
                     reads=[b_osb, b_rcp] + RG, writes=[b_ytok[0], b_ytok[1]])
                for ch in range(4):
                    hb = (ch % 2) * 256
                    for tl in range(2):
                        S.op("pe", lambda e, ch=ch, tl=tl, hb=hb: e.transpose(out=psum[:, 7, hb + tl * 128:hb + (tl + 1) * 128],
                                                                             in_=ytok[:, tl, ch * 128:(ch + 1) * 128], identity=ident_f[:]),
                             reads=[b_ytok[tl], b_identf], writes=[pbuf[7]])
                    S.op("dve", lambda e, ch=ch, hb=hb: e.tensor_tensor(out=sgT[:, ch, tcols], in0=psum[:, 7, hb:hb + 256], in1=sgT[:, ch, tcols],
                                                                        op=ALU.mult),
                         reads=[pbuf[7], b_sg[B]] + RG, writes=[b_sg[B]])
                yield 1.0

            def run(g):
                for _ in g:
                    pass

            def chain(*gens):
                for g in gens:
                    for wgt in g:
                        yield wgt

            def par(ga, gb):
                ta = tb_ = 0.0
                a_done = b_done = False
                while not (a_done and b_done):
                    if b_done or (not a_done and ta <= tb_):
                        try:
                            wgt = next(ga); ta += wgt
                            yield wgt * 0.5
                        except StopIteration:
                            a_done = True
                            ta = float("inf")
                    else:
                        try:
                            wgt = next(gb); tb_ += wgt
                            yield wgt * 0.5
                        except StopIteration:
                            b_done = True
                            tb_ = float("inf")

            def interleave(ga, na, gb, nb_):
                a_done = b_done = False
                pa = pb_ = 0.0
                while not (a_done and b_done):
                    if b_done or (not a_done and pa * nb_ <= pb_ * na):
                        try:
                            pa += next(ga)
                        except StopIteration:
                            a_done = True
                    else:
                        try:
                            pb_ += next(gb)
                        except StopIteration:
                            b_done = True

            def n_x(i):
                return 1.0 + 8.0 * ((128 * (i + 1) + 511) // 512)

            def n_y(i):
                return 1.0 if i < 2 else 2.5 * NB + 5.0

            NBLK = NT // 2

            def nothing():
                return
                yield 0.0

            order = list(range(NBLK - 1, -1, -1))

            def prep_x(B):
                return chain(gen_Qi(B), gen_D(2 * B), gen_D(2 * B + 1), gen_X(2 * B))

            def n_prep(B):
                return 4.0 + n_x(2 * B)

            o0 = order[0]
            run(gen_Qq(o0)); run(prep_x(o0))
            run(par(gen_Y(2 * o0), gen_X(2 * o0 + 1)))
            run(par(gen_Y(2 * o0 + 1), prep_x(order[1])))
            run(gen_Z(2 * o0)); run(gen_Z(2 * o0 + 1))
            for n, B in enumerate(order):
                if n + 1 < NBLK:
                    B1 = order[n + 1]
                    i2, i3 = 2 * B1, 2 * B1 + 1
                    if n + 2 < NBLK:
                        nxt = prep_x(order[n + 2])
                        n_nxt = n_prep(order[n + 2])
                    else:
                        nxt = nothing()
                        n_nxt = 0.0
                    side = chain(gen_Qq(B1), par(gen_Y(i2), gen_X(i3)), par(gen_Y(i3), nxt))
                    n_side = 4.0 + 0.5 * (n_y(i2) + n_x(i3)) + 0.5 * (n_y(i3) + n_nxt)
                    interleave(gen_W(B), 8.0 * (2 * B + 2) + 2.0, side, n_side)
                    run(gen_Z(i2)); run(gen_Z(i3))
                else:
                    run(gen_W(B))

            phase_barrier()
            S.op("dve", lambda e: e.tensor_tensor(out=gvT[:], in0=modT[:, l, 16:24, b], in1=gpostT[:, l, :], op=ALU.mult),
                 reads=[b_modT, b_small] + RG, writes=[b_gvT])
            bcast_rows(lambda k: gvT[:, k:k + 1], gbc, b_gbc, dgE, b_dgE, [b_gvT])
            for g in range(4):
                load_w([(lambda stg: stage3(stg, 256), None)], None,
                       [(woutb[:, :, g * 256:(g + 1) * 256], lambda stg: stage3(stg, 256), [b_woutb[g]])])
            for tt in range(NT):
                tsl = slice(tt * 128, (tt + 1) * 128)
                for nh in range(2):
                    bk = (tt * 2 + nh) % 4
                    for k in range(8):
                        lhs = yconvT[:, k, tsl] if k < 4 else sgT[:, k - 4, tsl]
                        rd = [b_yconv[tt // 4]] if k < 4 else [b_sg[tt // 2]]
                        S.op("pe", lambda e, lhs=lhs, k=k, nh=nh, bk=bk: e.matmul(psum[:, bk, :], lhsT=lhs, rhs=woutb[:, k, nh * 512:(nh + 1) * 512],
                                                                                 start=(k == 0), stop=(k == 7)),
                             reads=rd + [b_woutb[2 * nh], b_woutb[2 * nh + 1]], writes=[pbuf[bk]])
                    S.op("act", lambda e, nh=nh, bk=bk: e.activation(out=ejunk[:], in_=psum[:, bk, :], func=AF.Square, accum_out=ess[:, nh:nh + 1]),
                         reads=[pbuf[bk]] + RG, writes=[b_ejunk, b_ess])
                S.op("dve", lambda e: e.tensor_tensor(out=ess[:, 2:3], in0=ess[:, 0:1], in1=ess[:, 1:2], op=ALU.add),
                     reads=[b_ess] + RG, writes=[b_ess])
                S.op("act", lambda e: e.activation(out=ess[:, 2:3], in_=ess[:, 2:3], func=AF.Sqrt, bias=EPS, scale=1.0 / D_MODEL),
                     reads=[b_ess] + RG, writes=[b_ess])
                S.op("dve", lambda e: e.reciprocal(out=ess[:, 3:4], in_=ess[:, 2:3]), reads=[b_ess] + RG, writes=[b_ess])
                for nh in range(2):
                    bk = (tt * 2 + nh) % 4
                    S.op("dve", lambda e, nh=nh, bk=bk: e.scalar_tensor_tensor(out=etmp[nh][:], in0=psum[:, bk, :], scalar=ess[:, 3:4],
                                                                               in1=gbc[:, nh * 512:(nh + 1) * 512], op0=ALU.mult, op1=ALU.mult),
                         reads=[pbuf[bk], b_ess, b_gbc] + RG, writes=[b_etmp[nh]])
                    S.op("dve", lambda e, nh=nh, tt=tt: e.tensor_tensor(out=xs_res[:, tt, nh * 512:(nh + 1) * 512],
                                                                         in0=xs_res[:, tt, nh * 512:(nh + 1) * 512], in1=etmp[nh][:], op=ALU.add),
                         reads=[b_etmp[nh], b_x[tt]] + RG, writes=[b_x[tt]])
                if last_layer and (tt % 4 == 3):
                    t0 = tt - 3
                    S.op("sp", lambda e, t0=t0: e.dma_start(out=out_d[b, t0 * 128:(t0 + 4) * 128, :].rearrange("(n p) d -> p n d", p=128),
                                                            in_=xs_res[:, t0:t0 + 4, :]),
                         reads=[b_x[t0], b_x[t0 + 1], b_x[t0 + 2], b_x[t0 + 3]])

        phase_barrier([b_pro, b_gc, b_tbrev, b_scr, b_wada[0], b_wada[1], b_tbc, b_modT])
        for b in range(nseq):
            for q4 in range(4):
                S.op("sp", lambda e, b=b, q4=q4: e.dma_start(out=xs_res[:, q4 * 4:(q4 + 1) * 4, :],
                                                             in_=x_d[b, q4 * 512:(q4 + 1) * 512, :].rearrange("(n p) d -> p n d", p=128)),
                     writes=[b_x[q4 * 4 + i] for i in range(4)])
            for li, l in enumerate(layers):
                block(b, l, li == len(layers) - 1)
        S.finish()
        S.emit()
        build_program.nins = S.nins
    return nc


def _fm(v, nchunk):
    L = v.shape[0]
    return np.ascontiguousarray(v.reshape(L, nchunk, 128).transpose(2, 0, 1)).astype(np.float32)


def pack_weights(inputs):
    f = lambda a: np.asarray(a, dtype=np.float32)
    w_in, w_pw2, w_uq, w_qidx, w_uv, w_out = (f(inputs[k]) for k in ("w_in", "w_pw2", "w_uq", "w_qidx", "w_uv", "w_out"))
    packs = np.zeros((DEPTH, 17, 128, 2048), np.float32)
    for l in range(DEPTH):
        win = w_in[l].reshape(8, 128, D_IN_PROJ).transpose(1, 0, 2)
        jobs = []
        jobs.append(w_pw2[l].reshape(4, 128, 512).transpose(1, 0, 2).reshape(128, 2048))
        for c in range(4):
            t = np.zeros((128, 8, 256), np.float32)
            t[:, :, 0:128] = win[:, :, c * 128:(c + 1) * 128]
            t[:, :, 128:256] = win[:, :, 512 + c * 128:512 + (c + 1) * 128]
            jobs.append(t.reshape(128, 2048))
        for g in range(2):
            jobs.append(win[:, :, 1024 + g * 256:1024 + (g + 1) * 256].reshape(128, 2048))
        jobs.append(w_uq[l].reshape(2, 128, 1024).transpose(1, 0, 2).reshape(128, 2048))
        t = np.zeros((128, 2048), np.float32)
        t[:, 0:1024] = w_qidx[l].reshape(2, 128, 512).transpose(1, 0, 2).reshape(128, 1024)
        t[:, 1024:1536] = w_uv[l].transpose(1, 0, 2).reshape(KV_LORA, N_HEADS * 64)
        jobs.append(t)
        jobs.append(win[:, :, 1536:1792].reshape(128, 2048))
        t = np.zeros((128, 8, 256), np.float32)
        t[:, :, 0:200] = win[:, :, 1792:1992]
        jobs.append(t.reshape(128, 2048))
        for g in range(2):
            jobs.append(win[:, :, 1992 + g * 256:1992 + (g + 1) * 256].reshape(128, 2048))
        wo = w_out[l].reshape(8, 128, D_MODEL).transpose(1, 0, 2)
        for g in range(4):
            jobs.append(wo[:, :, g * 256:(g + 1) * 256].reshape(128, 2048))
        assert len(jobs) == 17
        for j, t in enumerate(jobs):
            packs[l, j] = t
    return packs


def make_in_maps(inputs, ncores=NCORES, nseq=SEQ_PER_CORE):
    f = lambda a: np.ascontiguousarray(np.asarray(a, dtype=np.float32))
    x = f(inputs["x"]); c = f(inputs["c"])
    n = np.arange(NG) - 127
    oh = np.zeros((N_BUCKETS, NG), np.float32)
    oh[t5_bucket_np(n), np.arange(NG)] = 1.0
    conv_w = f(inputs["conv_w"])
    conv_wT = np.ascontiguousarray(conv_w.reshape(DEPTH, CONV_WIDTH, 4, 128).transpose(3, 0, 2, 1))
    rel_bias = f(inputs["rel_bias"])
    w_ada = f(inputs["w_ada"])
    w_adaP = np.ascontiguousarray(w_ada.reshape(DEPTH, 8, 128, 8, 384).transpose(0, 3, 2, 1, 4))
    shared = {
        "w_adaP": w_adaP, "b_adaT": _fm(f(inputs["b_ada"]), 24), "g_preT": _fm(f(inputs["g_pre"]), 8),
        "g_postT": _fm(f(inputs["g_post"]), 8), "wpack": pack_weights(inputs), "conv_wT": conv_wT,
        "conv_bT": _fm(f(inputs["conv_b"]), 4), "ln_gT": _fm(f(inputs["conv_ln_g"]), 4), "ln_bT": _fm(f(inputs["conv_ln_b"]), 4),
        "q_norm_gT": _fm(f(inputs["q_norm_g"]), 2), "kv_norm_gT": _fm(f(inputs["kv_norm_g"]), 1),
        "rel_bias": rel_bias, "rel_biasT": np.ascontiguousarray(rel_bias.T), "oh": oh,
    }
    maps = []
    for i in range(ncores):
        xb = x[i * nseq:(i + 1) * nseq]
        cb = c[i * nseq:(i + 1) * nseq]
        cTb = np.ascontiguousarray(cb.reshape(nseq, 8, 128).transpose(2, 1, 0))
        m = dict(shared)
        m["x"] = np.ascontiguousarray(xb)
        m["cT"] = cTb
        maps.append(m)
    return maps


def kernel(**inputs):
    nc = build_program(layers=(0, 1), nseq=SEQ_PER_CORE)
    in_maps = make_in_maps(inputs)
    res = run_bass_kernel_spmd(nc, in_maps, core_ids=list(range(NCORES)))
    outs = [np.asarray(r["out"], dtype=np.float32) for r in res.results]
    return np.concatenate(outs, axis=0)
```

```python
import math
from contextlib import ExitStack

import numpy as np
import concourse.bass as bass
import concourse.mybir as mybir
from concourse.bass_utils import run_bass_kernel_spmd

F32 = mybir.dt.float32
BF16 = mybir.dt.bfloat16
AF = mybir.ActivationFunctionType
ALU = mybir.AluOpType
AX = mybir.AxisListType

D_MODEL = 1024
SEQ = 2048
NT = SEQ // 128
DEPTH = 2
D_CONV = 512
CONV_WIDTH = 31
N_HEADS = 8
KV_LORA = 128
Q_LORA = 256
D_IDX = 64
TOPK = 256
N_BUCKETS = 32
MAX_DISTANCE = 128
EPS = 1e-6
D_IN_PROJ = 2504
NB = 14
NCORES = 8
SEQ_PER_CORE = 2
NG = 383


class Src:
    def __init__(self, name, sem, inc):
        self.name, self.sem, self.inc, self.count = name, sem, inc, 0


class Buf:
    __slots__ = ("name", "last_w", "readers")

    def __init__(self, name=""):
        self.name, self.last_w, self.readers = name, None, []


class _Rec:
    def __init__(self):
        self.call = None

    def __getattr__(self, name):
        def f(*a, **k):
            self.call = (name, a, k)
            return self
        return f


class Sched:
    ENG = ("pe", "act", "dve", "pool", "sp")

    def __init__(self, nc, stack, ndma=32):
        self.nc = nc
        self.src = {}
        self.prog = {e: [] for e in self.ENG}
        self.waited = {e: {} for e in self.ENG}
        for e in self.ENG:
            if e == "sp":
                continue
            self.src[e] = Src(e, stack.enter_context(nc.semaphore("sem_" + e)), 1)
        self.ring = [Src("dma%d" % i, stack.enter_context(nc.semaphore("semd%d" % i)), 16) for i in range(ndma)]
        self.ring_pos = 0
        self.nins = 0

    def _deps(self, eng, reads, writes):
        need = {}

        def add(tok):
            if tok is None:
                return
            s, c = tok
            if need.get(s, 0) < c:
                need[s] = c

        for b in reads:
            add(b.last_w)
        for b in writes:
            add(b.last_w)
            for r in b.readers:
                add(r)
        w = self.waited[eng]
        for s, c in need.items():
            if s.name == eng and eng == "pe":
                continue
            if w.get(s, 0) >= c:
                continue
            w[s] = c
            self.prog[eng].append(("w", s, c))

    def op(self, eng, fn, reads=(), writes=()):
        self._deps(eng, reads, writes)
        if eng == "sp":
            s = self.ring[self.ring_pos]
            self.ring_pos = (self.ring_pos + 1) % len(self.ring)
            if s.count and self.waited["sp"].get(s, 0) < s.count:
                self.waited["sp"][s] = s.count
                self.prog["sp"].append(("w", s, s.count))
        else:
            s = self.src[eng]
        s.count += s.inc
        rec = _Rec()
        fn(rec)
        assert rec.call is not None
        self.prog[eng].append(("i", rec.call, s, s.count))
        tok = (s, s.count)
        for b in writes:
            b.last_w = tok
            b.readers = []
        for b in reads:
            if len(b.readers) > 64:
                best = {}
                for (ss, cc) in b.readers:
                    if best.get(ss, 0) < cc:
                        best[ss] = cc
                b.readers = list(best.items())
            b.readers.append(tok)
        self.nins += 1
        return tok

    def finish(self):
        for s in self.ring:
            if s.count:
                self.prog["sp"].append(("w", s, s.count))

    def emit(self):
        nc = self.nc
        engmap = {"pe": "tensor", "act": "scalar", "dve": "vector", "pool": "gpsimd", "sp": "sync"}
        miles = {}
        for e in self.ENG:
            for it in self.prog[e]:
                if it[0] == "w" and it[1].inc == 1:
                    miles.setdefault(it[1], set()).add(it[2])
        rank = {}
        for src, st in miles.items():
            for r, idx in enumerate(sorted(st)):
                rank[(src, idx)] = r + 1
        self.n_inc = sum(len(v) for v in miles.values())
        with nc.Block() as block:
            for e in self.ENG:
                prog = self.prog[e]

                def body(engine, prog=prog):
                    for it in prog:
                        src = it[1] if it[0] == "w" else it[2]
                        if it[0] == "w":
                            if src.inc == 1:
                                engine.wait_ge(src.sem, rank[(src, it[2])])
                            else:
                                engine.wait_ge(src.sem, it[2])
                        else:
                            name, a, k = it[1]
                            ins = getattr(engine, name)(*a, **k)
                            if src.inc != 1:
                                ins.then_inc(src.sem, src.inc)
                            elif (src, it[3]) in rank:
                                ins.then_inc(src.sem, 1)

                getattr(block, engmap[e])(body)


def t5_bucket_np(n):
    max_exact = N_BUCKETS // 2
    n = np.maximum(n, 0)
    nf = np.maximum(n, 1).astype(np.float32)
    large = max_exact + (np.log(nf / np.float32(max_exact)) / np.float32(math.log(MAX_DISTANCE / max_exact))
                         * np.float32(N_BUCKETS - max_exact)).astype(np.int32)
    large = np.minimum(large, N_BUCKETS - 1)
    return np.where(n < max_exact, n, large)


def build_program(layers=(0, 1), nseq=SEQ_PER_CORE, debug=None):
    nc = bass.Bass("TRN2", target_bir_lowering=False)
    L = DEPTH
    dram = {}

    def din(name, shape):
        dram[name] = nc.dram_tensor(name, list(shape), F32, kind="ExternalInput").ap()
        return dram[name]

    x_d = din("x", [nseq, SEQ, D_MODEL])
    cT_d = din("cT", [128, 8, nseq])
    wada_d = din("w_adaP", [L, 8, 128, 8, 384])
    NJOB = 17
    wpack_d = din("wpack", [L, NJOB, 128, 2048])
    badaT_d = din("b_adaT", [128, L, 24])
    gpreT_d = din("g_preT", [128, L, 8])
    gpostT_d = din("g_postT", [128, L, 8])
    convw_d = din("conv_wT", [128, L, 4, CONV_WIDTH])
    convb_d = din("conv_bT", [128, L, 4])
    lng_d = din("ln_gT", [128, L, 4])
    lnb_d = din("ln_bT", [128, L, 4])
    qng_d = din("q_norm_gT", [128, L, 2])
    kvng_d = din("kv_norm_gT", [128, L, 1])
    relb_d = din("rel_bias", [N_BUCKETS, N_HEADS])
    relbT_d = din("rel_biasT", [N_HEADS, N_BUCKETS])
    oh_d = din("oh", [N_BUCKETS, NG])
    out_d = nc.dram_tensor("out", [nseq, SEQ, D_MODEL], F32, kind="ExternalOutput").ap()
    scr_d = nc.dram_tensor("scr_g", [N_HEADS, NG], F32, kind="Internal").ap()
    dbg_d = {}
    if debug:
        for name, shape in debug.items():
            dbg_d[name] = nc.dram_tensor("dbg_" + name, list(shape), F32, kind="ExternalOutput").ap()

    with ExitStack() as st:
        S = Sched(nc, st)
        off = [16640]

        def sb(name, shape, dt, at=None):
            nbytes = int(np.prod(shape[1:])) * (4 if dt == F32 else 2)
            nbytes = (nbytes + 31) // 32 * 32
            if at is None:
                at = off[0]
                off[0] += nbytes
            t = nc.alloc_sbuf_tensor_at(name, list(shape), dt, offset=at)
            return t

        psum = nc.alloc_psum_tensor("psum", [128, 8, 512], F32)
        pbuf = [Buf("ps%d" % i) for i in range(8)]

        def pbank(i):
            return psum[:, i, :]

        ident_f = sb("ident_f", [128, 128], F32); b_identf = Buf()
        ident_b = sb("ident_b", [128, 128], BF16); b_identb = Buf()
        ones_f = sb("ones_f", [128, 128], F32); b_onesf = Buf()
        ones_b = sb("ones_b", [128, 128], BF16); b_onesb = Buf()
        jf = sb("jf", [128, 128], F32); b_jf = Buf()
        causal = sb("causal", [128, 128], F32); b_causal = Buf()
        pow2 = sb("pow2", [128, NB + 2], F32); b_pow2 = Buf()
        negbig = sb("negbig", [128, 1], F32); b_negbig = Buf()
        cT = sb("cT", [128, 8, nseq], F32); b_cT = Buf()
        cact = sb("cact", [128, 8, nseq], F32); b_cact = Buf()
        modT = sb("modT", [128, L, 24, nseq], F32); b_modT = Buf()
        badaT = sb("badaT", [128, L, 24], F32); b_small = Buf()
        gpreT = sb("gpreT", [128, L, 8], F32)
        gpostT = sb("gpostT", [128, L, 8], F32)
        convw = sb("convw", [128, L, 4, CONV_WIDTH], F32)
        convb = sb("convb", [128, L, 4], F32)
        lng = sb("lng", [128, L, 4], F32)
        lnb = sb("lnb", [128, L, 4], F32)
        qng = sb("qng", [128, L, 2], F32)
        kvng = sb("kvng", [128, L, 1], F32)
        b31bc = sb("b31bc", [128, N_HEADS], F32); b_b31 = Buf()
        tbc = sb("tbc", [128, N_HEADS, 256], BF16); b_tbc = Buf()
        xs_res = sb("xres", [128, NT, D_MODEL], F32)
        b_x = [Buf("x%d" % i) for i in range(NT)]
        dummy = sb("dummy", [128, 8], F32)
        PB = off[0]
        LIMIT = 229376 - 64
        PX1 = PB
        PX2 = PX1 + 16384
        PX3 = PX2 + 16384
        PX4 = PX3 + 40704
        PX5 = PX4 + 57344

        S.op("pool", lambda e: e.memset(ident_f[:], 1.0), writes=[b_identf])
        S.op("pool", lambda e: e.affine_select(out=ident_f[:], in_=ident_f[:], pattern=[[-1, 128]], compare_op=ALU.is_equal,
                                               fill=0.0, base=0, channel_multiplier=1), reads=[b_identf], writes=[b_identf])
        S.op("pool", lambda e: e.tensor_copy(out=ident_b[:], in_=ident_f[:]), reads=[b_identf], writes=[b_identb])
        S.op("pool", lambda e: e.memset(ones_f[:], 1.0), writes=[b_onesf])
        S.op("pool", lambda e: e.memset(ones_b[:], 1.0), writes=[b_onesb])
        S.op("pool", lambda e: e.memset(jf[:], 1.0), writes=[b_jf])
        S.op("pool", lambda e: e.affine_select(out=jf[:], in_=jf[:], pattern=[[1, 128]], compare_op=ALU.is_equal,
                                               fill=0.0, base=-127, channel_multiplier=1), reads=[b_jf], writes=[b_jf])
        S.op("pool", lambda e: e.memset(causal[:], 0.0), writes=[b_causal])
        S.op("pool", lambda e: e.affine_select(out=causal[:], in_=causal[:], pattern=[[-1, 128]], compare_op=ALU.is_ge,
                                               fill=-1e30, base=0, channel_multiplier=1), reads=[b_causal], writes=[b_causal])
        for j in range(NB + 2):
            v = 2.0 ** (-min(j + 1, NB))
            S.op("pool", lambda e, j=j, v=v: e.memset(pow2[:, j:j + 1], v), writes=[b_pow2])
        S.op("pool", lambda e: e.memset(negbig[:], -1e29), writes=[b_negbig])

        for (t, d) in ((cT, cT_d), (badaT, badaT_d), (gpreT, gpreT_d), (gpostT, gpostT_d), (convw, convw_d), (convb, convb_d),
                       (lng, lng_d), (lnb, lnb_d), (qng, qng_d), (kvng, kvng_d)):
            S.op("sp", lambda e, t=t, d=d: e.dma_start(out=t[:], in_=d), writes=[b_small if t is not cT else b_cT])
        S.op("sp", lambda e: e.dma_start(out=b31bc[:], in_=bass.AP(tensor=relb_d.tensor, offset=31 * N_HEADS,
                                                                    ap=[[0, 128], [1, N_HEADS]])), writes=[b_b31])

        po = PX3
        relb_s = sb("relb_s", [128, N_HEADS], F32, at=po); po += 32
        relbT_s = sb("relbT_s", [128, N_BUCKETS], F32, at=po); po += 128
        nb31 = sb("nb31", [128, 1], F32, at=po); po += 32
        oh_s = sb("oh_s", [128, NG], F32, at=po); po += 1536
        gc_s = sb("gc_s", [128, NG], F32, at=po); po += 1536
        tbrev = sb("tbrev", [128, N_HEADS, 256], F32, at=po); po += 8192
        wada_st = [sb("wada_st%d" % i, [128, 8, 384], F32, at=po + i * 12288) for i in range(2)]
        po += 2 * 12288
        wada_bf = [sb("wada_bf%d" % i, [128, 8, 384], BF16, at=po + i * 6144) for i in range(2)]
        po += 2 * 6144
        cact_bf = sb("cact_bf", [128, 8, nseq], BF16, at=po); po += 64
        b_wadab = [Buf(), Buf()]; b_cactb = Buf()
        assert po <= LIMIT
        b_pro = Buf(); b_gc = Buf(); b_scr = Buf(); b_tbrev = Buf()
        b_wada = [Buf(), Buf()]

        S.op("sp", lambda e: e.dma_start(out=relb_s[0:N_BUCKETS, :], in_=relb_d), writes=[b_pro])
        S.op("sp", lambda e: e.dma_start(out=relbT_s[0:N_HEADS, :], in_=relbT_d), writes=[b_pro])
        S.op("sp", lambda e: e.dma_start(out=oh_s[0:N_BUCKETS, :], in_=oh_d), writes=[b_pro])
        S.op("dve", lambda e: e.tensor_scalar(out=nb31[0:N_HEADS, :], in0=relbT_s[0:N_HEADS, 31:32], scalar1=-1.0, scalar2=None,
                                              op0=ALU.mult), reads=[b_pro], writes=[b_pro])
        S.op("pe", lambda e: e.matmul(psum[0:N_HEADS, 0, 0:NG], lhsT=relb_s[0:N_BUCKETS, :], rhs=oh_s[0:N_BUCKETS, :],
                                      start=True, stop=True), reads=[b_pro], writes=[pbuf[0]])
        S.op("act", lambda e: e.activation(out=gc_s[0:N_HEADS, :], in_=psum[0:N_HEADS, 0, 0:NG], func=AF.Exp,
                                           bias=nb31[0:N_HEADS, 0:1], scale=1.0), reads=[pbuf[0], b_pro], writes=[b_gc])
        S.op("sp", lambda e: e.dma_start(out=scr_d, in_=gc_s[0:N_HEADS, :]), reads=[b_gc], writes=[b_scr])
        S.op("sp", lambda e: e.dma_start(out=tbrev[:], in_=bass.AP(tensor=scr_d.tensor, offset=0,
                                                                    ap=[[1, 128], [NG, N_HEADS], [1, 256]])),
             reads=[b_scr], writes=[b_tbrev])
        for h in range(N_HEADS):
            bk = 1 + (h % 2)
            S.op("pe", lambda e, h=h, bk=bk: e.matmul(psum[:, bk, 0:256], lhsT=jf[:], rhs=tbrev[:, h, :], start=True, stop=True),
                 reads=[b_jf, b_tbrev], writes=[pbuf[bk]])
            S.op("act", lambda e, h=h, bk=bk: e.activation(out=tbc[:, h, :], in_=psum[:, bk, 0:256], func=AF.Copy),
                 reads=[pbuf[bk]], writes=[b_tbc])

        S.op("act", lambda e: e.activation(out=cact[:], in_=cT[:], func=AF.Silu), reads=[b_cT], writes=[b_cact])
        S.op("act", lambda e: e.activation(out=cact_bf[:], in_=cT[:], func=AF.Silu), reads=[b_cT], writes=[b_cactb])
        gi = 0
        for l in layers:
            for g in range(8):
                slot = gi % 2
                gi += 1
                S.op("sp", lambda e, slot=slot, g=g, l=l: e.dma_start(out=wada_st[slot][:], in_=wada_d[l, g]),
                     writes=[b_wada[slot]])
                S.op("dve" if g % 2 == 0 else "pool", lambda e, slot=slot: e.tensor_copy(out=wada_bf[slot][:], in_=wada_st[slot][:]),
                     reads=[b_wada[slot]], writes=[b_wadab[slot]])
                for jj in range(3):
                    j = g * 3 + jj
                    for k in range(8):
                        S.op("pe", lambda e, slot=slot, jj=jj, j=j, k=k: e.matmul(
                            psum[:, 3, j * nseq:(j + 1) * nseq], lhsT=wada_bf[slot][:, k, jj * 128:(jj + 1) * 128],
                            rhs=cact_bf[:, k, :], start=(k == 0), stop=(k == 7)),
                            reads=[b_wadab[slot], b_cactb], writes=[pbuf[3]])
            S.op("dve", lambda e, l=l: e.tensor_tensor(
                out=modT[:, l, :, :], in0=psum[:, 3, 0:24 * nseq].rearrange("p (j b) -> p j b", b=nseq),
                in1=badaT[:, l, :].unsqueeze(2).to_broadcast([128, 24, nseq]), op=ALU.add),
                reads=[pbuf[3], b_small], writes=[b_modT])

        yconvT = sb("yconvT", [128, 4, SEQ], BF16, at=PX1)
        sgT = sb("sgT", [128, 4, SEQ], BF16, at=PX2)
        yc = sb("yc", [128, 4, SEQ], BF16, at=PX2)
        o = PX4
        hT = sb("hT", [128, 8, SEQ], BF16, at=o); o += 32768
        wst = [sb("wst%d" % i, [128, 2048], F32, at=o + i * 8192) for i in range(2)]; o += 16384
        wbf = [sb("wbf%d" % i, [128, 2048], BF16, at=o + i * 4096) for i in range(2)]; o += 8192
        assert o == PX5
        b_hT = [Buf("hT%d" % i) for i in range(4)]
        b_yconv = [Buf() for _ in range(4)]
        b_sg = [Buf() for _ in range(8)]
        b_yc = [Buf() for _ in range(4)]
        b_wst = [Buf(), Buf()]
        b_wbf = [Buf(), Buf()]

        o = PX3
        abc = sb("abc", [128, D_MODEL], F32, at=o); o += 4096
        shbc = sb("shbc", [128, D_MODEL], F32, at=o); o += 4096
        xs = [sb("xs%d" % i, [128, D_MODEL], F32, at=o + i * 4096) for i in range(2)]; o += 8192
        dg = [sb("dg%d" % i, [128, 128], F32, at=o + i * 512) for i in range(2)]; o += 1024
        vecT = sb("vecT", [128, 3, 8], F32, at=o); o += 96
        ssq = sb("ssq", [128, NT], F32, at=o); o += 64
        rs16 = sb("rs16", [128, NT], F32, at=o); o += 64
        rstd16 = sb("rstd16", [128, NT], F32, at=o); o += 64
        assert o <= PX4
        b_abc = Buf(); b_shbc = Buf(); b_xs = [Buf(), Buf()]; b_dg = [Buf(), Buf()]; b_vecT = Buf()
        b_ssq = Buf(); b_rs16 = Buf(); b_rstd16 = Buf()

        o = PX3
        apad = [sb("apad%d" % i, [128, 30 + SEQ], BF16, at=o + i * 4160) for i in range(2)]; o += 8320
        sig = [sb("sig%d" % i, [128, 512], BF16, at=o + i * 1024) for i in range(2)]; o += 2048
        cdiag = sb("cdiag", [128, CONV_WIDTH, 128], BF16, at=o); o += 7936
        ycsq = [sb("ycsq0", [128, 512], BF16, at=o), sb("ycsq1", [128, 512], BF16, at=PX3 + 16384)]; o += 1024
        zt1 = sb("zt1", [128, 4, 512], BF16, at=o); o += 4096
        m2b = sb("m2b", [128, 512], F32, at=o); o += 2048
        varb = sb("varb", [128, 512], F32, at=o); o += 2048
        dtmp = [sb("dtmp0", [128, 512], F32, at=o)] * 2; o += 2048
        zt0 = sb("zt0", [128, 4, 512], BF16, at=o); o += 4096
        ztb = [zt0, zt1]
        sgc = [sb("sgc%d" % i, [128, 512], BF16, at=o + i * 1024) for i in range(2)]; o += 2048
        wpw2b = sb("wpw2b", [128, 4, 512], BF16, at=o); o += 4096
        assert o <= PX4, o
        meanb4 = [sb("meanb4_%d" % i, [128, 512], F32, at=PX3 + i * 2048) for i in range(4)]
        rstdb4 = [sb("rstdb4_%d" % i, [128, 512], F32, at=PX3 + 8192 + i * 2048) for i in range(4)]
        assert 16384 + 1024 <= 8320 + 2048 + 7936
        b_apad = [Buf(), Buf()]; b_sig = [Buf(), Buf()]; b_cdiag = Buf(); b_ycsq = [Buf(), Buf()]
        b_mean4 = [Buf() for _ in range(4)]; b_m2b = Buf(); b_varb = Buf(); b_rstd4 = [Buf() for _ in range(4)]; b_dtmp = [Buf()] * 2
        b_ztb = [[Buf() for _ in range(4)] for _ in range(2)]; b_sgc = [Buf(), Buf()]; b_wpw2b = Buf()

        o = PX3
        cqnT = sb("cqnT", [128, 2, SEQ], BF16, at=o); o += 8192
        kvnT = sb("kvnT", [128, SEQ], BF16, at=o); o += 4096
        kidx2 = sb("kidx2", [128, SEQ], BF16, at=o); o += 4096
        vaug = sb("vaug", [128, NT, N_HEADS, 65], BF16, at=o); o += 16640
        wtok = sb("wtok", [128, NT, N_HEADS], F32, at=o); o += 512
        wuqb = sb("wuqb", [128, 2, 1024], BF16, at=o); o += 4096
        wqib = sb("wqib", [128, 2, 512], BF16, at=o); o += 2048
        wuvb = sb("wuvb", [128, 512], BF16, at=o); o += 1024
        assert o <= PX4, o
        o = PX5
        sqc = [sb("sqc0", [128, 512], BF16, at=o)] * 2; o += 1024
        rstdc = [sb("rstdc0", [128, 512], F32, at=o)] * 2; o += 2048
        kdw = sb("kdw", [128, 8, 128], BF16, at=o); o += 2048
        assert o <= LIMIT, o
        b_cqn = [Buf() for _ in range(8)]
        b_kvn = Buf(); b_kidx = Buf(); b_vaug = Buf(); b_wtok = Buf()
        b_wuqb = Buf(); b_wqib = Buf(); b_wuvb = Buf()
        b_sqc = [Buf()] * 2; b_rstdc = [Buf()] * 2; b_kdw = Buf()

        o = PX4
        qblk = [sb("qblk%d" % i, [128, N_HEADS, 256], BF16, at=o + i * 4096) for i in range(2)]; o += 8192
        qiblk = [sb("qiblk0", [128, 4, 256], BF16, at=o)] * 2; o += 2048
        score = [sb("score%d" % i, [128, SEQ], F32, at=o + i * 8192) for i in range(2)]; o += 16384
        maskts = [sb("maskts%d" % i, [128, SEQ], BF16, at=o + i * 4096) for i in range(2)]; o += 8192
        maskT = sb("maskT", [128, NT, 256], BF16, at=o); o += 8192
        rt = [sb("rt%d" % i, [128, 512], BF16, at=o + i * 1024) for i in range(4)]; o += 4096
        et = [sb("et%d" % i, [128, 256], BF16, at=o + i * 512) for i in range(4)]; o += 2048
        pm = [sb("pm%d" % i, [128, 256], BF16, at=o + i * 512) for i in range(4)]; o += 2048
        et2 = [nc.alloc_sbuf_tensor_at("et2_%d" % i, [128, 512], BF16, offset=o - 4096 + i * 1024) for i in range(2)]
        pm2 = [nc.alloc_sbuf_tensor_at("pm2_%d" % i, [128, 512], BF16, offset=o - 2048 + i * 1024) for i in range(2)]
        dgw = [sb("dgw%d" % i, [128, N_HEADS, 128], BF16, at=o + i * 2048) for i in range(2)]; o += 4096
        ytok = sb("ytok", [128, 2, 512], F32, at=o); o += 4096
        bis = sb("bis", [128, 8], F32, at=o); o += 32
        steps = sb("steps", [128, NB + 2], F32, at=o); o += 96
        cands = sb("cands", [128, NB + 2], F32, at=o); o += 96
        cnts = sb("cnts", [128, NB + 2], F32, at=o); o += 96
        sels = sb("sels", [128, NB + 2], F32, at=o); o += 96
        rcp = sb("rcp", [128, 2, N_HEADS], F32, at=o); o += 64
        osb = sb("osb", [128, 2, N_HEADS, 65], F32, at=o); o += 4160
        assert o <= LIMIT, (o, LIMIT)
        b_osb = Buf()
        b_qblk = [Buf(), Buf()]; b_qiblk = [Buf()] * 2; b_score = [Buf(), Buf()]; b_maskts = [Buf(), Buf()]
        b_maskT = [Buf(), Buf()]
        b_rt = [Buf() for _ in range(4)]; b_et2 = [Buf(), Buf()]; b_pm2 = [Buf(), Buf()]
        b_dgw = [Buf(), Buf()]; b_ytok = [Buf(), Buf()]; b_bis = Buf(); b_steps = Buf()
        b_cands = [Buf() for _ in range(NB + 2)]; b_cnts = [Buf() for _ in range(NB + 2)]; b_sels = [Buf() for _ in range(NB + 2)]
        b_rcp = Buf()

        o = PX3
        woutb = sb("woutb", [128, 8, 1024], BF16, at=o); o += 16384
        gbc = sb("gbc", [128, D_MODEL], F32, at=o); o += 4096
        etmp = [sb("etmp%d" % i, [128, 512], F32, at=o + i * 2048) for i in range(2)]; o += 4096
        ejunk = sb("ejunk", [128, 512], F32, at=o); o += 2048
        ess = sb("ess", [128, 4], F32, at=o); o += 32
        dgE = [sb("dgE%d" % i, [128, 128], F32, at=o + i * 512) for i in range(2)]; o += 1024
        gvT = sb("gvT", [128, 8], F32, at=o); o += 32
        assert o <= PX4, o
        b_woutb = [Buf() for _ in range(4)]; b_gbc = Buf(); b_etmp = [Buf(), Buf()]; b_ejunk = Buf(); b_ess = Buf()
        b_dgE = [Buf(), Buf()]; b_gvT = Buf()

        b_region = Buf("region")

        def phase_barrier(extra=()):
            S.op("pool", lambda e: e.memset(dummy[:, 0:1], 0.0), writes=[b_region] + list(extra))

        RG = [b_region]

        wcnt = [0]
        wjob = [0, 0]

        def load_w(parts, dst_writes, casts):
            slot = wcnt[0] % 2
            wcnt[0] += 1
            stg = wst[slot]
            jn = wjob[1]
            wjob[1] += 1
            assert jn < NJOB
            S.op("sp", lambda e, stg=stg, jn=jn: e.dma_start(out=stg[:], in_=wpack_d[wjob[0], jn]), reads=RG, writes=[b_wst[slot]])
            for (oap, ifn, obufs) in casts:
                S.op("dve", lambda e, oap=oap, ifn=ifn, stg=stg: e.tensor_copy(out=oap, in_=ifn(stg)),
                     reads=[b_wst[slot]] + RG, writes=obufs)

        def win_cols(l, c0, c1):
            return None

        def stage3(stg, n):
            return stg[:, 0:8 * n].rearrange("p (k n) -> p k n", k=8)

        def load_x_quarter(b, q4):
            S.op("sp", lambda e: e.dma_start(out=xs_res[:, q4 * 4:(q4 + 1) * 4, :],
                                             in_=x_d[b, q4 * 512:(q4 + 1) * 512, :].rearrange("(n p) d -> p n d", p=128)),
                 writes=[b_x[q4 * 4 + i] for i in range(4)])

        def bcast_rows(vec_col_fn, dst, dst_buf, dgs, b_dgs, extra_reads):
            for half in range(2):
                bk = 4 + half
                for kk in range(4):
                    k = half * 4 + kk
                    sl = k % 2
                    S.op("dve", lambda e, k=k, sl=sl: e.tensor_scalar(out=dgs[sl][:], in0=ident_f[:], scalar1=vec_col_fn(k),
                                                                      scalar2=None, op0=ALU.mult),
                         reads=[b_identf] + extra_reads + RG, writes=[b_dgs[sl]])
                    S.op("pe", lambda e, kk=kk, sl=sl, bk=bk: e.matmul(psum[:, bk, kk * 128:(kk + 1) * 128], lhsT=ones_f[:],
                                                                      rhs=dgs[sl][:], start=True, stop=True),
                         reads=[b_onesf, b_dgs[sl]], writes=[pbuf[bk]])
                S.op("act", lambda e, half=half, bk=bk: e.activation(out=dst[:, half * 512:(half + 1) * 512], in_=psum[:, bk, :],
                                                                     func=AF.Copy), reads=[pbuf[bk]] + RG, writes=[dst_buf])

        def block(b, l, last_layer):
            wjob[0] = l
            wjob[1] = 0
            phase_barrier()
            S.op("dve", lambda e: e.scalar_tensor_tensor(out=vecT[:, 0, :], in0=modT[:, l, 8:16, b], scalar=1.0, in1=gpreT[:, l, :],
                                                         op0=ALU.add, op1=ALU.mult), reads=[b_modT, b_small] + RG, writes=[b_vecT])
            S.op("dve", lambda e: e.tensor_copy(out=vecT[:, 1, :], in_=modT[:, l, 0:8, b]), reads=[b_modT] + RG, writes=[b_vecT])
            bcast_rows(lambda k: vecT[:, 0, k:k + 1], abc, b_abc, dg, b_dg, [b_vecT])
            bcast_rows(lambda k: vecT[:, 1, k:k + 1], shbc, b_shbc, dg, b_dg, [b_vecT])
            for tt in range(NT):
                S.op("act", lambda e, tt=tt: e.activation(out=xs[tt % 2][:], in_=xs_res[:, tt, :], func=AF.Square,
                                                          accum_out=ssq[:, tt:tt + 1]),
                     reads=[b_x[tt]] + RG, writes=[b_xs[tt % 2], b_ssq])
            S.op("act", lambda e: e.activation(out=rs16[:], in_=ssq[:], func=AF.Sqrt, bias=EPS, scale=1.0 / D_MODEL),
                 reads=[b_ssq] + RG, writes=[b_rs16])
            S.op("dve", lambda e: e.reciprocal(out=rstd16[:], in_=rs16[:]), reads=[b_rs16] + RG, writes=[b_rstd16])
            for tt in range(NT):
                sl = tt % 2
                S.op("dve", lambda e, tt=tt, sl=sl: e.scalar_tensor_tensor(out=xs[sl][:], in0=xs_res[:, tt, :], scalar=rstd16[:, tt:tt + 1],
                                                                           in1=abc[:], op0=ALU.mult, op1=ALU.mult),
                     reads=[b_x[tt], b_rstd16, b_abc] + RG, writes=[b_xs[sl]])
                S.op("dve", lambda e, sl=sl: e.tensor_tensor(out=xs[sl][:], in0=xs[sl][:], in1=shbc[:], op=ALU.add),
                     reads=[b_xs[sl], b_shbc] + RG, writes=[b_xs[sl]])
                for half in range(2):
                    bk = (tt * 2 + half) % 4
                    for kk in range(4):
                        k = half * 4 + kk
                        S.op("pe", lambda e, sl=sl, k=k, kk=kk, bk=bk: e.transpose(out=psum[:, bk, kk * 128:(kk + 1) * 128],
                                                                                  in_=xs[sl][:, k * 128:(k + 1) * 128], identity=ident_f[:]),
                             reads=[b_xs[sl], b_identf], writes=[pbuf[bk]])
                    S.op("act", lambda e, tt=tt, half=half, bk=bk: e.activation(
                        out=hT[:, half * 4:(half + 1) * 4, tt * 128:(tt + 1) * 128],
                        in_=psum[:, bk, :].rearrange("p (k t) -> p k t", k=4), func=AF.Copy),
                        reads=[pbuf[bk]] + RG, writes=[b_hT[tt // 4]])

            phase_barrier()

            def inproj(wtile_fn, bk, tb, n=512, k_list=range(8)):
                for k in k_list:
                    S.op("pe", lambda e, k=k: e.matmul(psum[:, bk, 0:n], lhsT=wtile_fn(k), rhs=hT[:, k, tb * 512:tb * 512 + n],
                                                       start=(k == 0), stop=(k == 7)),
                         reads=[b_hT[tb]] + wt_reads[0], writes=[pbuf[bk]])

            wt_reads = [[]]
            load_w([(lambda stg: stg[:].rearrange("p (k n) -> p k n", k=4), None)], None,
                   [(wpw2b[:], lambda stg: stg[:].rearrange("p (k n) -> p k n", k=4), [b_wpw2b])])
            for c in range(4):
                slot = wcnt[0] % 2
                wb = wbf[slot]
                wb3 = wb[:].rearrange("p (k n) -> p k n", k=8)
                load_w([(lambda stg: stage3(stg, 256)[:, :, 0:128], win_cols(l, c * 128, (c + 1) * 128)),
                        (lambda stg: stage3(stg, 256)[:, :, 128:256], win_cols(l, 512 + c * 128, 512 + (c + 1) * 128))], None,
                       [(wb3, lambda stg: stage3(stg, 256), [b_wbf[slot]])])
                ap_ = apad[c % 2]
                bap = b_apad[c % 2]
                S.op("pool", lambda e, ap_=ap_: e.memset(ap_[:, 0:30], 0.0), reads=RG, writes=[bap])
                for tb in range(4):
                    wt_reads[0] = [b_wbf[slot]]
                    inproj(lambda k: wb3[:, k, 128:256], 0, tb)
                    inproj(lambda k: wb3[:, k, 0:128], 1, tb)
                    sl = tb % 2
                    S.op("act", lambda e, sl=sl: e.activation(out=sig[sl][:], in_=psum[:, 0, :], func=AF.Sigmoid),
                         reads=[pbuf[0]] + RG, writes=[b_sig[sl]])
                    S.op("dve", lambda e, sl=sl, tb=tb, ap_=ap_: e.tensor_tensor(out=ap_[:, 30 + tb * 512:30 + (tb + 1) * 512], in0=psum[:, 1, :],
                                                                                in1=sig[sl][:], op=ALU.mult),
                         reads=[pbuf[1], b_sig[sl]] + RG, writes=[bap])
                    for k in range(tb * 8, min(CONV_WIDTH, tb * 8 + 8)):
                        S.op("dve", lambda e, k=k, c=c: e.tensor_scalar(out=cdiag[:, k, :], in0=ident_b[:], scalar1=convw[:, l, c, k:k + 1],
                                                                        scalar2=None, op0=ALU.mult),
                             reads=[b_identb, b_small] + RG, writes=[b_cdiag])
                for tb in range(4):
                    bk = 2 + (tb % 2)
                    for k in range(CONV_WIDTH):
                        S.op("pe", lambda e, k=k, tb=tb, bk=bk, ap_=ap_: e.matmul(psum[:, bk, :], lhsT=cdiag[:, k, :],
                                                                                 rhs=ap_[:, k + tb * 512:k + tb * 512 + 512],
                                                                                 start=(k == 0), stop=(k == CONV_WIDTH - 1)),
                             reads=[b_cdiag, bap], writes=[pbuf[bk]])
                    S.op("act", lambda e, tb=tb, bk=bk, c=c: e.activation(out=yc[:, c, tb * 512:(tb + 1) * 512], in_=psum[:, bk, :],
                                                                          func=AF.Identity, bias=convb[:, l, c:c + 1], scale=1.0),
                         reads=[pbuf[bk], b_small] + RG, writes=[b_yc[c]])
            gslot = []
            for g in range(2):
                sl_ = wcnt[0] % 2
                gslot.append(sl_)
                load_w([(lambda stg: stage3(stg, 256), win_cols(l, 1024 + g * 256, 1024 + (g + 1) * 256))], None,
                       [(wbf[sl_][:].rearrange("p (k n) -> p k n", k=8), lambda stg: stage3(stg, 256), [b_wbf[sl_]])])
            alias_w = [b_apad[0], b_apad[1], b_sig[0], b_sig[1], b_cdiag]
            for tb in range(4):
                tsl = slice(tb * 512, (tb + 1) * 512)
                b4, b5 = 4 + 2 * (tb % 2), 5 + 2 * (tb % 2)
                for c in range(4):
                    S.op("pe", lambda e, c=c: e.matmul(psum[:, b4, :], lhsT=ones_b[:], rhs=yc[:, c, tsl], start=(c == 0), stop=(c == 3)),
                         reads=[b_onesb, b_yc[c]], writes=[pbuf[b4]])
                for c in range(4):
                    sl = c % 2
                    S.op("act", lambda e, c=c, sl=sl: e.activation(out=ycsq[sl][:], in_=yc[:, c, tsl], func=AF.Square),
                         reads=[b_yc[c]] + RG, writes=[b_ycsq[sl]] + (alias_w if sl == 1 else []))
                    S.op("pe", lambda e, c=c, sl=sl: e.matmul(psum[:, b5, :], lhsT=ones_b[:], rhs=ycsq[sl][:], start=(c == 0), stop=(c == 3)),
                         reads=[b_onesb, b_ycsq[sl]], writes=[pbuf[b5]])
                S.op("act", lambda e: e.activation(out=meanb4[tb][:], in_=psum[:, b4, :], func=AF.Copy, scale=1.0 / D_CONV),
                     reads=[pbuf[b4]] + RG, writes=[b_mean4[tb]] + alias_w)
                S.op("dve", lambda e: e.tensor_tensor(out=m2b[:], in0=meanb4[tb][:], in1=meanb4[tb][:], op=ALU.mult),
                     reads=[b_mean4[tb]] + RG, writes=[b_m2b])
                S.op("dve", lambda e: e.scalar_tensor_tensor(out=varb[:], in0=psum[:, b5, :], scalar=1.0 / D_CONV, in1=m2b[:],
                                                             op0=ALU.mult, op1=ALU.subtract),
                     reads=[pbuf[b5], b_m2b] + RG, writes=[b_varb])
                S.op("dve", lambda e: e.tensor_scalar(out=varb[:], in0=varb[:], scalar1=0.0, scalar2=None, op0=ALU.max),
                     reads=[b_varb] + RG, writes=[b_varb])
                S.op("act", lambda e: e.activation(out=rstdb4[tb][:], in_=varb[:], func=AF.Sqrt, bias=EPS, scale=1.0),
                     reads=[b_varb] + RG, writes=[b_rstd4[tb]] + alias_w)
                S.op("dve", lambda e: e.reciprocal(out=rstdb4[tb][:], in_=rstdb4[tb][:]), reads=[b_rstd4[tb]] + RG, writes=[b_rstd4[tb]])
            def ln_apply(tb, c):
                tsl = slice(tb * 512, (tb + 1) * 512)
                zt = ztb[tb % 2]; b_zt = b_ztb[tb % 2]
                sl = c % 2
                S.op("dve", lambda e: e.tensor_tensor(out=dtmp[sl][:], in0=yc[:, c, tsl], in1=meanb4[tb][:], op=ALU.subtract),
                     reads=[b_yc[c], b_mean4[tb]] + RG, writes=[b_dtmp[sl]])
                S.op("dve", lambda e: e.tensor_tensor(out=dtmp[sl][:], in0=dtmp[sl][:], in1=rstdb4[tb][:], op=ALU.mult),
                     reads=[b_dtmp[sl], b_rstd4[tb]] + RG, writes=[b_dtmp[sl]])
                S.op("act", lambda e: e.activation(out=zt[:, c, :], in_=dtmp[sl][:], func=AF.Silu,
                                                   bias=lnb[:, l, c:c + 1], scale=lng[:, l, c:c + 1]),
                     reads=[b_dtmp[sl], b_small] + RG, writes=[b_zt[c]])

            for c in range(4):
                ln_apply(0, c)
            for tb in range(4):
                tsl = slice(tb * 512, (tb + 1) * 512)
                zt = ztb[tb % 2]; b_zt = b_ztb[tb % 2]
                for c2 in range(4):
                    bk = 0 + (c2 % 2)
                    bg = 2 + (c2 % 2)
                    for c in range(4):
                        S.op("pe", lambda e, c=c, c2=c2, bk=bk: e.matmul(psum[:, bk, :], lhsT=wpw2b[:, c, c2 * 128:(c2 + 1) * 128],
                                                                        rhs=zt[:, c, :], start=(c == 0), stop=(c == 3)),
                             reads=[b_wpw2b, b_zt[c]], writes=[pbuf[bk]])
                    gs_ = gslot[c2 // 2]
                    wt_reads[0] = [b_wbf[gs_]]
                    wg3 = wbf[gs_][:].rearrange("p (k n) -> p k n", k=8)
                    inproj(lambda k: wg3[:, k, (c2 % 2) * 128:(c2 % 2 + 1) * 128], bg, tb)
                    if tb + 1 < 4:
                        ln_apply(tb + 1, c2)
                    sl = c2 % 2
                    S.op("act", lambda e, sl=sl, bg=bg: e.activation(out=sgc[sl][:], in_=psum[:, bg, :], func=AF.Silu),
                         reads=[pbuf[bg]] + RG, writes=[b_sgc[sl]])
                    S.op("dve", lambda e, sl=sl, c2=c2, bk=bk: e.tensor_tensor(out=yconvT[:, c2, tsl], in0=psum[:, bk, :], in1=sgc[sl][:],
                                                                              op=ALU.mult),
                         reads=[pbuf[bk], b_sgc[sl]] + RG, writes=[b_yconv[tb]])

            phase_barrier()
            attn_scale = (8 ** -0.5) * (D_IDX ** -0.5)
            S.op("pool", lambda e: e.memset(vaug[:, :, :, 64:65], 1.0), reads=RG, writes=[b_vaug])
            load_w([(lambda stg: stg[:].rearrange("p (k n) -> p k n", k=2), None)], None,
                   [(wuqb[:], lambda stg: stg[:].rearrange("p (k n) -> p k n", k=2), [b_wuqb])])
            load_w([(lambda stg: stg[:, 0:1024].rearrange("p (k n) -> p k n", k=2), None),
                    (lambda stg: stg[:, 1024:1536], None)], None,
                   [(wqib[:], lambda stg: stg[:, 0:1024].rearrange("p (k n) -> p k n", k=2), [b_wqib]),
                    (wuvb[:], lambda stg: stg[:, 1024:1536], [b_wuvb])])
            s1 = wcnt[0] % 2
            w1 = wbf[s1][:].rearrange("p (k n) -> p k n", k=8)
            load_w([(lambda stg: stage3(stg, 256), win_cols(l, 1536, 1792))], None, [(w1, lambda stg: stage3(stg, 256), [b_wbf[s1]])])
            s2 = wcnt[0] % 2
            w2 = wbf[s2][:].rearrange("p (k n) -> p k n", k=8)
            load_w([(lambda stg: stage3(stg, 256)[:, :, 0:200], win_cols(l, 1792, 1992))], None,
                   [(w2[:, :, 0:200], lambda stg: stage3(stg, 256)[:, :, 0:200], [b_wbf[s2]]),
                    (kdw[:, :, 0:64], lambda stg: stage3(stg, 256)[:, :, 128:192], [b_kdw]),
                    (kdw[:, :, 64:128], lambda stg: stage3(stg, 256)[:, :, 128:192], [b_kdw])])
            for tb in range(4):
                tsl = slice(tb * 512, (tb + 1) * 512)
                wt_reads[0] = [b_wbf[s1]]
                inproj(lambda k: w1[:, k, 0:128], 0, tb)
                inproj(lambda k: w1[:, k, 128:256], 1, tb)
                for ch in range(2):
                    S.op("act", lambda e, ch=ch: e.activation(out=sqc[ch][:], in_=psum[:, ch, :], func=AF.Square),
                         reads=[pbuf[ch]] + RG, writes=[b_sqc[ch]])
                    S.op("pe", lambda e, ch=ch: e.matmul(psum[:, 2, :], lhsT=ones_b[:], rhs=sqc[ch][:], start=(ch == 0), stop=(ch == 1)),
                         reads=[b_onesb, b_sqc[ch]], writes=[pbuf[2]])
                S.op("act", lambda e: e.activation(out=rstdc[0][:], in_=psum[:, 2, :], func=AF.Sqrt, bias=EPS, scale=1.0 / Q_LORA),
                     reads=[pbuf[2]] + RG, writes=[b_rstdc[0]])
                S.op("dve", lambda e: e.reciprocal(out=rstdc[0][:], in_=rstdc[0][:]), reads=[b_rstdc[0]] + RG, writes=[b_rstdc[0]])
                for ch in range(2):
                    S.op("dve", lambda e, ch=ch: e.scalar_tensor_tensor(out=cqnT[:, ch, tsl], in0=psum[:, ch, :], scalar=qng[:, l, ch:ch + 1],
                                                                        in1=rstdc[0][:], op0=ALU.mult, op1=ALU.mult),
                         reads=[pbuf[ch], b_small, b_rstdc[0]] + RG, writes=[b_cqn[2 * tb], b_cqn[2 * tb + 1]])
                wt_reads[0] = [b_wbf[s2]]
                inproj(lambda k: w2[:, k, 0:128], 3, tb)
                S.op("act", lambda e: e.activation(out=sqc[0][:], in_=psum[:, 3, :], func=AF.Square), reads=[pbuf[3]] + RG, writes=[b_sqc[0]])
                S.op("pe", lambda e: e.matmul(psum[:, 4, :], lhsT=ones_b[:], rhs=sqc[0][:], start=True, stop=True),
                     reads=[b_onesb, b_sqc[0]], writes=[pbuf[4]])
                S.op("act", lambda e: e.activation(out=rstdc[1][:], in_=psum[:, 4, :], func=AF.Sqrt, bias=EPS, scale=1.0 / KV_LORA),
                     reads=[pbuf[4]] + RG, writes=[b_rstdc[1]])
                S.op("dve", lambda e: e.reciprocal(out=rstdc[1][:], in_=rstdc[1][:]), reads=[b_rstdc[1]] + RG, writes=[b_rstdc[1]])
                S.op("dve", lambda e: e.scalar_tensor_tensor(out=kvnT[:, tsl], in0=psum[:, 3, :], scalar=kvng[:, l, 0:1], in1=rstdc[1][:],
                                                             op0=ALU.mult, op1=ALU.mult),
                     reads=[pbuf[3], b_small, b_rstdc[1]] + RG, writes=[b_kvn])
                wt_reads[0] = [b_kdw]
                inproj(lambda k: kdw[:, k, :], 5, tb)
                S.op("act", lambda e: e.activation(out=kidx2[:, tsl], in_=psum[:, 5, :], func=AF.Copy), reads=[pbuf[5]] + RG, writes=[b_kidx])
                for t4 in range(4):
                    tt = tb * 4 + t4
                    for k in range(8):
                        S.op("pe", lambda e, k=k, tt=tt, t4=t4: e.matmul(psum[:, 6, t4 * 8:(t4 + 1) * 8], lhsT=hT[:, k, tt * 128:(tt + 1) * 128],
                                                                        rhs=w2[:, k, 192:200], start=(k == 0), stop=(k == 7)),
                             reads=[b_hT[tb], b_wbf[s2]], writes=[pbuf[6]])
                S.op("act", lambda e, tb=tb: e.activation(out=wtok[:, tb * 4:(tb + 1) * 4, :],
                                                          in_=psum[:, 6, 0:32].rearrange("p (t h) -> p t h", h=8), func=AF.Copy, scale=attn_scale),
                     reads=[pbuf[6]] + RG, writes=[b_wtok])
            for g in range(2):
                sg_ = wcnt[0] % 2
                wg = wbf[sg_][:].rearrange("p (k n) -> p k n", k=8)
                load_w([(lambda stg: stage3(stg, 256), win_cols(l, 1992 + g * 256, 1992 + (g + 1) * 256))], None,
                       [(wg, lambda stg: stage3(stg, 256), [b_wbf[sg_]])])
                for cc in range(2):
                    ch = g * 2 + cc
                    for tb in range(4):
                        bk = (cc * 4 + tb) % 2
                        wt_reads[0] = [b_wbf[sg_]]
                        inproj(lambda k: wg[:, k, cc * 128:(cc + 1) * 128], bk, tb)
                        S.op("act", lambda e, ch=ch, tb=tb, bk=bk: e.activation(out=sgT[:, ch, tb * 512:(tb + 1) * 512], in_=psum[:, bk, :],
                                                                               func=AF.Silu),
                             reads=[pbuf[bk]] + RG, writes=[b_sg[2 * tb], b_sg[2 * tb + 1]])
            for stl in range(NT):
                bk = 2 + (stl % 2)
                S.op("pe", lambda e, stl=stl, bk=bk: e.matmul(psum[:, bk, :], lhsT=kvnT[:, stl * 128:(stl + 1) * 128], rhs=wuvb[:],
                                                             start=True, stop=True), reads=[b_kvn, b_wuvb], writes=[pbuf[bk]])
                S.op("act", lambda e, stl=stl, bk=bk: e.activation(out=vaug[:, stl, :, 0:64],
                                                                   in_=psum[:, bk, :].rearrange("p (h d) -> p h d", h=N_HEADS), func=AF.Copy),
                     reads=[pbuf[bk]] + RG, writes=[b_vaug])

            phase_barrier()
            sm_scale = KV_LORA ** -0.5
            cnt_r = [0]; cnt_e = [0]
            S.op("pool", lambda e: e.memset(maskT[:], 0.0), reads=RG, writes=[b_maskT[0], b_maskT[1]])
            DSK = 2
            psb = psum[:, 7, :].bitcast(BF16).rearrange("p (j t) -> p j t", t=128)

            def gen_Qq(B):
                qs = B % 2
                tcols = slice(B * 256, (B + 1) * 256)
                for hp in range(4):
                    for hh in range(2):
                        h = hp * 2 + hh
                        for ch in range(2):
                            S.op("pe", lambda e, h=h, hh=hh, ch=ch: e.matmul(psum[:, 7, hh * 256:(hh + 1) * 256],
                                                                             lhsT=wuqb[:, ch, h * 128:(h + 1) * 128], rhs=cqnT[:, ch, tcols],
                                                                             start=(ch == 0), stop=(ch == 1)),
                                 reads=[b_wuqb, b_cqn[B]], writes=[pbuf[7]])
                    S.op("act", lambda e, hp=hp: e.activation(out=qblk[qs][:, hp * 2:hp * 2 + 2, :],
                                                              in_=psum[:, 7, :].rearrange("p (h t) -> p h t", h=2), func=AF.Copy),
                         reads=[pbuf[7]] + RG, writes=[b_qblk[qs]])
                    yield 1.0

            def gen_Qi(B):
                qs = B % 2
                tcols = slice(B * 256, (B + 1) * 256)
                for pp in range(2):
                    for pq in range(2):
                        pr = pp * 2 + pq
                        for ch in range(2):
                            S.op("pe", lambda e, pr=pr, pq=pq, ch=ch: e.matmul(psum[:, 7, pq * 256:(pq + 1) * 256],
                                                                               lhsT=wqib[:, ch, pr * 128:(pr + 1) * 128], rhs=cqnT[:, ch, tcols],
                                                                               start=(ch == 0), stop=(ch == 1)),
                                 reads=[b_wqib, b_cqn[B]], writes=[pbuf[7]])
                    S.op("act", lambda e, pp=pp: e.activation(out=qiblk[qs][:, pp * 2:pp * 2 + 2, :],
                                                              in_=psum[:, 7, :].rearrange("p (h t) -> p h t", h=2), func=AF.Copy),
                         reads=[pbuf[7]] + RG, writes=[b_qiblk[qs]])
                    yield 1.0

            def gen_D(i):
                dw = dgw[i % 2]; bdw = b_dgw[i % 2]
                for h in range(N_HEADS):
                    S.op("dve", lambda e, h=h: e.tensor_scalar(out=dw[:, h, :], in0=ident_b[:], scalar1=wtok[:, i, h:h + 1],
                                                               scalar2=None, op0=ALU.mult),
                         reads=[b_identb, b_wtok] + RG, writes=[bdw])
                yield 1.0

            def gen_X(i):
                B, tl = i // 2, i % 2
                qs = B % 2
                Lk = 128 * (i + 1)
                sc = score[i % 2]; bsc = b_score[i % 2]
                dw = dgw[i % 2]; bdw = b_dgw[i % 2]
                nsb = (Lk + 511) // 512

                def emit_dm(item):
                    sbk, h, rs_, c0, w = item
                    S.op("pe", lambda e: e.matmul(psum[:, 6, 0:w], lhsT=dw[:, h, :], rhs=rt[rs_][:, 0:w],
                                                  start=(h == 0), stop=(h == N_HEADS - 1)),
                         reads=[bdw, b_rt[rs_]], writes=[pbuf[6]])
                    if h == N_HEADS - 1:
                        last = (sbk == nsb - 1)
                        wc = w - 128 if last else w
                        if wc > 0:
                            S.op("act", lambda e: e.activation(out=sc[:, c0:c0 + wc], in_=psum[:, 6, 0:wc], func=AF.Copy),
                                 reads=[pbuf[6]] + RG, writes=[bsc])
                        if last:
                            S.op("dve", lambda e: e.tensor_tensor(out=sc[:, c0 + wc:c0 + wc + 128], in0=psum[:, 6, wc:wc + 128],
                                                                  in1=causal[:], op=ALU.add),
                                 reads=[pbuf[6], b_causal] + RG, writes=[bsc])

                pend = []
                for sbk in range(nsb):
                    c0 = sbk * 512
                    w = min(Lk, c0 + 512) - c0
                    for pr in range(4):
                        items = []
                        for hf in range(2):
                            h = pr * 2 + hf
                            rb = 4 + hf
                            rs_ = cnt_r[0] % 4
                            cnt_r[0] += 1
                            S.op("pe", lambda e, hf=hf, rb=rb: e.matmul(
                                psum[:, rb, 0:w], lhsT=qiblk[qs][hf * 64:(hf + 1) * 64, pr, tl * 128:(tl + 1) * 128],
                                rhs=kidx2[hf * 64:(hf + 1) * 64, c0:c0 + w], start=True, stop=True),
                                reads=[b_qiblk[qs], b_kidx], writes=[pbuf[rb]])
                            items.append((sbk, h, rs_, c0, w, rb))
                        for (sbk_, h, rs_, c0_, w_, rb) in items:
                            S.op("act", lambda e, rb=rb, rs_=rs_: e.activation(out=rt[rs_][:, 0:w], in_=psum[:, rb, 0:w], func=AF.Relu),
                                 reads=[pbuf[rb]] + RG, writes=[b_rt[rs_]])
                        for it in pend:
                            emit_dm(it)
                        pend = [it[:5] for it in items]
                        yield 2.0
                for it in pend:
                    emit_dm(it)
                yield 1.0

            dve_counting = [False]

            def gen_Y(i):
                Lk = 128 * (i + 1)
                sc = score[i % 2]; bsc = b_score[i % 2]
                mk = maskts[i % 2]; bmk = b_maskts[i % 2]
                if i < 2:
                    thr_ap = negbig[:, 0:1]
                    thr_reads = [b_negbig]
                else:
                    dve_counting[0] = True
                    S.op("dve", lambda e: e.tensor_reduce(out=bis[:, 0:1], in_=sc[:, 0:Lk], axis=AX.X, op=ALU.max),
                         reads=[bsc] + RG, writes=[b_bis])
                    yield 2.0
                    S.op("dve", lambda e: e.tensor_reduce(out=bis[:, 1:2], in_=sc[:, 0:128 * i], axis=AX.X, op=ALU.min),
                         reads=[bsc] + RG, writes=[b_bis])
                    yield 2.0
                    S.op("dve", lambda e: e.tensor_tensor(out=bis[:, 2:3], in0=bis[:, 0:1], in1=bis[:, 1:2], op=ALU.subtract),
                         reads=[b_bis] + RG, writes=[b_bis])
                    S.op("dve", lambda e: e.tensor_scalar(out=steps[:], in0=pow2[:], scalar1=bis[:, 2:3], scalar2=None, op0=ALU.mult),
                         reads=[b_pow2, b_bis] + RG, writes=[b_steps])
                    S.op("dve", lambda e: e.tensor_tensor(out=cands[:, 0:1], in0=bis[:, 1:2], in1=steps[:, 0:1], op=ALU.add),
                         reads=[b_bis, b_steps] + RG, writes=[b_cands[0]])
                    yield 1.0
                    for j in range(NB):
                        S.op("dve", lambda e, j=j: e.tensor_scalar(out=mk[:, 0:Lk], in0=sc[:, 0:Lk], scalar1=cands[:, j:j + 1],
                                                                   scalar2=None, op0=ALU.is_ge, op1=ALU.add,
                                                                   accum_out=cnts[:, j:j + 1]),
                             reads=[bsc, b_cands[j]] + RG, writes=[bmk, b_cnts[j]])
                        S.op("dve", lambda e, j=j: e.tensor_scalar(out=sels[:, j:j + 1], in0=cnts[:, j:j + 1], scalar1=TOPK - 0.5,
                                                                   scalar2=steps[:, j:j + 1], op0=ALU.is_ge, op1=ALU.mult),
                             reads=[b_cnts[j], b_steps] + RG, writes=[b_sels[j]])
                        S.op("dve", lambda e, j=j: e.scalar_tensor_tensor(out=cands[:, j + 1:j + 2], in0=sels[:, j:j + 1],
                                                                          scalar=cands[:, j:j + 1], in1=steps[:, j + 1:j + 2],
                                                                          op0=ALU.add, op1=ALU.subtract),
                             reads=[b_sels[j], b_cands[j], b_steps] + RG, writes=[b_cands[j + 1]])
                        yield 2.5
                    thr_ap = cands[:, NB:NB + 1]
                    thr_reads = [b_cands[NB]]
                S.op("dve", lambda e: e.tensor_scalar(out=mk[:, 0:Lk], in0=sc[:, 0:Lk], scalar1=thr_ap, scalar2=None, op0=ALU.is_ge),
                     reads=[bsc] + thr_reads + RG, writes=[bmk])
                dve_counting[0] = False
                yield 1.0

            def gen_Z(i):
                tl = i % 2
                mk = maskts[i % 2]; bmk = b_maskts[i % 2]
                for j0 in range(0, i + 1, 8):
                    n = min(8, i + 1 - j0)
                    for jj in range(n):
                        j = j0 + jj
                        S.op("pe", lambda e, j=j, jj=jj: e.transpose(out=psb[:, jj, :], in_=mk[:, j * 128:(j + 1) * 128], identity=ident_b[:]),
                             reads=[bmk, b_identb], writes=[pbuf[7]])
                    S.op("act", lambda e, j0=j0, n=n: e.activation(out=maskT[:, j0:j0 + n, tl * 128:(tl + 1) * 128], in_=psb[:, 0:n, :],
                                                                   func=AF.Copy),
                         reads=[pbuf[7]] + RG, writes=[b_maskT[tl]])
                    yield 1.0

            def gen_W(B):
                i0, i1 = B * 2, B * 2 + 1
                qs = B % 2
                tcols = slice(B * 256, (B + 1) * 256)
                ob = [0, 1]

                def emit_pv(item):
                    h, jp, es = item
                    for half in range(2):
                        j = jp + half
                        for tl in range(2):
                            if j > (i0 if tl == 0 else i1):
                                continue
                            lastj = i0 if tl == 0 else i1
                            S.op("pe", lambda e, tl=tl, lastj=lastj, j=j, half=half: e.matmul(
                                psum[:, ob[tl], 0:65], lhsT=pm2[es][:, half * 256 + tl * 128:half * 256 + (tl + 1) * 128], rhs=vaug[:, j, h, :],
                                start=(j == 0), stop=(j == lastj)),
                                reads=[b_pm2[es], b_vaug], writes=[pbuf[ob[tl]]])
                    if jp + 1 == i1:
                        for tl in range(2):
                            S.op("act", lambda e, tl=tl: e.activation(out=osb[:, tl, h, :], in_=psum[:, ob[tl], 0:65], func=AF.Copy),
                                 reads=[pbuf[ob[tl]]] + RG, writes=[b_osb])

                pend = []
                for h in range(N_HEADS):
                    for jp in range(0, i1 + 1, 2):
                        qb = 2 + (cnt_e[0] % 2)
                        es = cnt_e[0] % 2
                        cnt_e[0] += 1
                        for half in range(2):
                            j = jp + half
                            S.op("pe", lambda e, j=j, half=half: e.matmul(psum[:, qb, half * 256:(half + 1) * 256],
                                                                          lhsT=kvnT[:, j * 128:(j + 1) * 128],
                                                                          rhs=qblk[qs][:, h, :], start=True, stop=True),
                                 reads=[b_kvn, b_qblk[qs]], writes=[pbuf[qb]])
                        S.op("act", lambda e: e.activation(out=et2[es][:], in_=psum[:, qb, :], func=AF.Exp,
                                                           bias=b31bc[:, h:h + 1], scale=sm_scale),
                             reads=[pbuf[qb], b_b31] + RG, writes=[b_et2[es]])
                        meng = "pool" if dve_counting[0] else "dve"
                        for half in range(2):
                            j = jp + half
                            if j >= i0 - 1:
                                if j == i0 - 1:
                                    tc0, tc1, u0 = 0, 128, 128
                                elif j == i0:
                                    tc0, tc1, u0 = 0, 256, 0
                                else:
                                    tc0, tc1, u0 = 128, 256, 0
                                hb = half * 256
                                S.op(meng, lambda e, tc0=tc0, tc1=tc1, u0=u0, hb=hb: e.tensor_tensor(
                                    out=et2[es][:, hb + tc0:hb + tc1], in0=et2[es][:, hb + tc0:hb + tc1],
                                    in1=tbc[:, h, u0:u0 + (tc1 - tc0)], op=ALU.mult),
                                    reads=[b_et2[es], b_tbc] + RG, writes=[b_et2[es]])
                        S.op(meng, lambda e: e.tensor_tensor(out=pm2[es][:].rearrange("p (a t) -> p a t", a=2),
                                                             in0=et2[es][:].rearrange("p (a t) -> p a t", a=2),
                                                             in1=maskT[:, jp:jp + 2, :], op=ALU.mult),
                             reads=[b_et2[es], b_maskT[0], b_maskT[1]] + RG, writes=[b_pm2[es]])
                        pend.append((h, jp, es))
                        if len(pend) > 1:
                            emit_pv(pend.pop(0))
                        yield 2.0
                while pend:
                    emit_pv(pend.pop(0))
                yield 1.0
                S.op("dve", lambda e: e.reciprocal(out=rcp[:], in_=osb[:, :, :, 64]), reads=[b_osb] + RG, writes=[b_rcp])
                S.op("dve", lambda e: e.tensor_tensor(out=ytok[:].rearrange("p t (h d) -> p t h d", h=N_HEADS), in0=osb[:, :, :, 0:64],
                                                      in1=rcp[:].unsqueeze(3).to_broadcast([128, 2, N_HEADS, 64]), op=ALU.mult),
                     reads=[b_osb, b_rcp] + RG, writes=[b_ytok[0], b_ytok[1]])
                for ch in range(4):
                    hb = (ch % 2) * 256
                    for tl in range(2):
                        S.op("pe", lambda e, ch=ch, tl=tl, hb=hb: e.transpose(out=psum[:, 7, hb + tl * 128:hb + (tl + 1) * 128],
                                                                             in_=ytok[:, tl, ch * 128:(ch + 1) * 128], identity=ident_f[:]),
                             reads=[b_ytok[tl], b_identf], writes=[pbuf[7]])
                    S.op("dve", lambda e, ch=ch, hb=hb: e.tensor_tensor(out=sgT[:, ch, tcols], in0=psum[:, 7, hb:hb + 256], in1=sgT[:, ch, tcols],
                                                                        op=ALU.mult),
                         reads=[pbuf[7], b_sg[B]] + RG, writes=[b_sg[B]])
                yield 1.0

            def run(g):
                for _ in g:
                    pass

            def chain(*gens):
                for g in gens:
                    for wgt in g:
                        yield wgt

            def par(ga, gb):
                ta = tb_ = 0.0
                a_done = b_done = False
                while not (a_done and b_done):
                    if b_done or (not a_done and ta <= tb_):
                        try:
                            wgt = next(ga); ta += wgt
                            yield wgt * 0.5
                        except StopIteration:
                            a_done = True
                            ta = float("inf")
                    else:
                        try:
                            wgt = next(gb); tb_ += wgt
                            yield wgt * 0.5
                        except StopIteration:
                            b_done = True
                            tb_ = float("inf")

            def interleave(ga, na, gb, nb_):
                a_done = b_done = False
                pa = pb_ = 0.0
                while not (a_done and b_done):
                    if b_done or (not a_done and pa * nb_ <= pb_ * na):
                        try:
                            pa += next(ga)
                        except StopIteration:
                            a_done = True
                    else:
                        try:
                            pb_ += next(gb)
                        except StopIteration:
                            b_done = True

            def n_x(i):
                return 1.0 + 8.0 * ((128 * (i + 1) + 511) // 512)

            def n_y(i):
                return 1.0 if i < 2 else 2.5 * NB + 5.0

            NBLK = NT // 2

            def nothing():
                return
                yield 0.0

            run(gen_Qq(0)); run(gen_Qi(0)); run(gen_D(0)); run(gen_D(1)); run(gen_X(0)); run(gen_Y(0)); run(gen_X(1)); run(gen_Y(1))
            run(gen_Z(0)); run(gen_Z(1))
            run(gen_Qi(1)); run(gen_D(2)); run(gen_D(3)); run(gen_X(2))
            for B in range(NBLK):
                if B + 1 < NBLK:
                    i2, i3 = 2 * B + 2, 2 * B + 3
                    if B + 2 < NBLK:
                        nxt = chain(gen_Qi(B + 2), gen_D(i2 + 2), gen_D(i3 + 2), gen_X(i2 + 2))
                        n_nxt = 4.0 + n_x(i2 + 2)
                    else:
                        nxt = nothing()
                        n_nxt = 0.0
                    side = chain(gen_Qq(B + 1), par(gen_Y(i2), gen_X(i3)), par(gen_Y(i3), nxt))
                    n_side = 4.0 + 0.5 * (n_y(i2) + n_x(i3)) + 0.5 * (n_y(i3) + n_nxt)
                    interleave(gen_W(B), 8.0 * (2 * B + 2) + 2.0, side, n_side)
                    run(gen_Z(i2)); run(gen_Z(i3))
                else:
                    run(gen_W(B))

            phase_barrier()
            S.op("dve", lambda e: e.tensor_tensor(out=gvT[:], in0=modT[:, l, 16:24, b], in1=gpostT[:, l, :], op=ALU.mult),
                 reads=[b_modT, b_small] + RG, writes=[b_gvT])
            bcast_rows(lambda k: gvT[:, k:k + 1], gbc, b_gbc, dgE, b_dgE, [b_gvT])
            for g in range(4):
                load_w([(lambda stg: stage3(stg, 256), None)], None,
                       [(woutb[:, :, g * 256:(g + 1) * 256], lambda stg: stage3(stg, 256), [b_woutb[g]])])
            for tt in range(NT):
                tsl = slice(tt * 128, (tt + 1) * 128)
                for nh in range(2):
                    bk = (tt * 2 + nh) % 4
                    for k in range(8):
                        lhs = yconvT[:, k, tsl] if k < 4 else sgT[:, k - 4, tsl]
                        rd = [b_yconv[tt // 4]] if k < 4 else [b_sg[tt // 2]]
                        S.op("pe", lambda e, lhs=lhs, k=k, nh=nh, bk=bk: e.matmul(psum[:, bk, :], lhsT=lhs, rhs=woutb[:, k, nh * 512:(nh + 1) * 512],
                                                                                 start=(k == 0), stop=(k == 7)),
                             reads=rd + [b_woutb[2 * nh], b_woutb[2 * nh + 1]], writes=[pbuf[bk]])
                    S.op("act", lambda e, nh=nh, bk=bk: e.activation(out=ejunk[:], in_=psum[:, bk, :], func=AF.Square, accum_out=ess[:, nh:nh + 1]),
                         reads=[pbuf[bk]] + RG, writes=[b_ejunk, b_ess])
                S.op("dve", lambda e: e.tensor_tensor(out=ess[:, 2:3], in0=ess[:, 0:1], in1=ess[:, 1:2], op=ALU.add),
                     reads=[b_ess] + RG, writes=[b_ess])
                S.op("act", lambda e: e.activation(out=ess[:, 2:3], in_=ess[:, 2:3], func=AF.Sqrt, bias=EPS, scale=1.0 / D_MODEL),
                     reads=[b_ess] + RG, writes=[b_ess])
                S.op("dve", lambda e: e.reciprocal(out=ess[:, 3:4], in_=ess[:, 2:3]), reads=[b_ess] + RG, writes=[b_ess])
                for nh in range(2):
                    bk = (tt * 2 + nh) % 4
                    S.op("dve", lambda e, nh=nh, bk=bk: e.scalar_tensor_tensor(out=etmp[nh][:], in0=psum[:, bk, :], scalar=ess[:, 3:4],
                                                                               in1=gbc[:, nh * 512:(nh + 1) * 512], op0=ALU.mult, op1=ALU.mult),
                         reads=[pbuf[bk], b_ess, b_gbc] + RG, writes=[b_etmp[nh]])
                    S.op("dve", lambda e, nh=nh, tt=tt: e.tensor_tensor(out=xs_res[:, tt, nh * 512:(nh + 1) * 512],
                                                                         in0=xs_res[:, tt, nh * 512:(nh + 1) * 512], in1=etmp[nh][:], op=ALU.add),
                         reads=[b_etmp[nh], b_x[tt]] + RG, writes=[b_x[tt]])
                if last_layer and (tt % 4 == 3):
                    t0 = tt - 3
                    S.op("sp", lambda e, t0=t0: e.dma_start(out=out_d[b, t0 * 128:(t0 + 4) * 128, :].rearrange("(n p) d -> p n d", p=128),
                                                            in_=xs_res[:, t0:t0 + 4, :]),
                         reads=[b_x[t0], b_x[t0 + 1], b_x[t0 + 2], b_x[t0 + 3]])
                    if b + 1 < nseq:
                        load_x_quarter(b + 1, t0 // 4)

        phase_barrier([b_pro, b_gc, b_tbrev, b_scr, b_wada[0], b_wada[1], b_wadab[0], b_wadab[1], b_cactb, b_tbc, b_modT])
        for q4 in range(4):
            load_x_quarter(0, q4)
        for b in range(nseq):
            for li, l in enumerate(layers):
                block(b, l, li == len(layers) - 1)
        S.finish()
        S.emit()
        build_program.nins = S.nins
    return nc


def _fm(v, nchunk):
    L = v.shape[0]
    return np.ascontiguousarray(v.reshape(L, nchunk, 128).transpose(2, 0, 1)).astype(np.float32)


def pack_weights(inputs):
    f = lambda a: np.asarray(a, dtype=np.float32)
    w_in, w_pw2, w_uq, w_qidx, w_uv, w_out = (f(inputs[k]) for k in ("w_in", "w_pw2", "w_uq", "w_qidx", "w_uv", "w_out"))
    packs = np.zeros((DEPTH, 17, 128, 2048), np.float32)
    for l in range(DEPTH):
        win = w_in[l].reshape(8, 128, D_IN_PROJ).transpose(1, 0, 2)
        jobs = []
        jobs.append(w_pw2[l].reshape(4, 128, 512).transpose(1, 0, 2).reshape(128, 2048))
        for c in range(4):
            t = np.zeros((128, 8, 256), np.float32)
            t[:, :, 0:128] = win[:, :, c * 128:(c + 1) * 128]
            t[:, :, 128:256] = win[:, :, 512 + c * 128:512 + (c + 1) * 128]
            jobs.append(t.reshape(128, 2048))
        for g in range(2):
            jobs.append(win[:, :, 1024 + g * 256:1024 + (g + 1) * 256].reshape(128, 2048))
        jobs.append(w_uq[l].reshape(2, 128, 1024).transpose(1, 0, 2).reshape(128, 2048))
        t = np.zeros((128, 2048), np.float32)
        t[:, 0:1024] = w_qidx[l].reshape(2, 128, 512).transpose(1, 0, 2).reshape(128, 1024)
        t[:, 1024:1536] = w_uv[l].transpose(1, 0, 2).reshape(KV_LORA, N_HEADS * 64)
        jobs.append(t)
        jobs.append(win[:, :, 1536:1792].reshape(128, 2048))
        t = np.zeros((128, 8, 256), np.float32)
        t[:, :, 0:200] = win[:, :, 1792:1992]
        jobs.append(t.reshape(128, 2048))
        for g in range(2):
            jobs.append(win[:, :, 1992 + g * 256:1992 + (g + 1) * 256].reshape(128, 2048))
        wo = w_out[l].reshape(8, 128, D_MODEL).transpose(1, 0, 2)
        for g in range(4):
            jobs.append(wo[:, :, g * 256:(g + 1) * 256].reshape(128, 2048))
        assert len(jobs) == 17
        for j, t in enumerate(jobs):
            packs[l, j] = t
    return packs


def make_in_maps(inputs, ncores=NCORES, nseq=SEQ_PER_CORE):
    f = lambda a: np.ascontiguousarray(np.asarray(a, dtype=np.float32))
    x = f(inputs["x"]); c = f(inputs["c"])
    n = np.arange(NG) - 127
    oh = np.zeros((N_BUCKETS, NG), np.float32)
    oh[t5_bucket_np(n), np.arange(NG)] = 1.0
    conv_w = f(inputs["conv_w"])
    conv_wT = np.ascontiguousarray(conv_w.reshape(DEPTH, CONV_WIDTH, 4, 128).transpose(3, 0, 2, 1))
    rel_bias = f(inputs["rel_bias"])
    w_ada = f(inputs["w_ada"])
    w_adaP = np.ascontiguousarray(w_ada.reshape(DEPTH, 8, 128, 8, 384).transpose(0, 3, 2, 1, 4))
    shared = {
        "w_adaP": w_adaP, "b_adaT": _fm(f(inputs["b_ada"]), 24), "g_preT": _fm(f(inputs["g_pre"]), 8),
        "g_postT": _fm(f(inputs["g_post"]), 8), "wpack": pack_weights(inputs), "conv_wT": conv_wT,
        "conv_bT": _fm(f(inputs["conv_b"]), 4), "ln_gT": _fm(f(inputs["conv_ln_g"]), 4), "ln_bT": _fm(f(inputs["conv_ln_b"]), 4),
        "q_norm_gT": _fm(f(inputs["q_norm_g"]), 2), "kv_norm_gT": _fm(f(inputs["kv_norm_g"]), 1),
        "rel_bias": rel_bias, "rel_biasT": np.ascontiguousarray(rel_bias.T), "oh": oh,
    }
    maps = []
    for i in range(ncores):
        xb = x[i * nseq:(i + 1) * nseq]
        cb = c[i * nseq:(i + 1) * nseq]
        cTb = np.ascontiguousarray(cb.reshape(nseq, 8, 128).transpose(2, 1, 0))
        m = dict(shared)
        m["x"] = np.ascontiguousarray(xb)
        m["cT"] = cTb
        maps.append(m)
    return maps


def kernel(**inputs):
    nc = build_program(layers=(0, 1), nseq=SEQ_PER_CORE)
    in_maps = make_in_maps(inputs)
    res = run_bass_kernel_spmd(nc, in_maps, core_ids=list(range(NCORES)))
    outs = [np.asarray(r["out"], dtype=np.float32) for r in res.results]
    return np.concatenate(outs, axis=0)
```

```python
import math
from contextlib import ExitStack

import numpy as np
import concourse.bass as bass
import concourse.mybir as mybir
from concourse.bass_utils import run_bass_kernel_spmd

F32 = mybir.dt.float32
BF16 = mybir.dt.bfloat16
AF = mybir.ActivationFunctionType
ALU = mybir.AluOpType
AX = mybir.AxisListType

D_MODEL = 1024
SEQ = 2048
NT = SEQ // 128
DEPTH = 2
D_CONV = 512
CONV_WIDTH = 31
N_HEADS = 8
KV_LORA = 128
Q_LORA = 256
D_IDX = 64
TOPK = 256
N_BUCKETS = 32
MAX_DISTANCE = 128
EPS = 1e-6
D_IN_PROJ = 2504
NB = 14
NCORES = 8
SEQ_PER_CORE = 2
NG = 383


class Src:
    def __init__(self, name, sem, inc):
        self.name, self.sem, self.inc, self.count = name, sem, inc, 0


class Buf:
    __slots__ = ("name", "last_w", "readers")

    def __init__(self, name=""):
        self.name, self.last_w, self.readers = name, None, []


class _Rec:
    def __init__(self):
        self.call = None

    def __getattr__(self, name):
        def f(*a, **k):
            self.call = (name, a, k)
            return self
        return f


class Sched:
    ENG = ("pe", "act", "dve", "pool", "sp")

    def __init__(self, nc, stack, ndma=32):
        self.nc = nc
        self.src = {}
        self.prog = {e: [] for e in self.ENG}
        self.waited = {e: {} for e in self.ENG}
        for e in self.ENG:
            if e == "sp":
                continue
            self.src[e] = Src(e, stack.enter_context(nc.semaphore("sem_" + e)), 1)
        self.ring = [Src("dma%d" % i, stack.enter_context(nc.semaphore("semd%d" % i)), 16) for i in range(ndma)]
        self.ring_pos = 0
        self.nins = 0

    def _deps(self, eng, reads, writes):
        need = {}

        def add(tok):
            if tok is None:
                return
            s, c = tok
            if need.get(s, 0) < c:
                need[s] = c

        for b in reads:
            add(b.last_w)
        for b in writes:
            add(b.last_w)
            for r in b.readers:
                add(r)
        w = self.waited[eng]
        for s, c in need.items():
            if s.name == eng and eng == "pe":
                continue
            if w.get(s, 0) >= c:
                continue
            w[s] = c
            self.prog[eng].append(("w", s, c))

    def op(self, eng, fn, reads=(), writes=()):
        self._deps(eng, reads, writes)
        if eng == "sp":
            s = self.ring[self.ring_pos]
            self.ring_pos = (self.ring_pos + 1) % len(self.ring)
            if s.count and self.waited["sp"].get(s, 0) < s.count:
                self.waited["sp"][s] = s.count
                self.prog["sp"].append(("w", s, s.count))
        else:
            s = self.src[eng]
        s.count += s.inc
        rec = _Rec()
        fn(rec)
        assert rec.call is not None
        self.prog[eng].append(("i", rec.call, s, s.count))
        tok = (s, s.count)
        for b in writes:
            b.last_w = tok
            b.readers = []
        for b in reads:
            if len(b.readers) > 64:
                best = {}
                for (ss, cc) in b.readers:
                    if best.get(ss, 0) < cc:
                        best[ss] = cc
                b.readers = list(best.items())
            b.readers.append(tok)
        self.nins += 1
        return tok

    def finish(self):
        for s in self.ring:
            if s.count:
                self.prog["sp"].append(("w", s, s.count))

    def emit(self):
        nc = self.nc
        engmap = {"pe": "tensor", "act": "scalar", "dve": "vector", "pool": "gpsimd", "sp": "sync"}
        miles = {}
        for e in self.ENG:
            for it in self.prog[e]:
                if it[0] == "w" and it[1].inc == 1:
                    miles.setdefault(it[1], set()).add(it[2])
        rank = {}
        for src, st in miles.items():
            for r, idx in enumerate(sorted(st)):
                rank[(src, idx)] = r + 1
        self.n_inc = sum(len(v) for v in miles.values())
        with nc.Block() as block:
            for e in self.ENG:
                prog = self.prog[e]

                def body(engine, prog=prog):
                    for it in prog:
                        src = it[1] if it[0] == "w" else it[2]
                        if it[0] == "w":
                            if src.inc == 1:
                                engine.wait_ge(src.sem, rank[(src, it[2])])
                            else:
                                engine.wait_ge(src.sem, it[2])
                        else:
                            name, a, k = it[1]
                            ins = getattr(engine, name)(*a, **k)
                            if src.inc != 1:
                                ins.then_inc(src.sem, src.inc)
                            elif (src, it[3]) in rank:
                                ins.then_inc(src.sem, 1)

                getattr(block, engmap[e])(body)


def t5_bucket_np(n):
    max_exact = N_BUCKETS // 2
    n = np.maximum(n, 0)
    nf = np.maximum(n, 1).astype(np.float32)
    large = max_exact + (np.log(nf / np.float32(max_exact)) / np.float32(math.log(MAX_DISTANCE / max_exact))
                         * np.float32(N_BUCKETS - max_exact)).astype(np.int32)
    large = np.minimum(large, N_BUCKETS - 1)
    return np.where(n < max_exact, n, large)


def build_program(layers=(0, 1), nseq=SEQ_PER_CORE, debug=None):
    nc = bass.Bass("TRN2", target_bir_lowering=False)
    L = DEPTH
    dram = {}

    def din(name, shape):
        dram[name] = nc.dram_tensor(name, list(shape), F32, kind="ExternalInput").ap()
        return dram[name]

    x_d = din("x", [nseq, SEQ, D_MODEL])
    cT_d = din("cT", [128, 8, nseq])
    wada_d = din("w_adaP", [L, 8, 128, 8, 384])
    NJOB = 17
    wpack_d = din("wpack", [L, NJOB, 128, 2048])
    badaT_d = din("b_adaT", [128, L, 24])
    gpreT_d = din("g_preT", [128, L, 8])
    gpostT_d = din("g_postT", [128, L, 8])
    convw_d = din("conv_wT", [128, L, 4, CONV_WIDTH])
    convb_d = din("conv_bT", [128, L, 4])
    lng_d = din("ln_gT", [128, L, 4])
    lnb_d = din("ln_bT", [128, L, 4])
    qng_d = din("q_norm_gT", [128, L, 2])
    kvng_d = din("kv_norm_gT", [128, L, 1])
    relb_d = din("rel_bias", [N_BUCKETS, N_HEADS])
    relbT_d = din("rel_biasT", [N_HEADS, N_BUCKETS])
    oh_d = din("oh", [N_BUCKETS, NG])
    out_d = nc.dram_tensor("out", [nseq, SEQ, D_MODEL], F32, kind="ExternalOutput").ap()
    scr_d = nc.dram_tensor("scr_g", [N_HEADS, NG], F32, kind="Internal").ap()
    dbg_d = {}
    if debug:
        for name, shape in debug.items():
            dbg_d[name] = nc.dram_tensor("dbg_" + name, list(shape), F32, kind="ExternalOutput").ap()

    with ExitStack() as st:
        S = Sched(nc, st)
        off = [16640]

        def sb(name, shape, dt, at=None):
            nbytes = int(np.prod(shape[1:])) * (4 if dt == F32 else 2)
            nbytes = (nbytes + 31) // 32 * 32
            if at is None:
                at = off[0]
                off[0] += nbytes
            t = nc.alloc_sbuf_tensor_at(name, list(shape), dt, offset=at)
            return t

        psum = nc.alloc_psum_tensor("psum", [128, 8, 512], F32)
        pbuf = [Buf("ps%d" % i) for i in range(8)]

        def pbank(i):
            return psum[:, i, :]

        ident_f = sb("ident_f", [128, 128], F32); b_identf = Buf()
        ident_b = sb("ident_b", [128, 128], BF16); b_identb = Buf()
        ones_f = sb("ones_f", [128, 128], F32); b_onesf = Buf()
        ones_b = sb("ones_b", [128, 128], BF16); b_onesb = Buf()
        jf = sb("jf", [128, 128], F32); b_jf = Buf()
        causal = sb("causal", [128, 128], F32); b_causal = Buf()
        pow2 = sb("pow2", [128, NB + 2], F32); b_pow2 = Buf()
        negbig = sb("negbig", [128, 1], F32); b_negbig = Buf()
        cT = sb("cT", [128, 8, nseq], F32); b_cT = Buf()
        cact = sb("cact", [128, 8, nseq], F32); b_cact = Buf()
        modT = sb("modT", [128, L, 24, nseq], F32); b_modT = Buf()
        badaT = sb("badaT", [128, L, 24], F32); b_small = Buf()
        gpreT = sb("gpreT", [128, L, 8], F32)
        gpostT = sb("gpostT", [128, L, 8], F32)
        convw = sb("convw", [128, L, 4, CONV_WIDTH], F32)
        convb = sb("convb", [128, L, 4], F32)
        lng = sb("lng", [128, L, 4], F32)
        lnb = sb("lnb", [128, L, 4], F32)
        qng = sb("qng", [128, L, 2], F32)
        kvng = sb("kvng", [128, L, 1], F32)
        b31bc = sb("b31bc", [128, N_HEADS], F32); b_b31 = Buf()
        tbc = sb("tbc", [128, N_HEADS, 256], BF16); b_tbc = Buf()
        xs_res = sb("xres", [128, NT, D_MODEL], F32)
        b_x = [Buf("x%d" % i) for i in range(NT)]
        dummy = sb("dummy", [128, 8], F32)
        PB = off[0]
        LIMIT = 229376 - 64
        PX1 = PB
        PX2 = PX1 + 16384
        PX3 = PX2 + 16384
        PX4 = PX3 + 40704
        PX5 = PX4 + 57344

        S.op("pool", lambda e: e.memset(ident_f[:], 1.0), writes=[b_identf])
        S.op("pool", lambda e: e.affine_select(out=ident_f[:], in_=ident_f[:], pattern=[[-1, 128]], compare_op=ALU.is_equal,
                                               fill=0.0, base=0, channel_multiplier=1), reads=[b_identf], writes=[b_identf])
        S.op("pool", lambda e: e.tensor_copy(out=ident_b[:], in_=ident_f[:]), reads=[b_identf], writes=[b_identb])
        S.op("pool", lambda e: e.memset(ones_f[:], 1.0), writes=[b_onesf])
        S.op("pool", lambda e: e.memset(ones_b[:], 1.0), writes=[b_onesb])
        S.op("pool", lambda e: e.memset(jf[:], 1.0), writes=[b_jf])
        S.op("pool", lambda e: e.affine_select(out=jf[:], in_=jf[:], pattern=[[1, 128]], compare_op=ALU.is_equal,
                                               fill=0.0, base=-127, channel_multiplier=1), reads=[b_jf], writes=[b_jf])
        S.op("pool", lambda e: e.memset(causal[:], 0.0), writes=[b_causal])
        S.op("pool", lambda e: e.affine_select(out=causal[:], in_=causal[:], pattern=[[-1, 128]], compare_op=ALU.is_ge,
                                               fill=-1e30, base=0, channel_multiplier=1), reads=[b_causal], writes=[b_causal])
        for j in range(NB + 2):
            v = 2.0 ** (-min(j + 1, NB))
            S.op("pool", lambda e, j=j, v=v: e.memset(pow2[:, j:j + 1], v), writes=[b_pow2])
        S.op("pool", lambda e: e.memset(negbig[:], -1e29), writes=[b_negbig])

        for (t, d) in ((cT, cT_d), (badaT, badaT_d), (gpreT, gpreT_d), (gpostT, gpostT_d), (convw, convw_d), (convb, convb_d),
                       (lng, lng_d), (lnb, lnb_d), (qng, qng_d), (kvng, kvng_d)):
            S.op("sp", lambda e, t=t, d=d: e.dma_start(out=t[:], in_=d), writes=[b_small if t is not cT else b_cT])
        S.op("sp", lambda e: e.dma_start(out=b31bc[:], in_=bass.AP(tensor=relb_d.tensor, offset=31 * N_HEADS,
                                                                    ap=[[0, 128], [1, N_HEADS]])), writes=[b_b31])

        po = PX3
        relb_s = sb("relb_s", [128, N_HEADS], F32, at=po); po += 32
        relbT_s = sb("relbT_s", [128, N_BUCKETS], F32, at=po); po += 128
        nb31 = sb("nb31", [128, 1], F32, at=po); po += 32
        oh_s = sb("oh_s", [128, NG], F32, at=po); po += 1536
        gc_s = sb("gc_s", [128, NG], F32, at=po); po += 1536
        tbrev = sb("tbrev", [128, N_HEADS, 256], F32, at=po); po += 8192
        wada_st = [sb("wada_st%d" % i, [128, 8, 384], F32, at=po + i * 12288) for i in range(2)]
        po += 2 * 12288
        wada_bf = [sb("wada_bf%d" % i, [128, 8, 384], BF16, at=po + i * 6144) for i in range(2)]
        po += 2 * 6144
        cact_bf = sb("cact_bf", [128, 8, nseq], BF16, at=po); po += 64
        b_wadab = [Buf(), Buf()]; b_cactb = Buf()
        assert po <= LIMIT
        b_pro = Buf(); b_gc = Buf(); b_scr = Buf(); b_tbrev = Buf()
        b_wada = [Buf(), Buf()]

        S.op("sp", lambda e: e.dma_start(out=relb_s[0:N_BUCKETS, :], in_=relb_d), writes=[b_pro])
        S.op("sp", lambda e: e.dma_start(out=relbT_s[0:N_HEADS, :], in_=relbT_d), writes=[b_pro])
        S.op("sp", lambda e: e.dma_start(out=oh_s[0:N_BUCKETS, :], in_=oh_d), writes=[b_pro])
        S.op("dve", lambda e: e.tensor_scalar(out=nb31[0:N_HEADS, :], in0=relbT_s[0:N_HEADS, 31:32], scalar1=-1.0, scalar2=None,
                                              op0=ALU.mult), reads=[b_pro], writes=[b_pro])
        S.op("pe", lambda e: e.matmul(psum[0:N_HEADS, 0, 0:NG], lhsT=relb_s[0:N_BUCKETS, :], rhs=oh_s[0:N_BUCKETS, :],
                                      start=True, stop=True), reads=[b_pro], writes=[pbuf[0]])
        S.op("act", lambda e: e.activation(out=gc_s[0:N_HEADS, :], in_=psum[0:N_HEADS, 0, 0:NG], func=AF.Exp,
                                           bias=nb31[0:N_HEADS, 0:1], scale=1.0), reads=[pbuf[0], b_pro], writes=[b_gc])
        S.op("sp", lambda e: e.dma_start(out=scr_d, in_=gc_s[0:N_HEADS, :]), reads=[b_gc], writes=[b_scr])
        S.op("sp", lambda e: e.dma_start(out=tbrev[:], in_=bass.AP(tensor=scr_d.tensor, offset=0,
                                                                    ap=[[1, 128], [NG, N_HEADS], [1, 256]])),
             reads=[b_scr], writes=[b_tbrev])
        for h in range(N_HEADS):
            bk = 1 + (h % 2)
            S.op("pe", lambda e, h=h, bk=bk: e.matmul(psum[:, bk, 0:256], lhsT=jf[:], rhs=tbrev[:, h, :], start=True, stop=True),
                 reads=[b_jf, b_tbrev], writes=[pbuf[bk]])
            S.op("act", lambda e, h=h, bk=bk: e.activation(out=tbc[:, h, :], in_=psum[:, bk, 0:256], func=AF.Copy),
                 reads=[pbuf[bk]], writes=[b_tbc])

        S.op("act", lambda e: e.activation(out=cact[:], in_=cT[:], func=AF.Silu), reads=[b_cT], writes=[b_cact])
        S.op("act", lambda e: e.activation(out=cact_bf[:], in_=cT[:], func=AF.Silu), reads=[b_cT], writes=[b_cactb])
        gi = 0
        for l in layers:
            for g in range(8):
                slot = gi % 2
                gi += 1
                S.op("sp", lambda e, slot=slot, g=g, l=l: e.dma_start(out=wada_st[slot][:], in_=wada_d[l, g]),
                     writes=[b_wada[slot]])
                S.op("dve" if g % 2 == 0 else "pool", lambda e, slot=slot: e.tensor_copy(out=wada_bf[slot][:], in_=wada_st[slot][:]),
                     reads=[b_wada[slot]], writes=[b_wadab[slot]])
                for jj in range(3):
                    j = g * 3 + jj
                    for k in range(8):
                        S.op("pe", lambda e, slot=slot, jj=jj, j=j, k=k: e.matmul(
                            psum[:, 3, j * nseq:(j + 1) * nseq], lhsT=wada_bf[slot][:, k, jj * 128:(jj + 1) * 128],
                            rhs=cact_bf[:, k, :], start=(k == 0), stop=(k == 7)),
                            reads=[b_wadab[slot], b_cactb], writes=[pbuf[3]])
            S.op("dve", lambda e, l=l: e.tensor_tensor(
                out=modT[:, l, :, :], in0=psum[:, 3, 0:24 * nseq].rearrange("p (j b) -> p j b", b=nseq),
                in1=badaT[:, l, :].unsqueeze(2).to_broadcast([128, 24, nseq]), op=ALU.add),
                reads=[pbuf[3], b_small], writes=[b_modT])

        yconvT = sb("yconvT", [128, 4, SEQ], BF16, at=PX1)
        sgT = sb("sgT", [128, 4, SEQ], BF16, at=PX2)
        yc = sb("yc", [128, 4, SEQ], BF16, at=PX2)
        o = PX4
        hT = sb("hT", [128, 8, SEQ], BF16, at=o); o += 32768
        wst = [sb("wst%d" % i, [128, 2048], F32, at=o + i * 8192) for i in range(2)]; o += 16384
        wbf = [sb("wbf%d" % i, [128, 2048], BF16, at=o + i * 4096) for i in range(2)]; o += 8192
        assert o == PX5
        b_hT = [Buf("hT%d" % i) for i in range(4)]
        b_yconv = [Buf() for _ in range(4)]
        b_sg = [Buf() for _ in range(8)]
        b_yc = [Buf() for _ in range(4)]
        b_wst = [Buf(), Buf()]
        b_wbf = [Buf(), Buf()]

        o = PX3
        abc = sb("abc", [128, D_MODEL], F32, at=o); o += 4096
        shbc = sb("shbc", [128, D_MODEL], F32, at=o); o += 4096
        xs = [sb("xs%d" % i, [128, D_MODEL], F32, at=o + i * 4096) for i in range(2)]; o += 8192
        dg = [sb("dg%d" % i, [128, 128], F32, at=o + i * 512) for i in range(2)]; o += 1024
        vecT = sb("vecT", [128, 3, 8], F32, at=o); o += 96
        ssq = sb("ssq", [128, NT], F32, at=o); o += 64
        rs16 = sb("rs16", [128, NT], F32, at=o); o += 64
        rstd16 = sb("rstd16", [128, NT], F32, at=o); o += 64
        assert o <= PX4
        b_abc = Buf(); b_shbc = Buf(); b_xs = [Buf(), Buf()]; b_dg = [Buf(), Buf()]; b_vecT = Buf()
        b_ssq = Buf(); b_rs16 = Buf(); b_rstd16 = Buf()

        o = PX3
        apad = [sb("apad%d" % i, [128, 30 + SEQ], BF16, at=o + i * 4160) for i in range(2)]; o += 8320
        sig = [sb("sig%d" % i, [128, 512], BF16, at=o + i * 1024) for i in range(2)]; o += 2048
        cdiag = sb("cdiag", [128, CONV_WIDTH, 128], BF16, at=o); o += 7936
        ycsq = [sb("ycsq0", [128, 512], BF16, at=o), sb("ycsq1", [128, 512], BF16, at=PX3 + 16384)]; o += 1024
        zt1 = sb("zt1", [128, 4, 512], BF16, at=o); o += 4096
        m2b = sb("m2b", [128, 512], F32, at=o); o += 2048
        varb = sb("varb", [128, 512], F32, at=o); o += 2048
        dtmp = [sb("dtmp0", [128, 512], F32, at=o)] * 2; o += 2048
        zt0 = sb("zt0", [128, 4, 512], BF16, at=o); o += 4096
        ztb = [zt0, zt1]
        sgc = [sb("sgc%d" % i, [128, 512], BF16, at=o + i * 1024) for i in range(2)]; o += 2048
        wpw2b = sb("wpw2b", [128, 4, 512], BF16, at=o); o += 4096
        assert o <= PX4, o
        meanb4 = [sb("meanb4_%d" % i, [128, 512], F32, at=PX3 + i * 2048) for i in range(4)]
        rstdb4 = [sb("rstdb4_%d" % i, [128, 512], F32, at=PX3 + 8192 + i * 2048) for i in range(4)]
        assert 16384 + 1024 <= 8320 + 2048 + 7936
        b_apad = [Buf(), Buf()]; b_sig = [Buf(), Buf()]; b_cdiag = Buf(); b_ycsq = [Buf(), Buf()]
        b_mean4 = [Buf() for _ in range(4)]; b_m2b = Buf(); b_varb = Buf(); b_rstd4 = [Buf() for _ in range(4)]; b_dtmp = [Buf()] * 2
        b_ztb = [[Buf() for _ in range(4)] for _ in range(2)]; b_sgc = [Buf(), Buf()]; b_wpw2b = Buf()

        o = PX3
        cqnT = sb("cqnT", [128, 2, SEQ], BF16, at=o); o += 8192
        kvnT = sb("kvnT", [128, SEQ], BF16, at=o); o += 4096
        kidx2 = sb("kidx2", [128, SEQ], BF16, at=o); o += 4096
        vaug = sb("vaug", [128, NT, N_HEADS, 65], BF16, at=o); o += 16640
        wtok = sb("wtok", [128, NT, N_HEADS], F32, at=o); o += 512
        wuqb = sb("wuqb", [128, 2, 1024], BF16, at=o); o += 4096
        wqib = sb("wqib", [128, 2, 512], BF16, at=o); o += 2048
        wuvb = sb("wuvb", [128, 512], BF16, at=o); o += 1024
        assert o <= PX4, o
        o = PX5
        sqc = [sb("sqc0", [128, 512], BF16, at=o)] * 2; o += 1024
        rstdc = [sb("rstdc0", [128, 512], F32, at=o)] * 2; o += 2048
        kdw = sb("kdw", [128, 8, 128], BF16, at=o); o += 2048
        assert o <= LIMIT, o
        b_cqn = [Buf() for _ in range(8)]
        b_kvn = Buf(); b_kidx = Buf(); b_vaug = Buf(); b_wtok = Buf()
        b_wuqb = Buf(); b_wqib = Buf(); b_wuvb = Buf()
        b_sqc = [Buf()] * 2; b_rstdc = [Buf()] * 2; b_kdw = Buf()

        o = PX4
        qblk = [sb("qblk%d" % i, [128, N_HEADS, 256], BF16, at=o + i * 4096) for i in range(2)]; o += 8192
        qiblk = [sb("qiblk0", [128, 4, 256], BF16, at=o)] * 2; o += 2048
        score = [sb("score%d" % i, [128, SEQ], F32, at=o + i * 8192) for i in range(2)]; o += 16384
        maskts = [sb("maskts%d" % i, [128, SEQ], BF16, at=o + i * 4096) for i in range(2)]; o += 8192
        maskT = sb("maskT", [128, NT, 256], BF16, at=o); o += 8192
        rt = [sb("rt%d" % i, [128, 512], BF16, at=o + i * 1024) for i in range(4)]; o += 4096
        et = [sb("et%d" % i, [128, 256], BF16, at=o + i * 512) for i in range(4)]; o += 2048
        pm = [sb("pm%d" % i, [128, 256], BF16, at=o + i * 512) for i in range(4)]; o += 2048
        dgw = [sb("dgw%d" % i, [128, N_HEADS, 128], BF16, at=o + i * 2048) for i in range(2)]; o += 4096
        ytok = sb("ytok", [128, 2, 512], F32, at=o); o += 4096
        bis = sb("bis", [128, 8], F32, at=o); o += 32
        steps = sb("steps", [128, NB + 2], F32, at=o); o += 96
        cands = sb("cands", [128, NB + 2], F32, at=o); o += 96
        cnts = sb("cnts", [128, NB + 2], F32, at=o); o += 96
        sels = sb("sels", [128, NB + 2], F32, at=o); o += 96
        rcp = sb("rcp", [128, 2, N_HEADS], F32, at=o); o += 64
        osb = sb("osb", [128, 2, N_HEADS, 65], F32, at=o); o += 4160
        assert o <= LIMIT, (o, LIMIT)
        b_osb = Buf()
        b_qblk = [Buf(), Buf()]; b_qiblk = [Buf()] * 2; b_score = [Buf(), Buf()]; b_maskts = [Buf(), Buf()]
        b_maskT = [Buf(), Buf()]
        b_rt = [Buf() for _ in range(4)]; b_et = [Buf() for _ in range(4)]; b_pm = [Buf() for _ in range(4)]
        b_dgw = [Buf(), Buf()]; b_ytok = [Buf(), Buf()]; b_bis = Buf(); b_steps = Buf()
        b_cands = [Buf() for _ in range(NB + 2)]; b_cnts = [Buf() for _ in range(NB + 2)]; b_sels = [Buf() for _ in range(NB + 2)]
        b_rcp = Buf()

        o = PX3
        woutb = sb("woutb", [128, 8, 1024], BF16, at=o); o += 16384
        gbc = sb("gbc", [128, D_MODEL], F32, at=o); o += 4096
        etmp = [sb("etmp%d" % i, [128, 512], F32, at=o + i * 2048) for i in range(2)]; o += 4096
        ejunk = sb("ejunk", [128, 512], F32, at=o); o += 2048
        ess = sb("ess", [128, 4], F32, at=o); o += 32
        dgE = [sb("dgE%d" % i, [128, 128], F32, at=o + i * 512) for i in range(2)]; o += 1024
        gvT = sb("gvT", [128, 8], F32, at=o); o += 32
        assert o <= PX4, o
        b_woutb = [Buf() for _ in range(4)]; b_gbc = Buf(); b_etmp = [Buf(), Buf()]; b_ejunk = Buf(); b_ess = Buf()
        b_dgE = [Buf(), Buf()]; b_gvT = Buf()

        b_region = Buf("region")

        def phase_barrier(extra=()):
            S.op("pool", lambda e: e.memset(dummy[:, 0:1], 0.0), writes=[b_region] + list(extra))

        RG = [b_region]

        wcnt = [0]
        wjob = [0, 0]

        def load_w(parts, dst_writes, casts):
            slot = wcnt[0] % 2
            wcnt[0] += 1
            stg = wst[slot]
            jn = wjob[1]
            wjob[1] += 1
            assert jn < NJOB
            S.op("sp", lambda e, stg=stg, jn=jn: e.dma_start(out=stg[:], in_=wpack_d[wjob[0], jn]), reads=RG, writes=[b_wst[slot]])
            for (oap, ifn, obufs) in casts:
                S.op("dve", lambda e, oap=oap, ifn=ifn, stg=stg: e.tensor_copy(out=oap, in_=ifn(stg)),
                     reads=[b_wst[slot]] + RG, writes=obufs)

        def win_cols(l, c0, c1):
            return None

        def stage3(stg, n):
            return stg[:, 0:8 * n].rearrange("p (k n) -> p k n", k=8)

        def load_x_quarter(b, q4):
            S.op("sp", lambda e: e.dma_start(out=xs_res[:, q4 * 4:(q4 + 1) * 4, :],
                                             in_=x_d[b, q4 * 512:(q4 + 1) * 512, :].rearrange("(n p) d -> p n d", p=128)),
                 writes=[b_x[q4 * 4 + i] for i in range(4)])

        def bcast_rows(vec_col_fn, dst, dst_buf, dgs, b_dgs, extra_reads):
            for half in range(2):
                bk = 4 + half
                for kk in range(4):
                    k = half * 4 + kk
                    sl = k % 2
                    S.op("dve", lambda e, k=k, sl=sl: e.tensor_scalar(out=dgs[sl][:], in0=ident_f[:], scalar1=vec_col_fn(k),
                                                                      scalar2=None, op0=ALU.mult),
                         reads=[b_identf] + extra_reads + RG, writes=[b_dgs[sl]])
                    S.op("pe", lambda e, kk=kk, sl=sl, bk=bk: e.matmul(psum[:, bk, kk * 128:(kk + 1) * 128], lhsT=ones_f[:],
                                                                      rhs=dgs[sl][:], start=True, stop=True),
                         reads=[b_onesf, b_dgs[sl]], writes=[pbuf[bk]])
                S.op("act", lambda e, half=half, bk=bk: e.activation(out=dst[:, half * 512:(half + 1) * 512], in_=psum[:, bk, :],
                                                                     func=AF.Copy), reads=[pbuf[bk]] + RG, writes=[dst_buf])

        def block(b, l, last_layer):
            wjob[0] = l
            wjob[1] = 0
            phase_barrier()
            S.op("dve", lambda e: e.scalar_tensor_tensor(out=vecT[:, 0, :], in0=modT[:, l, 8:16, b], scalar=1.0, in1=gpreT[:, l, :],
                                                         op0=ALU.add, op1=ALU.mult), reads=[b_modT, b_small] + RG, writes=[b_vecT])
            S.op("dve", lambda e: e.tensor_copy(out=vecT[:, 1, :], in_=modT[:, l, 0:8, b]), reads=[b_modT] + RG, writes=[b_vecT])
            bcast_rows(lambda k: vecT[:, 0, k:k + 1], abc, b_abc, dg, b_dg, [b_vecT])
            bcast_rows(lambda k: vecT[:, 1, k:k + 1], shbc, b_shbc, dg, b_dg, [b_vecT])
            for tt in range(NT):
                S.op("act", lambda e, tt=tt: e.activation(out=xs[tt % 2][:], in_=xs_res[:, tt, :], func=AF.Square,
                                                          accum_out=ssq[:, tt:tt + 1]),
                     reads=[b_x[tt]] + RG, writes=[b_xs[tt % 2], b_ssq])
            S.op("act", lambda e: e.activation(out=rs16[:], in_=ssq[:], func=AF.Sqrt, bias=EPS, scale=1.0 / D_MODEL),
                 reads=[b_ssq] + RG, writes=[b_rs16])
            S.op("dve", lambda e: e.reciprocal(out=rstd16[:], in_=rs16[:]), reads=[b_rs16] + RG, writes=[b_rstd16])
            for tt in range(NT):
                sl = tt % 2
                S.op("dve", lambda e, tt=tt, sl=sl: e.scalar_tensor_tensor(out=xs[sl][:], in0=xs_res[:, tt, :], scalar=rstd16[:, tt:tt + 1],
                                                                           in1=abc[:], op0=ALU.mult, op1=ALU.mult),
                     reads=[b_x[tt], b_rstd16, b_abc] + RG, writes=[b_xs[sl]])
                S.op("dve", lambda e, sl=sl: e.tensor_tensor(out=xs[sl][:], in0=xs[sl][:], in1=shbc[:], op=ALU.add),
                     reads=[b_xs[sl], b_shbc] + RG, writes=[b_xs[sl]])
                for half in range(2):
                    bk = (tt * 2 + half) % 4
                    for kk in range(4):
                        k = half * 4 + kk
                        S.op("pe", lambda e, sl=sl, k=k, kk=kk, bk=bk: e.transpose(out=psum[:, bk, kk * 128:(kk + 1) * 128],
                                                                                  in_=xs[sl][:, k * 128:(k + 1) * 128], identity=ident_f[:]),
                             reads=[b_xs[sl], b_identf], writes=[pbuf[bk]])
                    S.op("act", lambda e, tt=tt, half=half, bk=bk: e.activation(
                        out=hT[:, half * 4:(half + 1) * 4, tt * 128:(tt + 1) * 128],
                        in_=psum[:, bk, :].rearrange("p (k t) -> p k t", k=4), func=AF.Copy),
                        reads=[pbuf[bk]] + RG, writes=[b_hT[tt // 4]])

            phase_barrier()

            def inproj(wtile_fn, bk, tb, n=512, k_list=range(8)):
                for k in k_list:
                    S.op("pe", lambda e, k=k: e.matmul(psum[:, bk, 0:n], lhsT=wtile_fn(k), rhs=hT[:, k, tb * 512:tb * 512 + n],
                                                       start=(k == 0), stop=(k == 7)),
                         reads=[b_hT[tb]] + wt_reads[0], writes=[pbuf[bk]])

            wt_reads = [[]]
            load_w([(lambda stg: stg[:].rearrange("p (k n) -> p k n", k=4), None)], None,
                   [(wpw2b[:], lambda stg: stg[:].rearrange("p (k n) -> p k n", k=4), [b_wpw2b])])
            for c in range(4):
                slot = wcnt[0] % 2
                wb = wbf[slot]
                wb3 = wb[:].rearrange("p (k n) -> p k n", k=8)
                load_w([(lambda stg: stage3(stg, 256)[:, :, 0:128], win_cols(l, c * 128, (c + 1) * 128)),
                        (lambda stg: stage3(stg, 256)[:, :, 128:256], win_cols(l, 512 + c * 128, 512 + (c + 1) * 128))], None,
                       [(wb3, lambda stg: stage3(stg, 256), [b_wbf[slot]])])
                ap_ = apad[c % 2]
                bap = b_apad[c % 2]
                S.op("pool", lambda e, ap_=ap_: e.memset(ap_[:, 0:30], 0.0), reads=RG, writes=[bap])
                for tb in range(4):
                    wt_reads[0] = [b_wbf[slot]]
                    inproj(lambda k: wb3[:, k, 128:256], 0, tb)
                    inproj(lambda k: wb3[:, k, 0:128], 1, tb)
                    sl = tb % 2
                    S.op("act", lambda e, sl=sl: e.activation(out=sig[sl][:], in_=psum[:, 0, :], func=AF.Sigmoid),
                         reads=[pbuf[0]] + RG, writes=[b_sig[sl]])
                    S.op("dve", lambda e, sl=sl, tb=tb, ap_=ap_: e.tensor_tensor(out=ap_[:, 30 + tb * 512:30 + (tb + 1) * 512], in0=psum[:, 1, :],
                                                                                in1=sig[sl][:], op=ALU.mult),
                         reads=[pbuf[1], b_sig[sl]] + RG, writes=[bap])
                    for k in range(tb * 8, min(CONV_WIDTH, tb * 8 + 8)):
                        S.op("dve", lambda e, k=k, c=c: e.tensor_scalar(out=cdiag[:, k, :], in0=ident_b[:], scalar1=convw[:, l, c, k:k + 1],
                                                                        scalar2=None, op0=ALU.mult),
                             reads=[b_identb, b_small] + RG, writes=[b_cdiag])
                for tb in range(4):
                    bk = 2 + (tb % 2)
                    for k in range(CONV_WIDTH):
                        S.op("pe", lambda e, k=k, tb=tb, bk=bk, ap_=ap_: e.matmul(psum[:, bk, :], lhsT=cdiag[:, k, :],
                                                                                 rhs=ap_[:, k + tb * 512:k + tb * 512 + 512],
                                                                                 start=(k == 0), stop=(k == CONV_WIDTH - 1)),
                             reads=[b_cdiag, bap], writes=[pbuf[bk]])
                    S.op("act", lambda e, tb=tb, bk=bk, c=c: e.activation(out=yc[:, c, tb * 512:(tb + 1) * 512], in_=psum[:, bk, :],
                                                                          func=AF.Identity, bias=convb[:, l, c:c + 1], scale=1.0),
                         reads=[pbuf[bk], b_small] + RG, writes=[b_yc[c]])
            gslot = []
            for g in range(2):
                sl_ = wcnt[0] % 2
                gslot.append(sl_)
                load_w([(lambda stg: stage3(stg, 256), win_cols(l, 1024 + g * 256, 1024 + (g + 1) * 256))], None,
                       [(wbf[sl_][:].rearrange("p (k n) -> p k n", k=8), lambda stg: stage3(stg, 256), [b_wbf[sl_]])])
            alias_w = [b_apad[0], b_apad[1], b_sig[0], b_sig[1], b_cdiag]
            for tb in range(4):
                tsl = slice(tb * 512, (tb + 1) * 512)
                b4, b5 = 4 + 2 * (tb % 2), 5 + 2 * (tb % 2)
                for c in range(4):
                    S.op("pe", lambda e, c=c: e.matmul(psum[:, b4, :], lhsT=ones_b[:], rhs=yc[:, c, tsl], start=(c == 0), stop=(c == 3)),
                         reads=[b_onesb, b_yc[c]], writes=[pbuf[b4]])
                for c in range(4):
                    sl = c % 2
                    S.op("act", lambda e, c=c, sl=sl: e.activation(out=ycsq[sl][:], in_=yc[:, c, tsl], func=AF.Square),
                         reads=[b_yc[c]] + RG, writes=[b_ycsq[sl]] + (alias_w if sl == 1 else []))
                    S.op("pe", lambda e, c=c, sl=sl: e.matmul(psum[:, b5, :], lhsT=ones_b[:], rhs=ycsq[sl][:], start=(c == 0), stop=(c == 3)),
                         reads=[b_onesb, b_ycsq[sl]], writes=[pbuf[b5]])
                S.op("act", lambda e: e.activation(out=meanb4[tb][:], in_=psum[:, b4, :], func=AF.Copy, scale=1.0 / D_CONV),
                     reads=[pbuf[b4]] + RG, writes=[b_mean4[tb]] + alias_w)
                S.op("dve", lambda e: e.tensor_tensor(out=m2b[:], in0=meanb4[tb][:], in1=meanb4[tb][:], op=ALU.mult),
                     reads=[b_mean4[tb]] + RG, writes=[b_m2b])
                S.op("dve", lambda e: e.scalar_tensor_tensor(out=varb[:], in0=psum[:, b5, :], scalar=1.0 / D_CONV, in1=m2b[:],
                                                             op0=ALU.mult, op1=ALU.subtract),
                     reads=[pbuf[b5], b_m2b] + RG, writes=[b_varb])
                S.op("dve", lambda e: e.tensor_scalar(out=varb[:], in0=varb[:], scalar1=0.0, scalar2=None, op0=ALU.max),
                     reads=[b_varb] + RG, writes=[b_varb])
                S.op("act", lambda e: e.activation(out=rstdb4[tb][:], in_=varb[:], func=AF.Sqrt, bias=EPS, scale=1.0),
                     reads=[b_varb] + RG, writes=[b_rstd4[tb]] + alias_w)
                S.op("dve", lambda e: e.reciprocal(out=rstdb4[tb][:], in_=rstdb4[tb][:]), reads=[b_rstd4[tb]] + RG, writes=[b_rstd4[tb]])
            def ln_apply(tb, c):
                tsl = slice(tb * 512, (tb + 1) * 512)
                zt = ztb[tb % 2]; b_zt = b_ztb[tb % 2]
                sl = c % 2
                S.op("dve", lambda e: e.tensor_tensor(out=dtmp[sl][:], in0=yc[:, c, tsl], in1=meanb4[tb][:], op=ALU.subtract),
                     reads=[b_yc[c], b_mean4[tb]] + RG, writes=[b_dtmp[sl]])
                S.op("dve", lambda e: e.tensor_tensor(out=dtmp[sl][:], in0=dtmp[sl][:], in1=rstdb4[tb][:], op=ALU.mult),
                     reads=[b_dtmp[sl], b_rstd4[tb]] + RG, writes=[b_dtmp[sl]])
                S.op("act", lambda e: e.activation(out=zt[:, c, :], in_=dtmp[sl][:], func=AF.Silu,
                                                   bias=lnb[:, l, c:c + 1], scale=lng[:, l, c:c + 1]),
                     reads=[b_dtmp[sl], b_small] + RG, writes=[b_zt[c]])

            for c in range(4):
                ln_apply(0, c)
            for tb in range(4):
                tsl = slice(tb * 512, (tb + 1) * 512)
                zt = ztb[tb % 2]; b_zt = b_ztb[tb % 2]
                for c2 in range(4):
                    bk = 0 + (c2 % 2)
                    bg = 2 + (c2 % 2)
                    for c in range(4):
                        S.op("pe", lambda e, c=c, c2=c2, bk=bk: e.matmul(psum[:, bk, :], lhsT=wpw2b[:, c, c2 * 128:(c2 + 1) * 128],
                                                                        rhs=zt[:, c, :], start=(c == 0), stop=(c == 3)),
                             reads=[b_wpw2b, b_zt[c]], writes=[pbuf[bk]])
                    gs_ = gslot[c2 // 2]
                    wt_reads[0] = [b_wbf[gs_]]
                    wg3 = wbf[gs_][:].rearrange("p (k n) -> p k n", k=8)
                    inproj(lambda k: wg3[:, k, (c2 % 2) * 128:(c2 % 2 + 1) * 128], bg, tb)
                    if tb + 1 < 4:
                        ln_apply(tb + 1, c2)
                    sl = c2 % 2
                    S.op("act", lambda e, sl=sl, bg=bg: e.activation(out=sgc[sl][:], in_=psum[:, bg, :], func=AF.Silu),
                         reads=[pbuf[bg]] + RG, writes=[b_sgc[sl]])
                    S.op("dve", lambda e, sl=sl, c2=c2, bk=bk: e.tensor_tensor(out=yconvT[:, c2, tsl], in0=psum[:, bk, :], in1=sgc[sl][:],
                                                                              op=ALU.mult),
                         reads=[pbuf[bk], b_sgc[sl]] + RG, writes=[b_yconv[tb]])

            phase_barrier()
            attn_scale = (8 ** -0.5) * (D_IDX ** -0.5)
            S.op("pool", lambda e: e.memset(vaug[:, :, :, 64:65], 1.0), reads=RG, writes=[b_vaug])
            load_w([(lambda stg: stg[:].rearrange("p (k n) -> p k n", k=2), None)], None,
                   [(wuqb[:], lambda stg: stg[:].rearrange("p (k n) -> p k n", k=2), [b_wuqb])])
            load_w([(lambda stg: stg[:, 0:1024].rearrange("p (k n) -> p k n", k=2), None),
                    (lambda stg: stg[:, 1024:1536], None)], None,
                   [(wqib[:], lambda stg: stg[:, 0:1024].rearrange("p (k n) -> p k n", k=2), [b_wqib]),
                    (wuvb[:], lambda stg: stg[:, 1024:1536], [b_wuvb])])
            s1 = wcnt[0] % 2
            w1 = wbf[s1][:].rearrange("p (k n) -> p k n", k=8)
            load_w([(lambda stg: stage3(stg, 256), win_cols(l, 1536, 1792))], None, [(w1, lambda stg: stage3(stg, 256), [b_wbf[s1]])])
            s2 = wcnt[0] % 2
            w2 = wbf[s2][:].rearrange("p (k n) -> p k n", k=8)
            load_w([(lambda stg: stage3(stg, 256)[:, :, 0:200], win_cols(l, 1792, 1992))], None,
                   [(w2[:, :, 0:200], lambda stg: stage3(stg, 256)[:, :, 0:200], [b_wbf[s2]]),
                    (kdw[:, :, 0:64], lambda stg: stage3(stg, 256)[:, :, 128:192], [b_kdw]),
                    (kdw[:, :, 64:128], lambda stg: stage3(stg, 256)[:, :, 128:192], [b_kdw])])
            for tb in range(4):
                tsl = slice(tb * 512, (tb + 1) * 512)
                wt_reads[0] = [b_wbf[s1]]
                inproj(lambda k: w1[:, k, 0:128], 0, tb)
                inproj(lambda k: w1[:, k, 128:256], 1, tb)
                for ch in range(2):
                    S.op("act", lambda e, ch=ch: e.activation(out=sqc[ch][:], in_=psum[:, ch, :], func=AF.Square),
                         reads=[pbuf[ch]] + RG, writes=[b_sqc[ch]])
                    S.op("pe", lambda e, ch=ch: e.matmul(psum[:, 2, :], lhsT=ones_b[:], rhs=sqc[ch][:], start=(ch == 0), stop=(ch == 1)),
                         reads=[b_onesb, b_sqc[ch]], writes=[pbuf[2]])
                S.op("act", lambda e: e.activation(out=rstdc[0][:], in_=psum[:, 2, :], func=AF.Sqrt, bias=EPS, scale=1.0 / Q_LORA),
                     reads=[pbuf[2]] + RG, writes=[b_rstdc[0]])
                S.op("dve", lambda e: e.reciprocal(out=rstdc[0][:], in_=rstdc[0][:]), reads=[b_rstdc[0]] + RG, writes=[b_rstdc[0]])
                for ch in range(2):
                    S.op("dve", lambda e, ch=ch: e.scalar_tensor_tensor(out=cqnT[:, ch, tsl], in0=psum[:, ch, :], scalar=qng[:, l, ch:ch + 1],
                                                                        in1=rstdc[0][:], op0=ALU.mult, op1=ALU.mult),
                         reads=[pbuf[ch], b_small, b_rstdc[0]] + RG, writes=[b_cqn[2 * tb], b_cqn[2 * tb + 1]])
                wt_reads[0] = [b_wbf[s2]]
                inproj(lambda k: w2[:, k, 0:128], 3, tb)
                S.op("act", lambda e: e.activation(out=sqc[0][:], in_=psum[:, 3, :], func=AF.Square), reads=[pbuf[3]] + RG, writes=[b_sqc[0]])
                S.op("pe", lambda e: e.matmul(psum[:, 4, :], lhsT=ones_b[:], rhs=sqc[0][:], start=True, stop=True),
                     reads=[b_onesb, b_sqc[0]], writes=[pbuf[4]])
                S.op("act", lambda e: e.activation(out=rstdc[1][:], in_=psum[:, 4, :], func=AF.Sqrt, bias=EPS, scale=1.0 / KV_LORA),
                     reads=[pbuf[4]] + RG, writes=[b_rstdc[1]])
                S.op("dve", lambda e: e.reciprocal(out=rstdc[1][:], in_=rstdc[1][:]), reads=[b_rstdc[1]] + RG, writes=[b_rstdc[1]])
                S.op("dve", lambda e: e.scalar_tensor_tensor(out=kvnT[:, tsl], in0=psum[:, 3, :], scalar=kvng[:, l, 0:1], in1=rstdc[1][:],
                                                             op0=ALU.mult, op1=ALU.mult),
                     reads=[pbuf[3], b_small, b_rstdc[1]] + RG, writes=[b_kvn])
                wt_reads[0] = [b_kdw]
                inproj(lambda k: kdw[:, k, :], 5, tb)
                S.op("act", lambda e: e.activation(out=kidx2[:, tsl], in_=psum[:, 5, :], func=AF.Copy), reads=[pbuf[5]] + RG, writes=[b_kidx])
                for t4 in range(4):
                    tt = tb * 4 + t4
                    for k in range(8):
                        S.op("pe", lambda e, k=k, tt=tt, t4=t4: e.matmul(psum[:, 6, t4 * 8:(t4 + 1) * 8], lhsT=hT[:, k, tt * 128:(tt + 1) * 128],
                                                                        rhs=w2[:, k, 192:200], start=(k == 0), stop=(k == 7)),
                             reads=[b_hT[tb], b_wbf[s2]], writes=[pbuf[6]])
                S.op("act", lambda e, tb=tb: e.activation(out=wtok[:, tb * 4:(tb + 1) * 4, :],
                                                          in_=psum[:, 6, 0:32].rearrange("p (t h) -> p t h", h=8), func=AF.Copy, scale=attn_scale),
                     reads=[pbuf[6]] + RG, writes=[b_wtok])
            for g in range(2):
                sg_ = wcnt[0] % 2
                wg = wbf[sg_][:].rearrange("p (k n) -> p k n", k=8)
                load_w([(lambda stg: stage3(stg, 256), win_cols(l, 1992 + g * 256, 1992 + (g + 1) * 256))], None,
                       [(wg, lambda stg: stage3(stg, 256), [b_wbf[sg_]])])
                for cc in range(2):
                    ch = g * 2 + cc
                    for tb in range(4):
                        bk = (cc * 4 + tb) % 2
                        wt_reads[0] = [b_wbf[sg_]]
                        inproj(lambda k: wg[:, k, cc * 128:(cc + 1) * 128], bk, tb)
                        S.op("act", lambda e, ch=ch, tb=tb, bk=bk: e.activation(out=sgT[:, ch, tb * 512:(tb + 1) * 512], in_=psum[:, bk, :],
                                                                               func=AF.Silu),
                             reads=[pbuf[bk]] + RG, writes=[b_sg[2 * tb], b_sg[2 * tb + 1]])
            for stl in range(NT):
                bk = 2 + (stl % 2)
                S.op("pe", lambda e, stl=stl, bk=bk: e.matmul(psum[:, bk, :], lhsT=kvnT[:, stl * 128:(stl + 1) * 128], rhs=wuvb[:],
                                                             start=True, stop=True), reads=[b_kvn, b_wuvb], writes=[pbuf[bk]])
                S.op("act", lambda e, stl=stl, bk=bk: e.activation(out=vaug[:, stl, :, 0:64],
                                                                   in_=psum[:, bk, :].rearrange("p (h d) -> p h d", h=N_HEADS), func=AF.Copy),
                     reads=[pbuf[bk]] + RG, writes=[b_vaug])

            phase_barrier()
            sm_scale = KV_LORA ** -0.5
            cnt_r = [0]; cnt_e = [0]
            DSK = 2
            psb = psum[:, 7, :].bitcast(BF16).rearrange("p (j t) -> p j t", t=128)

            def gen_Qq(B):
                qs = B % 2
                tcols = slice(B * 256, (B + 1) * 256)
                for hp in range(4):
                    for hh in range(2):
                        h = hp * 2 + hh
                        for ch in range(2):
                            S.op("pe", lambda e, h=h, hh=hh, ch=ch: e.matmul(psum[:, 7, hh * 256:(hh + 1) * 256],
                                                                             lhsT=wuqb[:, ch, h * 128:(h + 1) * 128], rhs=cqnT[:, ch, tcols],
                                                                             start=(ch == 0), stop=(ch == 1)),
                                 reads=[b_wuqb, b_cqn[B]], writes=[pbuf[7]])
                    S.op("act", lambda e, hp=hp: e.activation(out=qblk[qs][:, hp * 2:hp * 2 + 2, :],
                                                              in_=psum[:, 7, :].rearrange("p (h t) -> p h t", h=2), func=AF.Copy),
                         reads=[pbuf[7]] + RG, writes=[b_qblk[qs]])
                    yield 1.0

            def gen_Qi(B):
                qs = B % 2
                tcols = slice(B * 256, (B + 1) * 256)
                for pp in range(2):
                    for pq in range(2):
                        pr = pp * 2 + pq
                        for ch in range(2):
                            S.op("pe", lambda e, pr=pr, pq=pq, ch=ch: e.matmul(psum[:, 7, pq * 256:(pq + 1) * 256],
                                                                               lhsT=wqib[:, ch, pr * 128:(pr + 1) * 128], rhs=cqnT[:, ch, tcols],
                                                                               start=(ch == 0), stop=(ch == 1)),
                                 reads=[b_wqib, b_cqn[B]], writes=[pbuf[7]])
                    S.op("act", lambda e, pp=pp: e.activation(out=qiblk[qs][:, pp * 2:pp * 2 + 2, :],
                                                              in_=psum[:, 7, :].rearrange("p (h t) -> p h t", h=2), func=AF.Copy),
                         reads=[pbuf[7]] + RG, writes=[b_qiblk[qs]])
                    yield 1.0

            def gen_D(i):
                dw = dgw[i % 2]; bdw = b_dgw[i % 2]
                for h in range(N_HEADS):
                    S.op("dve", lambda e, h=h: e.tensor_scalar(out=dw[:, h, :], in0=ident_b[:], scalar1=wtok[:, i, h:h + 1],
                                                               scalar2=None, op0=ALU.mult),
                         reads=[b_identb, b_wtok] + RG, writes=[bdw])
                yield 1.0

            def gen_X(i):
                B, tl = i // 2, i % 2
                qs = B % 2
                Lk = 128 * (i + 1)
                sc = score[i % 2]; bsc = b_score[i % 2]
                dw = dgw[i % 2]; bdw = b_dgw[i % 2]
                nsb = (Lk + 511) // 512

                def emit_dm(item):
                    sbk, h, rs_, c0, w = item
                    S.op("pe", lambda e: e.matmul(psum[:, 6, 0:w], lhsT=dw[:, h, :], rhs=rt[rs_][:, 0:w],
                                                  start=(h == 0), stop=(h == N_HEADS - 1)),
                         reads=[bdw, b_rt[rs_]], writes=[pbuf[6]])
                    if h == N_HEADS - 1:
                        last = (sbk == nsb - 1)
                        wc = w - 128 if last else w
                        if wc > 0:
                            S.op("act", lambda e: e.activation(out=sc[:, c0:c0 + wc], in_=psum[:, 6, 0:wc], func=AF.Copy),
                                 reads=[pbuf[6]] + RG, writes=[bsc])
                        if last:
                            S.op("dve", lambda e: e.tensor_tensor(out=sc[:, c0 + wc:c0 + wc + 128], in0=psum[:, 6, wc:wc + 128],
                                                                  in1=causal[:], op=ALU.add),
                                 reads=[pbuf[6], b_causal] + RG, writes=[bsc])

                pend = []
                for sbk in range(nsb):
                    c0 = sbk * 512
                    w = min(Lk, c0 + 512) - c0
                    for pr in range(4):
                        items = []
                        for hf in range(2):
                            h = pr * 2 + hf
                            rb = 4 + hf
                            rs_ = cnt_r[0] % 4
                            cnt_r[0] += 1
                            S.op("pe", lambda e, hf=hf, rb=rb: e.matmul(
                                psum[:, rb, 0:w], lhsT=qiblk[qs][hf * 64:(hf + 1) * 64, pr, tl * 128:(tl + 1) * 128],
                                rhs=kidx2[hf * 64:(hf + 1) * 64, c0:c0 + w], start=True, stop=True),
                                reads=[b_qiblk[qs], b_kidx], writes=[pbuf[rb]])
                            items.append((sbk, h, rs_, c0, w, rb))
                        for (sbk_, h, rs_, c0_, w_, rb) in items:
                            S.op("act", lambda e, rb=rb, rs_=rs_: e.activation(out=rt[rs_][:, 0:w], in_=psum[:, rb, 0:w], func=AF.Relu),
                                 reads=[pbuf[rb]] + RG, writes=[b_rt[rs_]])
                        for it in pend:
                            emit_dm(it)
                        pend = [it[:5] for it in items]
                        yield 2.0
                for it in pend:
                    emit_dm(it)
                yield 1.0

            dve_counting = [False]

            def gen_Y(i):
                Lk = 128 * (i + 1)
                sc = score[i % 2]; bsc = b_score[i % 2]
                mk = maskts[i % 2]; bmk = b_maskts[i % 2]
                if i < 2:
                    thr_ap = negbig[:, 0:1]
                    thr_reads = [b_negbig]
                else:
                    dve_counting[0] = True
                    S.op("dve", lambda e: e.tensor_reduce(out=bis[:, 0:1], in_=sc[:, 0:Lk], axis=AX.X, op=ALU.max),
                         reads=[bsc] + RG, writes=[b_bis])
                    yield 2.0
                    S.op("dve", lambda e: e.tensor_reduce(out=bis[:, 1:2], in_=sc[:, 0:TOPK], axis=AX.X, op=ALU.min),
                         reads=[bsc] + RG, writes=[b_bis])
                    yield 2.0
                    S.op("dve", lambda e: e.tensor_tensor(out=bis[:, 2:3], in0=bis[:, 0:1], in1=bis[:, 1:2], op=ALU.subtract),
                         reads=[b_bis] + RG, writes=[b_bis])
                    S.op("dve", lambda e: e.tensor_scalar(out=steps[:], in0=pow2[:], scalar1=bis[:, 2:3], scalar2=None, op0=ALU.mult),
                         reads=[b_pow2, b_bis] + RG, writes=[b_steps])
                    S.op("dve", lambda e: e.tensor_tensor(out=cands[:, 0:1], in0=bis[:, 1:2], in1=steps[:, 0:1], op=ALU.add),
                         reads=[b_bis, b_steps] + RG, writes=[b_cands[0]])
                    yield 1.0
                    for j in range(NB):
                        S.op("dve", lambda e, j=j: e.tensor_scalar(out=mk[:, 0:Lk], in0=sc[:, 0:Lk], scalar1=cands[:, j:j + 1],
                                                                   scalar2=None, op0=ALU.is_ge, op1=ALU.add,
                                                                   accum_out=cnts[:, j:j + 1]),
                             reads=[bsc, b_cands[j]] + RG, writes=[bmk, b_cnts[j]])
                        S.op("dve", lambda e, j=j: e.tensor_scalar(out=sels[:, j:j + 1], in0=cnts[:, j:j + 1], scalar1=TOPK - 0.5,
                                                                   scalar2=steps[:, j:j + 1], op0=ALU.is_ge, op1=ALU.mult),
                             reads=[b_cnts[j], b_steps] + RG, writes=[b_sels[j]])
                        S.op("dve", lambda e, j=j: e.scalar_tensor_tensor(out=cands[:, j + 1:j + 2], in0=sels[:, j:j + 1],
                                                                          scalar=cands[:, j:j + 1], in1=steps[:, j + 1:j + 2],
                                                                          op0=ALU.add, op1=ALU.subtract),
                             reads=[b_sels[j], b_cands[j], b_steps] + RG, writes=[b_cands[j + 1]])
                        yield 2.5
                    thr_ap = cands[:, NB:NB + 1]
                    thr_reads = [b_cands[NB]]
                S.op("dve", lambda e: e.tensor_scalar(out=mk[:, 0:Lk], in0=sc[:, 0:Lk], scalar1=thr_ap, scalar2=None, op0=ALU.is_ge),
                     reads=[bsc] + thr_reads + RG, writes=[bmk])
                dve_counting[0] = False
                yield 1.0

            def gen_Z(i):
                tl = i % 2
                mk = maskts[i % 2]; bmk = b_maskts[i % 2]
                for j0 in range(0, i + 1, 8):
                    n = min(8, i + 1 - j0)
                    for jj in range(n):
                        j = j0 + jj
                        S.op("pe", lambda e, j=j, jj=jj: e.transpose(out=psb[:, jj, :], in_=mk[:, j * 128:(j + 1) * 128], identity=ident_b[:]),
                             reads=[bmk, b_identb], writes=[pbuf[7]])
                    S.op("act", lambda e, j0=j0, n=n: e.activation(out=maskT[:, j0:j0 + n, tl * 128:(tl + 1) * 128], in_=psb[:, 0:n, :],
                                                                   func=AF.Copy),
                         reads=[pbuf[7]] + RG, writes=[b_maskT[tl]])
                    yield 1.0

            def gen_W(B):
                i0, i1 = B * 2, B * 2 + 1
                qs = B % 2
                tcols = slice(B * 256, (B + 1) * 256)
                ob = [0, 1]

                def emit_pv(item):
                    h, j, es, c0 = item
                    for tl in range(2):
                        if tl * 128 < c0:
                            continue
                        lastj = i0 if tl == 0 else i1
                        S.op("pe", lambda e, tl=tl, lastj=lastj: e.matmul(
                            psum[:, ob[tl], 0:65], lhsT=pm[es][:, tl * 128:(tl + 1) * 128], rhs=vaug[:, j, h, :],
                            start=(j == 0), stop=(j == lastj)),
                            reads=[b_pm[es], b_vaug], writes=[pbuf[ob[tl]]])
                    if j == i1:
                        for tl in range(2):
                            S.op("act", lambda e, tl=tl: e.activation(out=osb[:, tl, h, :], in_=psum[:, ob[tl], 0:65], func=AF.Copy),
                                 reads=[pbuf[ob[tl]]] + RG, writes=[b_osb])

                pend = []
                for h in range(N_HEADS):
                    for j in range(i1 + 1):
                        c0 = 0 if j <= i0 else 128
                        qb = 2 + (cnt_e[0] % 2)
                        es = cnt_e[0] % 4
                        cnt_e[0] += 1
                        S.op("pe", lambda e, j=j, c0=c0, qb=qb, h=h: e.matmul(psum[:, qb, c0:256], lhsT=kvnT[:, j * 128:(j + 1) * 128],
                                                                             rhs=qblk[qs][:, h, c0:256], start=True, stop=True),
                             reads=[b_kvn, b_qblk[qs]], writes=[pbuf[qb]])
                        S.op("act", lambda e, c0=c0, qb=qb, es=es, h=h: e.activation(out=et[es][:, c0:256], in_=psum[:, qb, c0:256], func=AF.Exp,
                                                                                    bias=b31bc[:, h:h + 1], scale=sm_scale),
                             reads=[pbuf[qb], b_b31] + RG, writes=[b_et[es]])
                        meng = "pool" if dve_counting[0] else "dve"
                        if j >= i0 - 1:
                            if j == i0 - 1:
                                tc0, tc1, u0 = 0, 128, 128
                            elif j == i0:
                                tc0, tc1, u0 = 0, 256, 0
                            else:
                                tc0, tc1, u0 = 128, 256, 0
                            S.op(meng, lambda e, es=es, tc0=tc0, tc1=tc1, u0=u0, h=h: e.tensor_tensor(
                                out=et[es][:, tc0:tc1], in0=et[es][:, tc0:tc1], in1=tbc[:, h, u0:u0 + (tc1 - tc0)], op=ALU.mult),
                                reads=[b_et[es], b_tbc] + RG, writes=[b_et[es]])
                        S.op(meng, lambda e, es=es, c0=c0, j=j: e.tensor_tensor(out=pm[es][:, c0:256], in0=et[es][:, c0:256], in1=maskT[:, j, c0:256],
                                                                               op=ALU.mult),
                             reads=[b_et[es], b_maskT[0], b_maskT[1]] + RG, writes=[b_pm[es]])
                        pend.append((h, j, es, c0))
                        if len(pend) > DSK:
                            emit_pv(pend.pop(0))
                        yield 1.0
                while pend:
                    emit_pv(pend.pop(0))
                yield 1.0
                S.op("dve", lambda e: e.reciprocal(out=rcp[:], in_=osb[:, :, :, 64]), reads=[b_osb] + RG, writes=[b_rcp])
                S.op("dve", lambda e: e.tensor_tensor(out=ytok[:].rearrange("p t (h d) -> p t h d", h=N_HEADS), in0=osb[:, :, :, 0:64],
                                                      in1=rcp[:].unsqueeze(3).to_broadcast([128, 2, N_HEADS, 64]), op=ALU.mult),
                     reads=[b_osb, b_rcp] + RG, writes=[b_ytok[0], b_ytok[1]])
                for ch in range(4):
                    hb = (ch % 2) * 256
                    for tl in range(2):
                        S.op("pe", lambda e, ch=ch, tl=tl, hb=hb: e.transpose(out=psum[:, 7, hb + tl * 128:hb + (tl + 1) * 128],
                                                                             in_=ytok[:, tl, ch * 128:(ch + 1) * 128], identity=ident_f[:]),
                             reads=[b_ytok[tl], b_identf], writes=[pbuf[7]])
                    S.op("dve", lambda e, ch=ch, hb=hb: e.tensor_tensor(out=sgT[:, ch, tcols], in0=psum[:, 7, hb:hb + 256], in1=sgT[:, ch, tcols],
                                                                        op=ALU.mult),
                         reads=[pbuf[7], b_sg[B]] + RG, writes=[b_sg[B]])
                yield 1.0

            def run(g):
                for _ in g:
                    pass

            def chain(*gens):
                for g in gens:
                    for wgt in g:
                        yield wgt

            def par(ga, gb):
                ta = tb_ = 0.0
                a_done = b_done = False
                while not (a_done and b_done):
                    if b_done or (not a_done and ta <= tb_):
                        try:
                            wgt = next(ga); ta += wgt
                            yield wgt * 0.5
                        except StopIteration:
                            a_done = True
                            ta = float("inf")
                    else:
                        try:
                            wgt = next(gb); tb_ += wgt
                            yield wgt * 0.5
                        except StopIteration:
                            b_done = True
                            tb_ = float("inf")

            def interleave(ga, na, gb, nb_):
                a_done = b_done = False
                pa = pb_ = 0.0
                while not (a_done and b_done):
                    if b_done or (not a_done and pa * nb_ <= pb_ * na):
                        try:
                            pa += next(ga)
                        except StopIteration:
                            a_done = True
                    else:
                        try:
                            pb_ += next(gb)
                        except StopIteration:
                            b_done = True

            def n_x(i):
                return 1.0 + 8.0 * ((128 * (i + 1) + 511) // 512)

            def n_y(i):
                return 1.0 if i < 2 else 2.5 * NB + 5.0

            NBLK = NT // 2

            def nothing():
                return
                yield 0.0

            run(gen_Qq(0)); run(gen_Qi(0)); run(gen_D(0)); run(gen_D(1)); run(gen_X(0)); run(gen_Y(0)); run(gen_X(1)); run(gen_Y(1))
            run(gen_Z(0)); run(gen_Z(1))
            run(gen_Qi(1)); run(gen_D(2)); run(gen_D(3)); run(gen_X(2))
            for B in range(NBLK):
                if B + 1 < NBLK:
                    i2, i3 = 2 * B + 2, 2 * B + 3
                    if B + 2 < NBLK:
                        nxt = chain(gen_Qi(B + 2), gen_D(i2 + 2), gen_D(i3 + 2), gen_X(i2 + 2))
                        n_nxt = 4.0 + n_x(i2 + 2)
                    else:
                        nxt = nothing()
                        n_nxt = 0.0
                    side = chain(gen_Qq(B + 1), par(gen_Y(i2), gen_X(i3)), par(gen_Y(i3), nxt))
                    n_side = 4.0 + 0.5 * (n_y(i2) + n_x(i3)) + 0.5 * (n_y(i3) + n_nxt)
                    interleave(gen_W(B), 8.0 * (2 * B + 2) + 2.0, side, n_side)
                    run(gen_Z(i2)); run(gen_Z(i3))
                else:
                    run(gen_W(B))

            phase_barrier()
            S.op("dve", lambda e: e.tensor_tensor(out=gvT[:], in0=modT[:, l, 16:24, b], in1=gpostT[:, l, :], op=ALU.mult),
                 reads=[b_modT, b_small] + RG, writes=[b_gvT])
            bcast_rows(lambda k: gvT[:, k:k + 1], gbc, b_gbc, dgE, b_dgE, [b_gvT])
            for g in range(4):
                load_w([(lambda stg: stage3(stg, 256), None)], None,
                       [(woutb[:, :, g * 256:(g + 1) * 256], lambda stg: stage3(stg, 256), [b_woutb[g]])])
            for tt in range(NT):
                tsl = slice(tt * 128, (tt + 1) * 128)
                for nh in range(2):
                    bk = (tt * 2 + nh) % 4
                    for k in range(8):
                        lhs = yconvT[:, k, tsl] if k < 4 else sgT[:, k - 4, tsl]
                        rd = [b_yconv[tt // 4]] if k < 4 else [b_sg[tt // 2]]
                        S.op("pe", lambda e, lhs=lhs, k=k, nh=nh, bk=bk: e.matmul(psum[:, bk, :], lhsT=lhs, rhs=woutb[:, k, nh * 512:(nh + 1) * 512],
                                                                                 start=(k == 0), stop=(k == 7)),
                             reads=rd + [b_woutb[2 * nh], b_woutb[2 * nh + 1]], writes=[pbuf[bk]])
                    S.op("act", lambda e, nh=nh, bk=bk: e.activation(out=ejunk[:], in_=psum[:, bk, :], func=AF.Square, accum_out=ess[:, nh:nh + 1]),
                         reads=[pbuf[bk]] + RG, writes=[b_ejunk, b_ess])
                S.op("dve", lambda e: e.tensor_tensor(out=ess[:, 2:3], in0=ess[:, 0:1], in1=ess[:, 1:2], op=ALU.add),
                     reads=[b_ess] + RG, writes=[b_ess])
                S.op("act", lambda e: e.activation(out=ess[:, 2:3], in_=ess[:, 2:3], func=AF.Sqrt, bias=EPS, scale=1.0 / D_MODEL),
                     reads=[b_ess] + RG, writes=[b_ess])
                S.op("dve", lambda e: e.reciprocal(out=ess[:, 3:4], in_=ess[:, 2:3]), reads=[b_ess] + RG, writes=[b_ess])
                for nh in range(2):
                    bk = (tt * 2 + nh) % 4
                    S.op("dve", lambda e, nh=nh, bk=bk: e.scalar_tensor_tensor(out=etmp[nh][:], in0=psum[:, bk, :], scalar=ess[:, 3:4],
                                                                               in1=gbc[:, nh * 512:(nh + 1) * 512], op0=ALU.mult, op1=ALU.mult),
                         reads=[pbuf[bk], b_ess, b_gbc] + RG, writes=[b_etmp[nh]])
                    S.op("dve", lambda e, nh=nh, tt=tt: e.tensor_tensor(out=xs_res[:, tt, nh * 512:(nh + 1) * 512],
                                                                         in0=xs_res[:, tt, nh * 512:(nh + 1) * 512], in1=etmp[nh][:], op=ALU.add),
                         reads=[b_etmp[nh], b_x[tt]] + RG, writes=[b_x[tt]])
                if last_layer and (tt % 4 == 3):
                    t0 = tt - 3
                    S.op("sp", lambda e, t0=t0: e.dma_start(out=out_d[b, t0 * 128:(t0 + 4) * 128, :].rearrange("(n p) d -> p n d", p=128),
                                                            in_=xs_res[:, t0:t0 + 4, :]),
                         reads=[b_x[t0], b_x[t0 + 1], b_x[t0 + 2], b_x[t0 + 3]])
                    if b + 1 < nseq:
                        load_x_quarter(b + 1, t0 // 4)

        phase_barrier([b_pro, b_gc, b_tbrev, b_scr, b_wada[0], b_wada[1], b_wadab[0], b_wadab[1], b_cactb, b_tbc, b_modT])
        for q4 in range(4):
            load_x_quarter(0, q4)
        for b in range(nseq):
            for li, l in enumerate(layers):
                block(b, l, li == len(layers) - 1)
        S.finish()
        S.emit()
        build_program.nins = S.nins
    return nc


def _fm(v, nchunk):
    L = v.shape[0]
    return np.ascontiguousarray(v.reshape(L, nchunk, 128).transpose(2, 0, 1)).astype(np.float32)


def pack_weights(inputs):
    f = lambda a: np.asarray(a, dtype=np.float32)
    w_in, w_pw2, w_uq, w_qidx, w_uv, w_out = (f(inputs[k]) for k in ("w_in", "w_pw2", "w_uq", "w_qidx", "w_uv", "w_out"))
    packs = np.zeros((DEPTH, 17, 128, 2048), np.float32)
    for l in range(DEPTH):
        win = w_in[l].reshape(8, 128, D_IN_PROJ).transpose(1, 0, 2)
        jobs = []
        jobs.append(w_pw2[l].reshape(4, 128, 512).transpose(1, 0, 2).reshape(128, 2048))
        for c in range(4):
            t = np.zeros((128, 8, 256), np.float32)
            t[:, :, 0:128] = win[:, :, c * 128:(c + 1) * 128]
            t[:, :, 128:256] = win[:, :, 512 + c * 128:512 + (c + 1) * 128]
            jobs.append(t.reshape(128, 2048))
        for g in range(2):
            jobs.append(win[:, :, 1024 + g * 256:1024 + (g + 1) * 256].reshape(128, 2048))
        jobs.append(w_uq[l].reshape(2, 128, 1024).transpose(1, 0, 2).reshape(128, 2048))
        t = np.zeros((128, 2048), np.float32)
        t[:, 0:1024] = w_qidx[l].reshape(2, 128, 512).transpose(1, 0, 2).reshape(128, 1024)
        t[:, 1024:1536] = w_uv[l].transpose(1, 0, 2).reshape(KV_LORA, N_HEADS * 64)
        jobs.append(t)
        jobs.append(win[:, :, 1536:1792].reshape(128, 2048))
        t = np.zeros((128, 8, 256), np.float32)
        t[:, :, 0:200] = win[:, :, 1792:1992]
        jobs.append(t.reshape(128, 2048))
        for g in range(2):
            jobs.append(win[:, :, 1992 + g * 256:1992 + (g + 1) * 256].reshape(128, 2048))
        wo = w_out[l].reshape(8, 128, D_MODEL).transpose(1, 0, 2)
        for g in range(4):
            jobs.append(wo[:, :, g * 256:(g + 1) * 256].reshape(128, 2048))
        assert len(jobs) == 17
        for j, t in enumerate(jobs):
            packs[l, j] = t
    return packs


def make_in_maps(inputs, ncores=NCORES, nseq=SEQ_PER_CORE):
    f = lambda a: np.ascontiguousarray(np.asarray(a, dtype=np.float32))
    x = f(inputs["x"]); c = f(inputs["c"])
    n = np.arange(NG) - 127
    oh = np.zeros((N_BUCKETS, NG), np.float32)
    oh[t5_bucket_np(n), np.arange(NG)] = 1.0
    conv_w = f(inputs["conv_w"])
    conv_wT = np.ascontiguousarray(conv_w.reshape(DEPTH, CONV_WIDTH, 4, 128).transpose(3, 0, 2, 1))
    rel_bias = f(inputs["rel_bias"])
    w_ada = f(inputs["w_ada"])
    w_adaP = np.ascontiguousarray(w_ada.reshape(DEPTH, 8, 128, 8, 384).transpose(0, 3, 2, 1, 4))
    shared = {
        "w_adaP": w_adaP, "b_adaT": _fm(f(inputs["b_ada"]), 24), "g_preT": _fm(f(inputs["g_pre"]), 8),
        "g_postT": _fm(f(inputs["g_post"]), 8), "wpack": pack_weights(inputs), "conv_wT": conv_wT,
        "conv_bT": _fm(f(inputs["conv_b"]), 4), "ln_gT": _fm(f(inputs["conv_ln_g"]), 4), "ln_bT": _fm(f(inputs["conv_ln_b"]), 4),
        "q_norm_gT": _fm(f(inputs["q_norm_g"]), 2), "kv_norm_gT": _fm(f(inputs["kv_norm_g"]), 1),
        "rel_bias": rel_bias, "rel_biasT": np.ascontiguousarray(rel_bias.T), "oh": oh,
    }
    maps = []
    for i in range(ncores):
        xb = x[i * nseq:(i + 1) * nseq]
        cb = c[i * nseq:(i + 1) * nseq]
        cTb = np.ascontiguousarray(cb.reshape(nseq, 8, 128).transpose(2, 1, 0))
        m = dict(shared)
        m["x"] = np.ascontiguousarray(xb)
        m["cT"] = cTb
        maps.append(m)
    return maps


def kernel(**inputs):
    nc = build_program(layers=(0, 1), nseq=SEQ_PER_CORE)
    in_maps = make_in_maps(inputs)
    res = run_bass_kernel_spmd(nc, in_maps, core_ids=list(range(NCORES)))
    outs = [np.asarray(r["out"], dtype=np.float32) for r in res.results]
    return np.concatenate(outs, axis=0)
```

```python
import math
from contextlib import ExitStack

import numpy as np
import concourse.bass as bass
import concourse.mybir as mybir
from concourse.bass_utils import run_bass_kernel_spmd

F32 = mybir.dt.float32
BF16 = mybir.dt.bfloat16
AF = mybir.ActivationFunctionType
ALU = mybir.AluOpType
AX = mybir.AxisListType

D_MODEL = 1024
SEQ = 2048
NT = SEQ // 128
DEPTH = 2
D_CONV = 512
CONV_WIDTH = 31
N_HEADS = 8
KV_LORA = 128
Q_LORA = 256
D_IDX = 64
TOPK = 256
N_BUCKETS = 32
MAX_DISTANCE = 128
EPS = 1e-6
D_IN_PROJ = 2504
NB = 14
NCORES = 8
SEQ_PER_CORE = 2
NG = 383


class Src:
    def __init__(self, name, sem, inc):
        self.name, self.sem, self.inc, self.count = name, sem, inc, 0


class Buf:
    __slots__ = ("name", "last_w", "readers")

    def __init__(self, name=""):
        self.name, self.last_w, self.readers = name, None, []


class _Rec:
    def __init__(self):
        self.call = None

    def __getattr__(self, name):
        def f(*a, **k):
            self.call = (name, a, k)
            return self
        return f


class Sched:
    ENG = ("pe", "act", "dve", "pool", "sp")

    def __init__(self, nc, stack, ndma=32):
        self.nc = nc
        self.src = {}
        self.prog = {e: [] for e in self.ENG}
        self.waited = {e: {} for e in self.ENG}
        for e in self.ENG:
            if e == "sp":
                continue
            self.src[e] = Src(e, stack.enter_context(nc.semaphore("sem_" + e)), 1)
        self.ring = [Src("dma%d" % i, stack.enter_context(nc.semaphore("semd%d" % i)), 16) for i in range(ndma)]
        self.ring_pos = 0
        self.nins = 0

    def _deps(self, eng, reads, writes):
        need = {}

        def add(tok):
            if tok is None:
                return
            s, c = tok
            if need.get(s, 0) < c:
                need[s] = c

        for b in reads:
            add(b.last_w)
        for b in writes:
            add(b.last_w)
            for r in b.readers:
                add(r)
        w = self.waited[eng]
        for s, c in need.items():
            if s.name == eng and eng == "pe":
                continue
            if w.get(s, 0) >= c:
                continue
            w[s] = c
            self.prog[eng].append(("w", s, c))

    def op(self, eng, fn, reads=(), writes=()):
        self._deps(eng, reads, writes)
        if eng == "sp":
            s = self.ring[self.ring_pos]
            self.ring_pos = (self.ring_pos + 1) % len(self.ring)
            if s.count and self.waited["sp"].get(s, 0) < s.count:
                self.waited["sp"][s] = s.count
                self.prog["sp"].append(("w", s, s.count))
        else:
            s = self.src[eng]
        s.count += s.inc
        rec = _Rec()
        fn(rec)
        assert rec.call is not None
        self.prog[eng].append(("i", rec.call, s, s.count))
        tok = (s, s.count)
        for b in writes:
            b.last_w = tok
            b.readers = []
        for b in reads:
            if len(b.readers) > 64:
                best = {}
                for (ss, cc) in b.readers:
                    if best.get(ss, 0) < cc:
                        best[ss] = cc
                b.readers = list(best.items())
            b.readers.append(tok)
        self.nins += 1
        return tok

    def finish(self):
        for s in self.ring:
            if s.count:
                self.prog["sp"].append(("w", s, s.count))

    def emit(self):
        nc = self.nc
        engmap = {"pe": "tensor", "act": "scalar", "dve": "vector", "pool": "gpsimd", "sp": "sync"}
        miles = {}
        for e in self.ENG:
            for it in self.prog[e]:
                if it[0] == "w" and it[1].inc == 1:
                    miles.setdefault(it[1], set()).add(it[2])
        rank = {}
        for src, st in miles.items():
            for r, idx in enumerate(sorted(st)):
                rank[(src, idx)] = r + 1
        self.n_inc = sum(len(v) for v in miles.values())
        with nc.Block() as block:
            for e in self.ENG:
                prog = self.prog[e]

                def body(engine, prog=prog):
                    for it in prog:
                        src = it[1] if it[0] == "w" else it[2]
                        if it[0] == "w":
                            if src.inc == 1:
                                engine.wait_ge(src.sem, rank[(src, it[2])])
                            else:
                                engine.wait_ge(src.sem, it[2])
                        else:
                            name, a, k = it[1]
                            ins = getattr(engine, name)(*a, **k)
                            if src.inc != 1:
                                ins.then_inc(src.sem, src.inc)
                            elif (src, it[3]) in rank:
                                ins.then_inc(src.sem, 1)

                getattr(block, engmap[e])(body)


def t5_bucket_np(n):
    max_exact = N_BUCKETS // 2
    n = np.maximum(n, 0)
    nf = np.maximum(n, 1).astype(np.float32)
    large = max_exact + (np.log(nf / np.float32(max_exact)) / np.float32(math.log(MAX_DISTANCE / max_exact))
                         * np.float32(N_BUCKETS - max_exact)).astype(np.int32)
    large = np.minimum(large, N_BUCKETS - 1)
    return np.where(n < max_exact, n, large)


def build_program(layers=(0, 1), nseq=SEQ_PER_CORE, debug=None):
    nc = bass.Bass("TRN2", target_bir_lowering=False)
    L = DEPTH
    dram = {}

    def din(name, shape):
        dram[name] = nc.dram_tensor(name, list(shape), F32, kind="ExternalInput").ap()
        return dram[name]

    x_d = din("x", [nseq, SEQ, D_MODEL])
    cT_d = din("cT", [128, 8, nseq])
    wada_d = din("w_adaP", [L, 8, 128, 8, 384])
    NJOB = 17
    wpack_d = din("wpack", [L, NJOB, 128, 2048])
    badaT_d = din("b_adaT", [128, L, 24])
    gpreT_d = din("g_preT", [128, L, 8])
    gpostT_d = din("g_postT", [128, L, 8])
    convw_d = din("conv_wT", [128, L, 4, CONV_WIDTH])
    convb_d = din("conv_bT", [128, L, 4])
    lng_d = din("ln_gT", [128, L, 4])
    lnb_d = din("ln_bT", [128, L, 4])
    qng_d = din("q_norm_gT", [128, L, 2])
    kvng_d = din("kv_norm_gT", [128, L, 1])
    relb_d = din("rel_bias", [N_BUCKETS, N_HEADS])
    relbT_d = din("rel_biasT", [N_HEADS, N_BUCKETS])
    oh_d = din("oh", [N_BUCKETS, NG])
    out_d = nc.dram_tensor("out", [nseq, SEQ, D_MODEL], F32, kind="ExternalOutput").ap()
    scr_d = nc.dram_tensor("scr_g", [N_HEADS, NG], F32, kind="Internal").ap()
    dbg_d = {}
    if debug:
        for name, shape in debug.items():
            dbg_d[name] = nc.dram_tensor("dbg_" + name, list(shape), F32, kind="ExternalOutput").ap()

    with ExitStack() as st:
        S = Sched(nc, st)
        off = [16640]

        def sb(name, shape, dt, at=None):
            nbytes = int(np.prod(shape[1:])) * (4 if dt == F32 else 2)
            nbytes = (nbytes + 31) // 32 * 32
            if at is None:
                at = off[0]
                off[0] += nbytes
            t = nc.alloc_sbuf_tensor_at(name, list(shape), dt, offset=at)
            return t

        psum = nc.alloc_psum_tensor("psum", [128, 8, 512], F32)
        pbuf = [Buf("ps%d" % i) for i in range(8)]

        def pbank(i):
            return psum[:, i, :]

        ident_f = sb("ident_f", [128, 128], F32); b_identf = Buf()
        ident_b = sb("ident_b", [128, 128], BF16); b_identb = Buf()
        ones_f = sb("ones_f", [128, 128], F32); b_onesf = Buf()
        ones_b = sb("ones_b", [128, 128], BF16); b_onesb = Buf()
        jf = sb("jf", [128, 128], F32); b_jf = Buf()
        causal = sb("causal", [128, 128], F32); b_causal = Buf()
        pow2 = sb("pow2", [128, NB + 2], F32); b_pow2 = Buf()
        negbig = sb("negbig", [128, 1], F32); b_negbig = Buf()
        cT = sb("cT", [128, 8, nseq], F32); b_cT = Buf()
        cact = sb("cact", [128, 8, nseq], F32); b_cact = Buf()
        modT = sb("modT", [128, L, 24, nseq], F32); b_modT = Buf()
        badaT = sb("badaT", [128, L, 24], F32); b_small = Buf()
        gpreT = sb("gpreT", [128, L, 8], F32)
        gpostT = sb("gpostT", [128, L, 8], F32)
        convw = sb("convw", [128, L, 4, CONV_WIDTH], F32)
        convb = sb("convb", [128, L, 4], F32)
        lng = sb("lng", [128, L, 4], F32)
        lnb = sb("lnb", [128, L, 4], F32)
        qng = sb("qng", [128, L, 2], F32)
        kvng = sb("kvng", [128, L, 1], F32)
        b31bc = sb("b31bc", [128, N_HEADS], F32); b_b31 = Buf()
        tbc = sb("tbc", [128, N_HEADS, 256], BF16); b_tbc = Buf()
        xs_res = sb("xres", [128, NT, D_MODEL], F32)
        b_x = [Buf("x%d" % i) for i in range(NT)]
        dummy = sb("dummy", [128, 8], F32)
        PB = off[0]
        LIMIT = 229376 - 64
        PX1 = PB
        PX2 = PX1 + 16384
        PX3 = PX2 + 16384
        PX4 = PX3 + 40704
        PX5 = PX4 + 57344

        S.op("pool", lambda e: e.memset(ident_f[:], 1.0), writes=[b_identf])
        S.op("pool", lambda e: e.affine_select(out=ident_f[:], in_=ident_f[:], pattern=[[-1, 128]], compare_op=ALU.is_equal,
                                               fill=0.0, base=0, channel_multiplier=1), reads=[b_identf], writes=[b_identf])
        S.op("pool", lambda e: e.tensor_copy(out=ident_b[:], in_=ident_f[:]), reads=[b_identf], writes=[b_identb])
        S.op("pool", lambda e: e.memset(ones_f[:], 1.0), writes=[b_onesf])
        S.op("pool", lambda e: e.memset(ones_b[:], 1.0), writes=[b_onesb])
        S.op("pool", lambda e: e.memset(jf[:], 1.0), writes=[b_jf])
        S.op("pool", lambda e: e.affine_select(out=jf[:], in_=jf[:], pattern=[[1, 128]], compare_op=ALU.is_equal,
                                               fill=0.0, base=-127, channel_multiplier=1), reads=[b_jf], writes=[b_jf])
        S.op("pool", lambda e: e.memset(causal[:], 0.0), writes=[b_causal])
        S.op("pool", lambda e: e.affine_select(out=causal[:], in_=causal[:], pattern=[[-1, 128]], compare_op=ALU.is_ge,
                                               fill=-1e30, base=0, channel_multiplier=1), reads=[b_causal], writes=[b_causal])
        for j in range(NB + 2):
            v = 2.0 ** (-min(j + 1, NB))
            S.op("pool", lambda e, j=j, v=v: e.memset(pow2[:, j:j + 1], v), writes=[b_pow2])
        S.op("pool", lambda e: e.memset(negbig[:], -1e29), writes=[b_negbig])

        for (t, d) in ((cT, cT_d), (badaT, badaT_d), (gpreT, gpreT_d), (gpostT, gpostT_d), (convw, convw_d), (convb, convb_d),
                       (lng, lng_d), (lnb, lnb_d), (qng, qng_d), (kvng, kvng_d)):
            S.op("sp", lambda e, t=t, d=d: e.dma_start(out=t[:], in_=d), writes=[b_small if t is not cT else b_cT])
        S.op("sp", lambda e: e.dma_start(out=b31bc[:], in_=bass.AP(tensor=relb_d.tensor, offset=31 * N_HEADS,
                                                                    ap=[[0, 128], [1, N_HEADS]])), writes=[b_b31])

        po = PX3
        relb_s = sb("relb_s", [128, N_HEADS], F32, at=po); po += 32
        relbT_s = sb("relbT_s", [128, N_BUCKETS], F32, at=po); po += 128
        nb31 = sb("nb31", [128, 1], F32, at=po); po += 32
        oh_s = sb("oh_s", [128, NG], F32, at=po); po += 1536
        gc_s = sb("gc_s", [128, NG], F32, at=po); po += 1536
        tbrev = sb("tbrev", [128, N_HEADS, 256], F32, at=po); po += 8192
        wada_st = [sb("wada_st%d" % i, [128, 8, 384], F32, at=po + i * 12288) for i in range(2)]
        po += 2 * 12288
        wada_bf = [sb("wada_bf%d" % i, [128, 8, 384], BF16, at=po + i * 6144) for i in range(2)]
        po += 2 * 6144
        cact_bf = sb("cact_bf", [128, 8, nseq], BF16, at=po); po += 64
        b_wadab = [Buf(), Buf()]; b_cactb = Buf()
        assert po <= LIMIT
        b_pro = Buf(); b_gc = Buf(); b_scr = Buf(); b_tbrev = Buf()
        b_wada = [Buf(), Buf()]

        S.op("sp", lambda e: e.dma_start(out=relb_s[0:N_BUCKETS, :], in_=relb_d), writes=[b_pro])
        S.op("sp", lambda e: e.dma_start(out=relbT_s[0:N_HEADS, :], in_=relbT_d), writes=[b_pro])
        S.op("sp", lambda e: e.dma_start(out=oh_s[0:N_BUCKETS, :], in_=oh_d), writes=[b_pro])
        S.op("dve", lambda e: e.tensor_scalar(out=nb31[0:N_HEADS, :], in0=relbT_s[0:N_HEADS, 31:32], scalar1=-1.0, scalar2=None,
                                              op0=ALU.mult), reads=[b_pro], writes=[b_pro])
        S.op("pe", lambda e: e.matmul(psum[0:N_HEADS, 0, 0:NG], lhsT=relb_s[0:N_BUCKETS, :], rhs=oh_s[0:N_BUCKETS, :],
                                      start=True, stop=True), reads=[b_pro], writes=[pbuf[0]])
        S.op("act", lambda e: e.activation(out=gc_s[0:N_HEADS, :], in_=psum[0:N_HEADS, 0, 0:NG], func=AF.Exp,
                                           bias=nb31[0:N_HEADS, 0:1], scale=1.0), reads=[pbuf[0], b_pro], writes=[b_gc])
        S.op("sp", lambda e: e.dma_start(out=scr_d, in_=gc_s[0:N_HEADS, :]), reads=[b_gc], writes=[b_scr])
        S.op("sp", lambda e: e.dma_start(out=tbrev[:], in_=bass.AP(tensor=scr_d.tensor, offset=0,
                                                                    ap=[[1, 128], [NG, N_HEADS], [1, 256]])),
             reads=[b_scr], writes=[b_tbrev])
        for h in range(N_HEADS):
            bk = 1 + (h % 2)
            S.op("pe", lambda e, h=h, bk=bk: e.matmul(psum[:, bk, 0:256], lhsT=jf[:], rhs=tbrev[:, h, :], start=True, stop=True),
                 reads=[b_jf, b_tbrev], writes=[pbuf[bk]])
            S.op("act", lambda e, h=h, bk=bk: e.activation(out=tbc[:, h, :], in_=psum[:, bk, 0:256], func=AF.Copy),
                 reads=[pbuf[bk]], writes=[b_tbc])

        S.op("act", lambda e: e.activation(out=cact[:], in_=cT[:], func=AF.Silu), reads=[b_cT], writes=[b_cact])
        S.op("act", lambda e: e.activation(out=cact_bf[:], in_=cT[:], func=AF.Silu), reads=[b_cT], writes=[b_cactb])
        gi = 0
        for l in layers:
            for g in range(8):
                slot = gi % 2
                gi += 1
                S.op("sp", lambda e, slot=slot, g=g, l=l: e.dma_start(out=wada_st[slot][:], in_=wada_d[l, g]),
                     writes=[b_wada[slot]])
                S.op("dve" if g % 2 == 0 else "pool", lambda e, slot=slot: e.tensor_copy(out=wada_bf[slot][:], in_=wada_st[slot][:]),
                     reads=[b_wada[slot]], writes=[b_wadab[slot]])
                for jj in range(3):
                    j = g * 3 + jj
                    for k in range(8):
                        S.op("pe", lambda e, slot=slot, jj=jj, j=j, k=k: e.matmul(
                            psum[:, 3, j * nseq:(j + 1) * nseq], lhsT=wada_bf[slot][:, k, jj * 128:(jj + 1) * 128],
                            rhs=cact_bf[:, k, :], start=(k == 0), stop=(k == 7)),
                            reads=[b_wadab[slot], b_cactb], writes=[pbuf[3]])
            S.op("dve", lambda e, l=l: e.tensor_tensor(
                out=modT[:, l, :, :], in0=psum[:, 3, 0:24 * nseq].rearrange("p (j b) -> p j b", b=nseq),
                in1=badaT[:, l, :].unsqueeze(2).to_broadcast([128, 24, nseq]), op=ALU.add),
                reads=[pbuf[3], b_small], writes=[b_modT])

        yconvT = sb("yconvT", [128, 4, SEQ], BF16, at=PX1)
        sgT = sb("sgT", [128, 4, SEQ], BF16, at=PX2)
        yc = sb("yc", [128, 4, SEQ], BF16, at=PX2)
        o = PX4
        hT = sb("hT", [128, 8, SEQ], BF16, at=o); o += 32768
        wst = [sb("wst%d" % i, [128, 2048], F32, at=o + i * 8192) for i in range(2)]; o += 16384
        wbf = [sb("wbf%d" % i, [128, 2048], BF16, at=o + i * 4096) for i in range(2)]; o += 8192
        assert o == PX5
        b_hT = [Buf("hT%d" % i) for i in range(4)]
        b_yconv = [Buf() for _ in range(4)]
        b_sg = [Buf() for _ in range(8)]
        b_yc = [Buf() for _ in range(4)]
        b_wst = [Buf(), Buf()]
        b_wbf = [Buf(), Buf()]

        o = PX3
        abc = sb("abc", [128, D_MODEL], F32, at=o); o += 4096
        shbc = sb("shbc", [128, D_MODEL], F32, at=o); o += 4096
        xs = [sb("xs%d" % i, [128, D_MODEL], F32, at=o + i * 4096) for i in range(2)]; o += 8192
        dg = [sb("dg%d" % i, [128, 128], F32, at=o + i * 512) for i in range(2)]; o += 1024
        vecT = sb("vecT", [128, 3, 8], F32, at=o); o += 96
        ssq = sb("ssq", [128, NT], F32, at=o); o += 64
        rs16 = sb("rs16", [128, NT], F32, at=o); o += 64
        rstd16 = sb("rstd16", [128, NT], F32, at=o); o += 64
        assert o <= PX4
        b_abc = Buf(); b_shbc = Buf(); b_xs = [Buf(), Buf()]; b_dg = [Buf(), Buf()]; b_vecT = Buf()
        b_ssq = Buf(); b_rs16 = Buf(); b_rstd16 = Buf()

        o = PX3
        apad = [sb("apad%d" % i, [128, 30 + SEQ], BF16, at=o + i * 4160) for i in range(2)]; o += 8320
        sig = [sb("sig%d" % i, [128, 512], BF16, at=o + i * 1024) for i in range(2)]; o += 2048
        cdiag = sb("cdiag", [128, CONV_WIDTH, 128], BF16, at=o); o += 7936
        ycsq = [sb("ycsq0", [128, 512], BF16, at=o), sb("ycsq1", [128, 512], BF16, at=PX3 + 16384)]; o += 1024
        zt1 = sb("zt1", [128, 4, 512], BF16, at=o); o += 4096
        m2b = sb("m2b", [128, 512], F32, at=o); o += 2048
        varb = sb("varb", [128, 512], F32, at=o); o += 2048
        dtmp = [sb("dtmp0", [128, 512], F32, at=o)] * 2; o += 2048
        zt0 = sb("zt0", [128, 4, 512], BF16, at=o); o += 4096
        ztb = [zt0, zt1]
        sgc = [sb("sgc%d" % i, [128, 512], BF16, at=o + i * 1024) for i in range(2)]; o += 2048
        wpw2b = sb("wpw2b", [128, 4, 512], BF16, at=o); o += 4096
        assert o <= PX4, o
        meanb4 = [sb("meanb4_%d" % i, [128, 512], F32, at=PX3 + i * 2048) for i in range(4)]
        rstdb4 = [sb("rstdb4_%d" % i, [128, 512], F32, at=PX3 + 8192 + i * 2048) for i in range(4)]
        assert 16384 + 1024 <= 8320 + 2048 + 7936
        b_apad = [Buf(), Buf()]; b_sig = [Buf(), Buf()]; b_cdiag = Buf(); b_ycsq = [Buf(), Buf()]
        b_mean4 = [Buf() for _ in range(4)]; b_m2b = Buf(); b_varb = Buf(); b_rstd4 = [Buf() for _ in range(4)]; b_dtmp = [Buf()] * 2
        b_ztb = [[Buf() for _ in range(4)] for _ in range(2)]; b_sgc = [Buf(), Buf()]; b_wpw2b = Buf()

        o = PX3
        cqnT = sb("cqnT", [128, 2, SEQ], BF16, at=o); o += 8192
        kvnT = sb("kvnT", [128, SEQ], BF16, at=o); o += 4096
        kidx2 = sb("kidx2", [128, SEQ], BF16, at=o); o += 4096
        vaug = sb("vaug", [128, NT, N_HEADS, 65], BF16, at=o); o += 16640
        wtok = sb("wtok", [128, NT, N_HEADS], F32, at=o); o += 512
        wuqb = sb("wuqb", [128, 2, 1024], BF16, at=o); o += 4096
        wqib = sb("wqib", [128, 2, 512], BF16, at=o); o += 2048
        wuvb = sb("wuvb", [128, 512], BF16, at=o); o += 1024
        assert o <= PX4, o
        o = PX5
        sqc = [sb("sqc0", [128, 512], BF16, at=o)] * 2; o += 1024
        rstdc = [sb("rstdc0", [128, 512], F32, at=o)] * 2; o += 2048
        kdw = sb("kdw", [128, 8, 128], BF16, at=o); o += 2048
        assert o <= LIMIT, o
        b_cqn = [Buf() for _ in range(8)]
        b_kvn = Buf(); b_kidx = Buf(); b_vaug = Buf(); b_wtok = Buf()
        b_wuqb = Buf(); b_wqib = Buf(); b_wuvb = Buf()
        b_sqc = [Buf()] * 2; b_rstdc = [Buf()] * 2; b_kdw = Buf()

        o = PX4
        qblk = [sb("qblk%d" % i, [128, N_HEADS, 256], BF16, at=o + i * 4096) for i in range(2)]; o += 8192
        qiblk = [sb("qiblk0", [128, 4, 256], BF16, at=o)] * 2; o += 2048
        score = [sb("score%d" % i, [128, SEQ], F32, at=o + i * 8192) for i in range(2)]; o += 16384
        maskts = [sb("maskts%d" % i, [128, SEQ], BF16, at=o + i * 4096) for i in range(2)]; o += 8192
        maskT = sb("maskT", [128, NT, 256], BF16, at=o); o += 8192
        rt = [sb("rt%d" % i, [128, 512], BF16, at=o + i * 1024) for i in range(4)]; o += 4096
        et = [sb("et%d" % i, [128, 256], BF16, at=o + i * 512) for i in range(4)]; o += 2048
        pm = [sb("pm%d" % i, [128, 256], BF16, at=o + i * 512) for i in range(4)]; o += 2048
        dgw = [sb("dgw%d" % i, [128, N_HEADS, 128], BF16, at=o + i * 2048) for i in range(2)]; o += 4096
        ytok = sb("ytok", [128, 2, 512], F32, at=o); o += 4096
        bis = sb("bis", [128, 8], F32, at=o); o += 32
        steps = sb("steps", [128, NB + 2], F32, at=o); o += 96
        cands = sb("cands", [128, NB + 2], F32, at=o); o += 96
        cnts = sb("cnts", [128, NB + 2], F32, at=o); o += 96
        sels = sb("sels", [128, NB + 2], F32, at=o); o += 96
        rcp = sb("rcp", [128, 2, N_HEADS], F32, at=o); o += 64
        osb = sb("osb", [128, 2, N_HEADS, 65], F32, at=o); o += 4160
        assert o <= LIMIT, (o, LIMIT)
        b_osb = Buf()
        b_qblk = [Buf(), Buf()]; b_qiblk = [Buf()] * 2; b_score = [Buf(), Buf()]; b_maskts = [Buf(), Buf()]
        b_maskT = [Buf(), Buf()]
        b_rt = [Buf() for _ in range(4)]; b_et = [Buf() for _ in range(4)]; b_pm = [Buf() for _ in range(4)]
        b_dgw = [Buf(), Buf()]; b_ytok = [Buf(), Buf()]; b_bis = Buf(); b_steps = Buf()
        b_cands = [Buf() for _ in range(NB + 2)]; b_cnts = [Buf() for _ in range(NB + 2)]; b_sels = [Buf() for _ in range(NB + 2)]
        b_rcp = Buf()

        o = PX3
        woutb = sb("woutb", [128, 8, 1024], BF16, at=o); o += 16384
        gbc = sb("gbc", [128, D_MODEL], F32, at=o); o += 4096
        etmp = [sb("etmp%d" % i, [128, 512], F32, at=o + i * 2048) for i in range(2)]; o += 4096
        ejunk = sb("ejunk", [128, 512], F32, at=o); o += 2048
        ess = sb("ess", [128, 4], F32, at=o); o += 32
        dgE = [sb("dgE%d" % i, [128, 128], F32, at=o + i * 512) for i in range(2)]; o += 1024
        gvT = sb("gvT", [128, 8], F32, at=o); o += 32
        assert o <= PX4, o
        b_woutb = [Buf() for _ in range(4)]; b_gbc = Buf(); b_etmp = [Buf(), Buf()]; b_ejunk = Buf(); b_ess = Buf()
        b_dgE = [Buf(), Buf()]; b_gvT = Buf()

        b_region = Buf("region")

        def phase_barrier(extra=()):
            S.op("pool", lambda e: e.memset(dummy[:, 0:1], 0.0), writes=[b_region] + list(extra))

        RG = [b_region]

        wcnt = [0]
        wjob = [0, 0]

        def load_w(parts, dst_writes, casts):
            slot = wcnt[0] % 2
            wcnt[0] += 1
            stg = wst[slot]
            jn = wjob[1]
            wjob[1] += 1
            assert jn < NJOB
            S.op("sp", lambda e, stg=stg, jn=jn: e.dma_start(out=stg[:], in_=wpack_d[wjob[0], jn]), reads=RG, writes=[b_wst[slot]])
            for (oap, ifn, obufs) in casts:
                S.op("dve", lambda e, oap=oap, ifn=ifn, stg=stg: e.tensor_copy(out=oap, in_=ifn(stg)),
                     reads=[b_wst[slot]] + RG, writes=obufs)

        def win_cols(l, c0, c1):
            return None

        def stage3(stg, n):
            return stg[:, 0:8 * n].rearrange("p (k n) -> p k n", k=8)

        def load_x_quarter(b, q4):
            S.op("sp", lambda e: e.dma_start(out=xs_res[:, q4 * 4:(q4 + 1) * 4, :],
                                             in_=x_d[b, q4 * 512:(q4 + 1) * 512, :].rearrange("(n p) d -> p n d", p=128)),
                 writes=[b_x[q4 * 4 + i] for i in range(4)])

        def bcast_rows(vec_col_fn, dst, dst_buf, dgs, b_dgs, extra_reads):
            for half in range(2):
                bk = 4 + half
                for kk in range(4):
                    k = half * 4 + kk
                    sl = k % 2
                    S.op("dve", lambda e, k=k, sl=sl: e.tensor_scalar(out=dgs[sl][:], in0=ident_f[:], scalar1=vec_col_fn(k),
                                                                      scalar2=None, op0=ALU.mult),
                         reads=[b_identf] + extra_reads + RG, writes=[b_dgs[sl]])
                    S.op("pe", lambda e, kk=kk, sl=sl, bk=bk: e.matmul(psum[:, bk, kk * 128:(kk + 1) * 128], lhsT=ones_f[:],
                                                                      rhs=dgs[sl][:], start=True, stop=True),
                         reads=[b_onesf, b_dgs[sl]], writes=[pbuf[bk]])
                S.op("act", lambda e, half=half, bk=bk: e.activation(out=dst[:, half * 512:(half + 1) * 512], in_=psum[:, bk, :],
                                                                     func=AF.Copy), reads=[pbuf[bk]] + RG, writes=[dst_buf])

        def block(b, l, last_layer):
            wjob[0] = l
            wjob[1] = 0
            phase_barrier()
            S.op("dve", lambda e: e.scalar_tensor_tensor(out=vecT[:, 0, :], in0=modT[:, l, 8:16, b], scalar=1.0, in1=gpreT[:, l, :],
                                                         op0=ALU.add, op1=ALU.mult), reads=[b_modT, b_small] + RG, writes=[b_vecT])
            S.op("dve", lambda e: e.tensor_copy(out=vecT[:, 1, :], in_=modT[:, l, 0:8, b]), reads=[b_modT] + RG, writes=[b_vecT])
            bcast_rows(lambda k: vecT[:, 0, k:k + 1], abc, b_abc, dg, b_dg, [b_vecT])
            bcast_rows(lambda k: vecT[:, 1, k:k + 1], shbc, b_shbc, dg, b_dg, [b_vecT])
            for tt in range(NT):
                S.op("act", lambda e, tt=tt: e.activation(out=xs[tt % 2][:], in_=xs_res[:, tt, :], func=AF.Square,
                                                          accum_out=ssq[:, tt:tt + 1]),
                     reads=[b_x[tt]] + RG, writes=[b_xs[tt % 2], b_ssq])
            S.op("act", lambda e: e.activation(out=rs16[:], in_=ssq[:], func=AF.Sqrt, bias=EPS, scale=1.0 / D_MODEL),
                 reads=[b_ssq] + RG, writes=[b_rs16])
            S.op("dve", lambda e: e.reciprocal(out=rstd16[:], in_=rs16[:]), reads=[b_rs16] + RG, writes=[b_rstd16])
            for tt in range(NT):
                sl = tt % 2
                S.op("dve", lambda e, tt=tt, sl=sl: e.scalar_tensor_tensor(out=xs[sl][:], in0=xs_res[:, tt, :], scalar=rstd16[:, tt:tt + 1],
                                                                           in1=abc[:], op0=ALU.mult, op1=ALU.mult),
                     reads=[b_x[tt], b_rstd16, b_abc] + RG, writes=[b_xs[sl]])
                S.op("dve", lambda e, sl=sl: e.tensor_tensor(out=xs[sl][:], in0=xs[sl][:], in1=shbc[:], op=ALU.add),
                     reads=[b_xs[sl], b_shbc] + RG, writes=[b_xs[sl]])
                for half in range(2):
                    bk = (tt * 2 + half) % 4
                    for kk in range(4):
                        k = half * 4 + kk
                        S.op("pe", lambda e, sl=sl, k=k, kk=kk, bk=bk: e.transpose(out=psum[:, bk, kk * 128:(kk + 1) * 128],
                                                                                  in_=xs[sl][:, k * 128:(k + 1) * 128], identity=ident_f[:]),
                             reads=[b_xs[sl], b_identf], writes=[pbuf[bk]])
                    S.op("act", lambda e, tt=tt, half=half, bk=bk: e.activation(
                        out=hT[:, half * 4:(half + 1) * 4, tt * 128:(tt + 1) * 128],
                        in_=psum[:, bk, :].rearrange("p (k t) -> p k t", k=4), func=AF.Copy),
                        reads=[pbuf[bk]] + RG, writes=[b_hT[tt // 4]])

            phase_barrier()

            def inproj(wtile_fn, bk, tb, n=512, k_list=range(8)):
                for k in k_list:
                    S.op("pe", lambda e, k=k: e.matmul(psum[:, bk, 0:n], lhsT=wtile_fn(k), rhs=hT[:, k, tb * 512:tb * 512 + n],
                                                       start=(k == 0), stop=(k == 7)),
                         reads=[b_hT[tb]] + wt_reads[0], writes=[pbuf[bk]])

            wt_reads = [[]]
            load_w([(lambda stg: stg[:].rearrange("p (k n) -> p k n", k=4), None)], None,
                   [(wpw2b[:], lambda stg: stg[:].rearrange("p (k n) -> p k n", k=4), [b_wpw2b])])
            for c in range(4):
                slot = wcnt[0] % 2
                wb = wbf[slot]
                wb3 = wb[:].rearrange("p (k n) -> p k n", k=8)
                load_w([(lambda stg: stage3(stg, 256)[:, :, 0:128], win_cols(l, c * 128, (c + 1) * 128)),
                        (lambda stg: stage3(stg, 256)[:, :, 128:256], win_cols(l, 512 + c * 128, 512 + (c + 1) * 128))], None,
                       [(wb3, lambda stg: stage3(stg, 256), [b_wbf[slot]])])
                ap_ = apad[c % 2]
                bap = b_apad[c % 2]
                S.op("pool", lambda e, ap_=ap_: e.memset(ap_[:, 0:30], 0.0), reads=RG, writes=[bap])
                for tb in range(4):
                    wt_reads[0] = [b_wbf[slot]]
                    inproj(lambda k: wb3[:, k, 128:256], 0, tb)
                    inproj(lambda k: wb3[:, k, 0:128], 1, tb)
                    sl = tb % 2
                    S.op("act", lambda e, sl=sl: e.activation(out=sig[sl][:], in_=psum[:, 0, :], func=AF.Sigmoid),
                         reads=[pbuf[0]] + RG, writes=[b_sig[sl]])
                    S.op("dve", lambda e, sl=sl, tb=tb, ap_=ap_: e.tensor_tensor(out=ap_[:, 30 + tb * 512:30 + (tb + 1) * 512], in0=psum[:, 1, :],
                                                                                in1=sig[sl][:], op=ALU.mult),
                         reads=[pbuf[1], b_sig[sl]] + RG, writes=[bap])
                    for k in range(tb * 8, min(CONV_WIDTH, tb * 8 + 8)):
                        S.op("dve", lambda e, k=k, c=c: e.tensor_scalar(out=cdiag[:, k, :], in0=ident_b[:], scalar1=convw[:, l, c, k:k + 1],
                                                                        scalar2=None, op0=ALU.mult),
                             reads=[b_identb, b_small] + RG, writes=[b_cdiag])
                for tb in range(4):
                    bk = 2 + (tb % 2)
                    for k in range(CONV_WIDTH):
                        S.op("pe", lambda e, k=k, tb=tb, bk=bk, ap_=ap_: e.matmul(psum[:, bk, :], lhsT=cdiag[:, k, :],
                                                                                 rhs=ap_[:, k + tb * 512:k + tb * 512 + 512],
                                                                                 start=(k == 0), stop=(k == CONV_WIDTH - 1)),
                             reads=[b_cdiag, bap], writes=[pbuf[bk]])
                    S.op("act", lambda e, tb=tb, bk=bk, c=c: e.activation(out=yc[:, c, tb * 512:(tb + 1) * 512], in_=psum[:, bk, :],
                                                                          func=AF.Identity, bias=convb[:, l, c:c + 1], scale=1.0),
                         reads=[pbuf[bk], b_small] + RG, writes=[b_yc[c]])
            gslot = []
            for g in range(2):
                sl_ = wcnt[0] % 2
                gslot.append(sl_)
                load_w([(lambda stg: stage3(stg, 256), win_cols(l, 1024 + g * 256, 1024 + (g + 1) * 256))], None,
                       [(wbf[sl_][:].rearrange("p (k n) -> p k n", k=8), lambda stg: stage3(stg, 256), [b_wbf[sl_]])])
            alias_w = [b_apad[0], b_apad[1], b_sig[0], b_sig[1], b_cdiag]
            for tb in range(4):
                tsl = slice(tb * 512, (tb + 1) * 512)
                b4, b5 = 4 + 2 * (tb % 2), 5 + 2 * (tb % 2)
                for c in range(4):
                    S.op("pe", lambda e, c=c: e.matmul(psum[:, b4, :], lhsT=ones_b[:], rhs=yc[:, c, tsl], start=(c == 0), stop=(c == 3)),
                         reads=[b_onesb, b_yc[c]], writes=[pbuf[b4]])
                for c in range(4):
                    sl = c % 2
                    S.op("act", lambda e, c=c, sl=sl: e.activation(out=ycsq[sl][:], in_=yc[:, c, tsl], func=AF.Square),
                         reads=[b_yc[c]] + RG, writes=[b_ycsq[sl]] + (alias_w if sl == 1 else []))
                    S.op("pe", lambda e, c=c, sl=sl: e.matmul(psum[:, b5, :], lhsT=ones_b[:], rhs=ycsq[sl][:], start=(c == 0), stop=(c == 3)),
                         reads=[b_onesb, b_ycsq[sl]], writes=[pbuf[b5]])
                S.op("act", lambda e: e.activation(out=meanb4[tb][:], in_=psum[:, b4, :], func=AF.Copy, scale=1.0 / D_CONV),
                     reads=[pbuf[b4]] + RG, writes=[b_mean4[tb]] + alias_w)
                S.op("dve", lambda e: e.tensor_tensor(out=m2b[:], in0=meanb4[tb][:], in1=meanb4[tb][:], op=ALU.mult),
                     reads=[b_mean4[tb]] + RG, writes=[b_m2b])
                S.op("dve", lambda e: e.scalar_tensor_tensor(out=varb[:], in0=psum[:, b5, :], scalar=1.0 / D_CONV, in1=m2b[:],
                                                             op0=ALU.mult, op1=ALU.subtract),
                     reads=[pbuf[b5], b_m2b] + RG, writes=[b_varb])
                S.op("dve", lambda e: e.tensor_scalar(out=varb[:], in0=varb[:], scalar1=0.0, scalar2=None, op0=ALU.max),
                     reads=[b_varb] + RG, writes=[b_varb])
                S.op("act", lambda e: e.activation(out=rstdb4[tb][:], in_=varb[:], func=AF.Sqrt, bias=EPS, scale=1.0),
                     reads=[b_varb] + RG, writes=[b_rstd4[tb]] + alias_w)
                S.op("dve", lambda e: e.reciprocal(out=rstdb4[tb][:], in_=rstdb4[tb][:]), reads=[b_rstd4[tb]] + RG, writes=[b_rstd4[tb]])
            def ln_apply(tb, c):
                tsl = slice(tb * 512, (tb + 1) * 512)
                zt = ztb[tb % 2]; b_zt = b_ztb[tb % 2]
                sl = c % 2
                S.op("dve", lambda e: e.tensor_tensor(out=dtmp[sl][:], in0=yc[:, c, tsl], in1=meanb4[tb][:], op=ALU.subtract),
                     reads=[b_yc[c], b_mean4[tb]] + RG, writes=[b_dtmp[sl]])
                S.op("dve", lambda e: e.tensor_tensor(out=dtmp[sl][:], in0=dtmp[sl][:], in1=rstdb4[tb][:], op=ALU.mult),
                     reads=[b_dtmp[sl], b_rstd4[tb]] + RG, writes=[b_dtmp[sl]])
                S.op("act", lambda e: e.activation(out=zt[:, c, :], in_=dtmp[sl][:], func=AF.Silu,
                                                   bias=lnb[:, l, c:c + 1], scale=lng[:, l, c:c + 1]),
                     reads=[b_dtmp[sl], b_small] + RG, writes=[b_zt[c]])

            for c in range(4):
                ln_apply(0, c)
            for tb in range(4):
                tsl = slice(tb * 512, (tb + 1) * 512)
                zt = ztb[tb % 2]; b_zt = b_ztb[tb % 2]
                for c2 in range(4):
                    bk = 0 + (c2 % 2)
                    bg = 2 + (c2 % 2)
                    for c in range(4):
                        S.op("pe", lambda e, c=c, c2=c2, bk=bk: e.matmul(psum[:, bk, :], lhsT=wpw2b[:, c, c2 * 128:(c2 + 1) * 128],
                                                                        rhs=zt[:, c, :], start=(c == 0), stop=(c == 3)),
                             reads=[b_wpw2b, b_zt[c]], writes=[pbuf[bk]])
                    gs_ = gslot[c2 // 2]
                    wt_reads[0] = [b_wbf[gs_]]
                    wg3 = wbf[gs_][:].rearrange("p (k n) -> p k n", k=8)
                    inproj(lambda k: wg3[:, k, (c2 % 2) * 128:(c2 % 2 + 1) * 128], bg, tb)
                    if tb + 1 < 4:
                        ln_apply(tb + 1, c2)
                    sl = c2 % 2
                    S.op("act", lambda e, sl=sl, bg=bg: e.activation(out=sgc[sl][:], in_=psum[:, bg, :], func=AF.Silu),
                         reads=[pbuf[bg]] + RG, writes=[b_sgc[sl]])
                    S.op("dve", lambda e, sl=sl, c2=c2, bk=bk: e.tensor_tensor(out=yconvT[:, c2, tsl], in0=psum[:, bk, :], in1=sgc[sl][:],
                                                                              op=ALU.mult),
                         reads=[pbuf[bk], b_sgc[sl]] + RG, writes=[b_yconv[tb]])

            phase_barrier()
            attn_scale = (8 ** -0.5) * (D_IDX ** -0.5)
            S.op("pool", lambda e: e.memset(vaug[:, :, :, 64:65], 1.0), reads=RG, writes=[b_vaug])
            load_w([(lambda stg: stg[:].rearrange("p (k n) -> p k n", k=2), None)], None,
                   [(wuqb[:], lambda stg: stg[:].rearrange("p (k n) -> p k n", k=2), [b_wuqb])])
            load_w([(lambda stg: stg[:, 0:1024].rearrange("p (k n) -> p k n", k=2), None),
                    (lambda stg: stg[:, 1024:1536], None)], None,
                   [(wqib[:], lambda stg: stg[:, 0:1024].rearrange("p (k n) -> p k n", k=2), [b_wqib]),
                    (wuvb[:], lambda stg: stg[:, 1024:1536], [b_wuvb])])
            s1 = wcnt[0] % 2
            w1 = wbf[s1][:].rearrange("p (k n) -> p k n", k=8)
            load_w([(lambda stg: stage3(stg, 256), win_cols(l, 1536, 1792))], None, [(w1, lambda stg: stage3(stg, 256), [b_wbf[s1]])])
            s2 = wcnt[0] % 2
            w2 = wbf[s2][:].rearrange("p (k n) -> p k n", k=8)
            load_w([(lambda stg: stage3(stg, 256)[:, :, 0:200], win_cols(l, 1792, 1992))], None,
                   [(w2[:, :, 0:200], lambda stg: stage3(stg, 256)[:, :, 0:200], [b_wbf[s2]]),
                    (kdw[:, :, 0:64], lambda stg: stage3(stg, 256)[:, :, 128:192], [b_kdw]),
                    (kdw[:, :, 64:128], lambda stg: stage3(stg, 256)[:, :, 128:192], [b_kdw])])
            for tb in range(4):
                tsl = slice(tb * 512, (tb + 1) * 512)
                wt_reads[0] = [b_wbf[s1]]
                inproj(lambda k: w1[:, k, 0:128], 0, tb)
                inproj(lambda k: w1[:, k, 128:256], 1, tb)
                for ch in range(2):
                    S.op("act", lambda e, ch=ch: e.activation(out=sqc[ch][:], in_=psum[:, ch, :], func=AF.Square),
                         reads=[pbuf[ch]] + RG, writes=[b_sqc[ch]])
                    S.op("pe", lambda e, ch=ch: e.matmul(psum[:, 2, :], lhsT=ones_b[:], rhs=sqc[ch][:], start=(ch == 0), stop=(ch == 1)),
                         reads=[b_onesb, b_sqc[ch]], writes=[pbuf[2]])
                S.op("act", lambda e: e.activation(out=rstdc[0][:], in_=psum[:, 2, :], func=AF.Sqrt, bias=EPS, scale=1.0 / Q_LORA),
                     reads=[pbuf[2]] + RG, writes=[b_rstdc[0]])
                S.op("dve", lambda e: e.reciprocal(out=rstdc[0][:], in_=rstdc[0][:]), reads=[b_rstdc[0]] + RG, writes=[b_rstdc[0]])
                for ch in range(2):
                    S.op("dve", lambda e, ch=ch: e.scalar_tensor_tensor(out=cqnT[:, ch, tsl], in0=psum[:, ch, :], scalar=qng[:, l, ch:ch + 1],
                                                                        in1=rstdc[0][:], op0=ALU.mult, op1=ALU.mult),
                         reads=[pbuf[ch], b_small, b_rstdc[0]] + RG, writes=[b_cqn[2 * tb], b_cqn[2 * tb + 1]])
                wt_reads[0] = [b_wbf[s2]]
                inproj(lambda k: w2[:, k, 0:128], 3, tb)
                S.op("act", lambda e: e.activation(out=sqc[0][:], in_=psum[:, 3, :], func=AF.Square), reads=[pbuf[3]] + RG, writes=[b_sqc[0]])
                S.op("pe", lambda e: e.matmul(psum[:, 4, :], lhsT=ones_b[:], rhs=sqc[0][:], start=True, stop=True),
                     reads=[b_onesb, b_sqc[0]], writes=[pbuf[4]])
                S.op("act", lambda e: e.activation(out=rstdc[1][:], in_=psum[:, 4, :], func=AF.Sqrt, bias=EPS, scale=1.0 / KV_LORA),
                     reads=[pbuf[4]] + RG, writes=[b_rstdc[1]])
                S.op("dve", lambda e: e.reciprocal(out=rstdc[1][:], in_=rstdc[1][:]), reads=[b_rstdc[1]] + RG, writes=[b_rstdc[1]])
                S.op("dve", lambda e: e.scalar_tensor_tensor(out=kvnT[:, tsl], in0=psum[:, 3, :], scalar=kvng[:, l, 0:1], in1=rstdc[1][:],
                                                             op0=ALU.mult, op1=ALU.mult),
                     reads=[pbuf[3], b_small, b_rstdc[1]] + RG, writes=[b_kvn])
                wt_reads[0] = [b_kdw]
                inproj(lambda k: kdw[:, k, :], 5, tb)
                S.op("act", lambda e: e.activation(out=kidx2[:, tsl], in_=psum[:, 5, :], func=AF.Copy), reads=[pbuf[5]] + RG, writes=[b_kidx])
                for t4 in range(4):
                    tt = tb * 4 + t4
                    for k in range(8):
                        S.op("pe", lambda e, k=k, tt=tt, t4=t4: e.matmul(psum[:, 6, t4 * 8:(t4 + 1) * 8], lhsT=hT[:, k, tt * 128:(tt + 1) * 128],
                                                                        rhs=w2[:, k, 192:200], start=(k == 0), stop=(k == 7)),
                             reads=[b_hT[tb], b_wbf[s2]], writes=[pbuf[6]])
                S.op("act", lambda e, tb=tb: e.activation(out=wtok[:, tb * 4:(tb + 1) * 4, :],
                                                          in_=psum[:, 6, 0:32].rearrange("p (t h) -> p t h", h=8), func=AF.Copy, scale=attn_scale),
                     reads=[pbuf[6]] + RG, writes=[b_wtok])
            for g in range(2):
                sg_ = wcnt[0] % 2
                wg = wbf[sg_][:].rearrange("p (k n) -> p k n", k=8)
                load_w([(lambda stg: stage3(stg, 256), win_cols(l, 1992 + g * 256, 1992 + (g + 1) * 256))], None,
                       [(wg, lambda stg: stage3(stg, 256), [b_wbf[sg_]])])
                for cc in range(2):
                    ch = g * 2 + cc
                    for tb in range(4):
                        bk = (cc * 4 + tb) % 2
                        wt_reads[0] = [b_wbf[sg_]]
                        inproj(lambda k: wg[:, k, cc * 128:(cc + 1) * 128], bk, tb)
                        S.op("act", lambda e, ch=ch, tb=tb, bk=bk: e.activation(out=sgT[:, ch, tb * 512:(tb + 1) * 512], in_=psum[:, bk, :],
                                                                               func=AF.Silu),
                             reads=[pbuf[bk]] + RG, writes=[b_sg[2 * tb], b_sg[2 * tb + 1]])
            for stl in range(NT):
                bk = 2 + (stl % 2)
                S.op("pe", lambda e, stl=stl, bk=bk: e.matmul(psum[:, bk, :], lhsT=kvnT[:, stl * 128:(stl + 1) * 128], rhs=wuvb[:],
                                                             start=True, stop=True), reads=[b_kvn, b_wuvb], writes=[pbuf[bk]])
                S.op("act", lambda e, stl=stl, bk=bk: e.activation(out=vaug[:, stl, :, 0:64],
                                                                   in_=psum[:, bk, :].rearrange("p (h d) -> p h d", h=N_HEADS), func=AF.Copy),
                     reads=[pbuf[bk]] + RG, writes=[b_vaug])

            phase_barrier()
            sm_scale = KV_LORA ** -0.5
            cnt_r = [0]; cnt_e = [0]
            DSK = 2
            psb = psum[:, 7, :].bitcast(BF16).rearrange("p (j t) -> p j t", t=128)

            def gen_Qq(B):
                qs = B % 2
                tcols = slice(B * 256, (B + 1) * 256)
                for hp in range(4):
                    for hh in range(2):
                        h = hp * 2 + hh
                        for ch in range(2):
                            S.op("pe", lambda e, h=h, hh=hh, ch=ch: e.matmul(psum[:, 7, hh * 256:(hh + 1) * 256],
                                                                             lhsT=wuqb[:, ch, h * 128:(h + 1) * 128], rhs=cqnT[:, ch, tcols],
                                                                             start=(ch == 0), stop=(ch == 1)),
                                 reads=[b_wuqb, b_cqn[B]], writes=[pbuf[7]])
                    S.op("act", lambda e, hp=hp: e.activation(out=qblk[qs][:, hp * 2:hp * 2 + 2, :],
                                                              in_=psum[:, 7, :].rearrange("p (h t) -> p h t", h=2), func=AF.Copy),
                         reads=[pbuf[7]] + RG, writes=[b_qblk[qs]])
                    yield 1.0

            def gen_Qi(B):
                qs = B % 2
                tcols = slice(B * 256, (B + 1) * 256)
                for pp in range(2):
                    for pq in range(2):
                        pr = pp * 2 + pq
                        for ch in range(2):
                            S.op("pe", lambda e, pr=pr, pq=pq, ch=ch: e.matmul(psum[:, 7, pq * 256:(pq + 1) * 256],
                                                                               lhsT=wqib[:, ch, pr * 128:(pr + 1) * 128], rhs=cqnT[:, ch, tcols],
                                                                               start=(ch == 0), stop=(ch == 1)),
                                 reads=[b_wqib, b_cqn[B]], writes=[pbuf[7]])
                    S.op("act", lambda e, pp=pp: e.activation(out=qiblk[qs][:, pp * 2:pp * 2 + 2, :],
                                                              in_=psum[:, 7, :].rearrange("p (h t) -> p h t", h=2), func=AF.Copy),
                         reads=[pbuf[7]] + RG, writes=[b_qiblk[qs]])
                    yield 1.0

            def gen_D(i):
                dw = dgw[i % 2]; bdw = b_dgw[i % 2]
                for h in range(N_HEADS):
                    S.op("dve", lambda e, h=h: e.tensor_scalar(out=dw[:, h, :], in0=ident_b[:], scalar1=wtok[:, i, h:h + 1],
                                                               scalar2=None, op0=ALU.mult),
                         reads=[b_identb, b_wtok] + RG, writes=[bdw])
                yield 1.0

            def gen_X(i):
                B, tl = i // 2, i % 2
                qs = B % 2
                Lk = 128 * (i + 1)
                sc = score[i % 2]; bsc = b_score[i % 2]
                dw = dgw[i % 2]; bdw = b_dgw[i % 2]
                nsb = (Lk + 511) // 512

                def emit_dm(item):
                    sbk, h, rs_, c0, w = item
                    S.op("pe", lambda e: e.matmul(psum[:, 6, 0:w], lhsT=dw[:, h, :], rhs=rt[rs_][:, 0:w],
                                                  start=(h == 0), stop=(h == N_HEADS - 1)),
                         reads=[bdw, b_rt[rs_]], writes=[pbuf[6]])
                    if h == N_HEADS - 1:
                        last = (sbk == nsb - 1)
                        wc = w - 128 if last else w
                        if wc > 0:
                            S.op("act", lambda e: e.activation(out=sc[:, c0:c0 + wc], in_=psum[:, 6, 0:wc], func=AF.Copy),
                                 reads=[pbuf[6]] + RG, writes=[bsc])
                        if last:
                            S.op("dve", lambda e: e.tensor_tensor(out=sc[:, c0 + wc:c0 + wc + 128], in0=psum[:, 6, wc:wc + 128],
                                                                  in1=causal[:], op=ALU.add),
                                 reads=[pbuf[6], b_causal] + RG, writes=[bsc])

                pend = []
                for sbk in range(nsb):
                    c0 = sbk * 512
                    w = min(Lk, c0 + 512) - c0
                    for pr in range(4):
                        items = []
                        for hf in range(2):
                            h = pr * 2 + hf
                            rb = 4 + hf
                            rs_ = cnt_r[0] % 4
                            cnt_r[0] += 1
                            S.op("pe", lambda e, hf=hf, rb=rb: e.matmul(
                                psum[:, rb, 0:w], lhsT=qiblk[qs][hf * 64:(hf + 1) * 64, pr, tl * 128:(tl + 1) * 128],
                                rhs=kidx2[hf * 64:(hf + 1) * 64, c0:c0 + w], start=True, stop=True),
                                reads=[b_qiblk[qs], b_kidx], writes=[pbuf[rb]])
                            items.append((sbk, h, rs_, c0, w, rb))
                        for (sbk_, h, rs_, c0_, w_, rb) in items:
                            S.op("act", lambda e, rb=rb, rs_=rs_: e.activation(out=rt[rs_][:, 0:w], in_=psum[:, rb, 0:w], func=AF.Relu),
                                 reads=[pbuf[rb]] + RG, writes=[b_rt[rs_]])
                        for it in pend:
                            emit_dm(it)
                        pend = [it[:5] for it in items]
                        yield 2.0
                for it in pend:
                    emit_dm(it)
                yield 1.0

            dve_counting = [False]

            def gen_Y(i):
                Lk = 128 * (i + 1)
                sc = score[i % 2]; bsc = b_score[i % 2]
                mk = maskts[i % 2]; bmk = b_maskts[i % 2]
                if i < 2:
                    thr_ap = negbig[:, 0:1]
                    thr_reads = [b_negbig]
                else:
                    dve_counting[0] = True
                    S.op("dve", lambda e: e.tensor_reduce(out=bis[:, 0:1], in_=sc[:, 0:Lk], axis=AX.X, op=ALU.max),
                         reads=[bsc] + RG, writes=[b_bis])
                    yield 2.0
                    S.op("dve", lambda e: e.tensor_reduce(out=bis[:, 1:2], in_=sc[:, 0:TOPK], axis=AX.X, op=ALU.min),
                         reads=[bsc] + RG, writes=[b_bis])
                    yield 2.0
                    S.op("dve", lambda e: e.tensor_tensor(out=bis[:, 2:3], in0=bis[:, 0:1], in1=bis[:, 1:2], op=ALU.subtract),
                         reads=[b_bis] + RG, writes=[b_bis])
                    S.op("dve", lambda e: e.tensor_scalar(out=steps[:], in0=pow2[:], scalar1=bis[:, 2:3], scalar2=None, op0=ALU.mult),
                         reads=[b_pow2, b_bis] + RG, writes=[b_steps])
                    S.op("dve", lambda e: e.tensor_tensor(out=cands[:, 0:1], in0=bis[:, 1:2], in1=steps[:, 0:1], op=ALU.add),
                         reads=[b_bis, b_steps] + RG, writes=[b_cands[0]])
                    yield 1.0
                    nb_i = NB - (2 if Lk <= 512 else (1 if Lk <= 1024 else 0))
                    for j in range(nb_i):
                        S.op("dve", lambda e, j=j: e.tensor_scalar(out=mk[:, 0:Lk], in0=sc[:, 0:Lk], scalar1=cands[:, j:j + 1],
                                                                   scalar2=None, op0=ALU.is_ge, op1=ALU.add,
                                                                   accum_out=cnts[:, j:j + 1]),
                             reads=[bsc, b_cands[j]] + RG, writes=[bmk, b_cnts[j]])
                        S.op("dve", lambda e, j=j: e.tensor_scalar(out=sels[:, j:j + 1], in0=cnts[:, j:j + 1], scalar1=TOPK - 0.5,
                                                                   scalar2=steps[:, j:j + 1], op0=ALU.is_ge, op1=ALU.mult),
                             reads=[b_cnts[j], b_steps] + RG, writes=[b_sels[j]])
                        S.op("dve", lambda e, j=j: e.scalar_tensor_tensor(out=cands[:, j + 1:j + 2], in0=sels[:, j:j + 1],
                                                                          scalar=cands[:, j:j + 1], in1=steps[:, j + 1:j + 2],
                                                                          op0=ALU.add, op1=ALU.subtract),
                             reads=[b_sels[j], b_cands[j], b_steps] + RG, writes=[b_cands[j + 1]])
                        yield 2.5
                    if nb_i < NB:
                        S.op("dve", lambda e: e.scalar_tensor_tensor(out=cands[:, NB + 1:NB + 2], in0=cands[:, nb_i:nb_i + 1], scalar=1.0,
                                                                     in1=steps[:, nb_i:nb_i + 1], op0=ALU.mult, op1=ALU.subtract),
                             reads=[b_cands[nb_i], b_steps] + RG, writes=[b_cands[NB + 1]])
                        thr_ap = cands[:, NB + 1:NB + 2]
                        thr_reads = [b_cands[NB + 1]]
                    else:
                        thr_ap = cands[:, NB:NB + 1]
                        thr_reads = [b_cands[NB]]
                S.op("dve", lambda e: e.tensor_scalar(out=mk[:, 0:Lk], in0=sc[:, 0:Lk], scalar1=thr_ap, scalar2=None, op0=ALU.is_ge),
                     reads=[bsc] + thr_reads + RG, writes=[bmk])
                dve_counting[0] = False
                yield 1.0

            def gen_Z(i):
                tl = i % 2
                mk = maskts[i % 2]; bmk = b_maskts[i % 2]
                for j0 in range(0, i + 1, 8):
                    n = min(8, i + 1 - j0)
                    for jj in range(n):
                        j = j0 + jj
                        S.op("pe", lambda e, j=j, jj=jj: e.transpose(out=psb[:, jj, :], in_=mk[:, j * 128:(j + 1) * 128], identity=ident_b[:]),
                             reads=[bmk, b_identb], writes=[pbuf[7]])
                    S.op("act", lambda e, j0=j0, n=n: e.activation(out=maskT[:, j0:j0 + n, tl * 128:(tl + 1) * 128], in_=psb[:, 0:n, :],
                                                                   func=AF.Copy),
                         reads=[pbuf[7]] + RG, writes=[b_maskT[tl]])
                    yield 1.0

            def gen_W(B):
                i0, i1 = B * 2, B * 2 + 1
                qs = B % 2
                tcols = slice(B * 256, (B + 1) * 256)
                ob = [0, 1]

                def emit_pv(item):
                    h, j, es, c0 = item
                    for tl in range(2):
                        if tl * 128 < c0:
                            continue
                        lastj = i0 if tl == 0 else i1
                        S.op("pe", lambda e, tl=tl, lastj=lastj: e.matmul(
                            psum[:, ob[tl], 0:65], lhsT=pm[es][:, tl * 128:(tl + 1) * 128], rhs=vaug[:, j, h, :],
                            start=(j == 0), stop=(j == lastj)),
                            reads=[b_pm[es], b_vaug], writes=[pbuf[ob[tl]]])
                    if j == i1:
                        for tl in range(2):
                            S.op("act", lambda e, tl=tl: e.activation(out=osb[:, tl, h, :], in_=psum[:, ob[tl], 0:65], func=AF.Copy),
                                 reads=[pbuf[ob[tl]]] + RG, writes=[b_osb])

                pend = []
                for h in range(N_HEADS):
                    for j in range(i1 + 1):
                        c0 = 0 if j <= i0 else 128
                        qb = 2 + (cnt_e[0] % 2)
                        es = cnt_e[0] % 4
                        cnt_e[0] += 1
                        S.op("pe", lambda e, j=j, c0=c0, qb=qb, h=h: e.matmul(psum[:, qb, c0:256], lhsT=kvnT[:, j * 128:(j + 1) * 128],
                                                                             rhs=qblk[qs][:, h, c0:256], start=True, stop=True),
                             reads=[b_kvn, b_qblk[qs]], writes=[pbuf[qb]])
                        S.op("act", lambda e, c0=c0, qb=qb, es=es, h=h: e.activation(out=et[es][:, c0:256], in_=psum[:, qb, c0:256], func=AF.Exp,
                                                                                    bias=b31bc[:, h:h + 1], scale=sm_scale),
                             reads=[pbuf[qb], b_b31] + RG, writes=[b_et[es]])
                        meng = "pool" if dve_counting[0] else "dve"
                        if j >= i0 - 1:
                            if j == i0 - 1:
                                tc0, tc1, u0 = 0, 128, 128
                            elif j == i0:
                                tc0, tc1, u0 = 0, 256, 0
                            else:
                                tc0, tc1, u0 = 128, 256, 0
                            S.op(meng, lambda e, es=es, tc0=tc0, tc1=tc1, u0=u0, h=h: e.tensor_tensor(
                                out=et[es][:, tc0:tc1], in0=et[es][:, tc0:tc1], in1=tbc[:, h, u0:u0 + (tc1 - tc0)], op=ALU.mult),
                                reads=[b_et[es], b_tbc] + RG, writes=[b_et[es]])
                        S.op(meng, lambda e, es=es, c0=c0, j=j: e.tensor_tensor(out=pm[es][:, c0:256], in0=et[es][:, c0:256], in1=maskT[:, j, c0:256],
                                                                               op=ALU.mult),
                             reads=[b_et[es], b_maskT[0], b_maskT[1]] + RG, writes=[b_pm[es]])
                        pend.append((h, j, es, c0))
                        if len(pend) > DSK:
                            emit_pv(pend.pop(0))
                        yield 1.0
                while pend:
                    emit_pv(pend.pop(0))
                yield 1.0
                S.op("dve", lambda e: e.reciprocal(out=rcp[:], in_=osb[:, :, :, 64]), reads=[b_osb] + RG, writes=[b_rcp])
                S.op("dve", lambda e: e.tensor_tensor(out=ytok[:].rearrange("p t (h d) -> p t h d", h=N_HEADS), in0=osb[:, :, :, 0:64],
                                                      in1=rcp[:].unsqueeze(3).to_broadcast([128, 2, N_HEADS, 64]), op=ALU.mult),
                     reads=[b_osb, b_rcp] + RG, writes=[b_ytok[0], b_ytok[1]])
                for ch in range(4):
                    hb = (ch % 2) * 256
                    for tl in range(2):
                        S.op("pe", lambda e, ch=ch, tl=tl, hb=hb: e.transpose(out=psum[:, 7, hb + tl * 128:hb + (tl + 1) * 128],
                                                                             in_=ytok[:, tl, ch * 128:(ch + 1) * 128], identity=ident_f[:]),
                             reads=[b_ytok[tl], b_identf], writes=[pbuf[7]])
                    S.op("dve", lambda e, ch=ch, hb=hb: e.tensor_tensor(out=sgT[:, ch, tcols], in0=psum[:, 7, hb:hb + 256], in1=sgT[:, ch, tcols],
                                                                        op=ALU.mult),
                         reads=[pbuf[7], b_sg[B]] + RG, writes=[b_sg[B]])
                yield 1.0

            def run(g):
                for _ in g:
                    pass

            def chain(*gens):
                for g in gens:
                    for wgt in g:
                        yield wgt

            def par(ga, gb):
                ta = tb_ = 0.0
                a_done = b_done = False
                while not (a_done and b_done):
                    if b_done or (not a_done and ta <= tb_):
                        try:
                            wgt = next(ga); ta += wgt
                            yield wgt * 0.5
                        except StopIteration:
                            a_done = True
                            ta = float("inf")
                    else:
                        try:
                            wgt = next(gb); tb_ += wgt
                            yield wgt * 0.5
                        except StopIteration:
                            b_done = True
                            tb_ = float("inf")

            def interleave(ga, na, gb, nb_):
                a_done = b_done = False
                pa = pb_ = 0.0
                while not (a_done and b_done):
                    if b_done or (not a_done and pa * nb_ <= pb_ * na):
                        try:
                            pa += next(ga)
                        except StopIteration:
                            a_done = True
                    else:
                        try:
                            pb_ += next(gb)
                        except StopIteration:
                            b_done = True

            def n_x(i):
                return 1.0 + 8.0 * ((128 * (i + 1) + 511) // 512)

            def n_y(i):
                return 1.0 if i < 2 else 2.5 * NB + 5.0

            NBLK = NT // 2

            def nothing():
                return
                yield 0.0

            run(gen_Qq(0)); run(gen_Qi(0)); run(gen_D(0)); run(gen_D(1)); run(gen_X(0)); run(gen_Y(0)); run(gen_X(1)); run(gen_Y(1))
            run(gen_Z(0)); run(gen_Z(1))
            run(gen_Qi(1)); run(gen_D(2)); run(gen_D(3)); run(gen_X(2))
            for B in range(NBLK):
                if B + 1 < NBLK:
                    i2, i3 = 2 * B + 2, 2 * B + 3
                    if B + 2 < NBLK:
                        nxt = chain(gen_Qi(B + 2), gen_D(i2 + 2), gen_D(i3 + 2), gen_X(i2 + 2))
                        n_nxt = 4.0 + n_x(i2 + 2)
                    else:
                        nxt = nothing()
                        n_nxt = 0.0
                    side = chain(gen_Qq(B + 1), par(gen_Y(i2), gen_X(i3)), par(gen_Y(i3), nxt))
                    n_side = 4.0 + 0.5 * (n_y(i2) + n_x(i3)) + 0.5 * (n_y(i3) + n_nxt)
                    interleave(gen_W(B), 8.0 * (2 * B + 2) + 2.0, side, n_side)
                    run(gen_Z(i2)); run(gen_Z(i3))
                else:
                    run(gen_W(B))

            phase_barrier()
            S.op("dve", lambda e: e.tensor_tensor(out=gvT[:], in0=modT[:, l, 16:24, b], in1=gpostT[:, l, :], op=ALU.mult),
                 reads=[b_modT, b_small] + RG, writes=[b_gvT])
            bcast_rows(lambda k: gvT[:, k:k + 1], gbc, b_gbc, dgE, b_dgE, [b_gvT])
            for g in range(4):
                load_w([(lambda stg: stage3(stg, 256), None)], None,
                       [(woutb[:, :, g * 256:(g + 1) * 256], lambda stg: stage3(stg, 256), [b_woutb[g]])])
            for tt in range(NT):
                tsl = slice(tt * 128, (tt + 1) * 128)
                for nh in range(2):
                    bk = (tt * 2 + nh) % 4
                    for k in range(8):
                        lhs = yconvT[:, k, tsl] if k < 4 else sgT[:, k - 4, tsl]
                        rd = [b_yconv[tt // 4]] if k < 4 else [b_sg[tt // 2]]
                        S.op("pe", lambda e, lhs=lhs, k=k, nh=nh, bk=bk: e.matmul(psum[:, bk, :], lhsT=lhs, rhs=woutb[:, k, nh * 512:(nh + 1) * 512],
                                                                                 start=(k == 0), stop=(k == 7)),
                             reads=rd + [b_woutb[2 * nh], b_woutb[2 * nh + 1]], writes=[pbuf[bk]])
                    S.op("act", lambda e, nh=nh, bk=bk: e.activation(out=ejunk[:], in_=psum[:, bk, :], func=AF.Square, accum_out=ess[:, nh:nh + 1]),
                         reads=[pbuf[bk]] + RG, writes=[b_ejunk, b_ess])
                S.op("dve", lambda e: e.tensor_tensor(out=ess[:, 2:3], in0=ess[:, 0:1], in1=ess[:, 1:2], op=ALU.add),
                     reads=[b_ess] + RG, writes=[b_ess])
                S.op("act", lambda e: e.activation(out=ess[:, 2:3], in_=ess[:, 2:3], func=AF.Sqrt, bias=EPS, scale=1.0 / D_MODEL),
                     reads=[b_ess] + RG, writes=[b_ess])
                S.op("dve", lambda e: e.reciprocal(out=ess[:, 3:4], in_=ess[:, 2:3]), reads=[b_ess] + RG, writes=[b_ess])
                for nh in range(2):
                    bk = (tt * 2 + nh) % 4
                    S.op("dve", lambda e, nh=nh, bk=bk: e.scalar_tensor_tensor(out=etmp[nh][:], in0=psum[:, bk, :], scalar=ess[:, 3:4],
                                                                               in1=gbc[:, nh * 512:(nh + 1) * 512], op0=ALU.mult, op1=ALU.mult),
                         reads=[pbuf[bk], b_ess, b_gbc] + RG, writes=[b_etmp[nh]])
                    S.op("dve", lambda e, nh=nh, tt=tt: e.tensor_tensor(out=xs_res[:, tt, nh * 512:(nh + 1) * 512],
                                                                         in0=xs_res[:, tt, nh * 512:(nh + 1) * 512], in1=etmp[nh][:], op=ALU.add),
                         reads=[b_etmp[nh], b_x[tt]] + RG, writes=[b_x[tt]])
                if last_layer and (tt % 4 == 3):
                    t0 = tt - 3
                    S.op("sp", lambda e, t0=t0: e.dma_start(out=out_d[b, t0 * 128:(t0 + 4) * 128, :].rearrange("(n p) d -> p n d", p=128),
                                                            in_=xs_res[:, t0:t0 + 4, :]),
                         reads=[b_x[t0], b_x[t0 + 1], b_x[t0 + 2], b_x[t0 + 3]])
                    if b + 1 < nseq:
                        load_x_quarter(b + 1, t0 // 4)

        phase_barrier([b_pro, b_gc, b_tbrev, b_scr, b_wada[0], b_wada[1], b_wadab[0], b_wadab[1], b_cactb, b_tbc, b_modT])
        for q4 in range(4):
            load_x_quarter(0, q4)
        for b in range(nseq):
            for li, l in enumerate(layers):
                block(b, l, li == len(layers) - 1)
        S.finish()
        S.emit()
        build_program.nins = S.nins
    return nc


def _fm(v, nchunk):
    L = v.shape[0]
    return np.ascontiguousarray(v.reshape(L, nchunk, 128).transpose(2, 0, 1)).astype(np.float32)


def pack_weights(inputs):
    f = lambda a: np.asarray(a, dtype=np.float32)
    w_in, w_pw2, w_uq, w_qidx, w_uv, w_out = (f(inputs[k]) for k in ("w_in", "w_pw2", "w_uq", "w_qidx", "w_uv", "w_out"))
    packs = np.zeros((DEPTH, 17, 128, 2048), np.float32)
    for l in range(DEPTH):
        win = w_in[l].reshape(8, 128, D_IN_PROJ).transpose(1, 0, 2)
        jobs = []
        jobs.append(w_pw2[l].reshape(4, 128, 512).transpose(1, 0, 2).reshape(128, 2048))
        for c in range(4):
            t = np.zeros((128, 8, 256), np.float32)
            t[:, :, 0:128] = win[:, :, c * 128:(c + 1) * 128]
            t[:, :, 128:256] = win[:, :, 512 + c * 128:512 + (c + 1) * 128]
            jobs.append(t.reshape(128, 2048))
        for g in range(2):
            jobs.append(win[:, :, 1024 + g * 256:1024 + (g + 1) * 256].reshape(128, 2048))
        jobs.append(w_uq[l].reshape(2, 128, 1024).transpose(1, 0, 2).reshape(128, 2048))
        t = np.zeros((128, 2048), np.float32)
        t[:, 0:1024] = w_qidx[l].reshape(2, 128, 512).transpose(1, 0, 2).reshape(128, 1024)
        t[:, 1024:1536] = w_uv[l].transpose(1, 0, 2).reshape(KV_LORA, N_HEADS * 64)
        jobs.append(t)
        jobs.append(win[:, :, 1536:1792].reshape(128, 2048))
        t = np.zeros((128, 8, 256), np.float32)
        t[:, :, 0:200] = win[:, :, 1792:1992]
        jobs.append(t.reshape(128, 2048))
        for g in range(2):
            jobs.append(win[:, :, 1992 + g * 256:1992 + (g + 1) * 256].reshape(128, 2048))
        wo = w_out[l].reshape(8, 128, D_MODEL).transpose(1, 0, 2)
        for g in range(4):
            jobs.append(wo[:, :, g * 256:(g + 1) * 256].reshape(128, 2048))
        assert len(jobs) == 17
        for j, t in enumerate(jobs):
            packs[l, j] = t
    return packs


def make_in_maps(inputs, ncores=NCORES, nseq=SEQ_PER_CORE):
    f = lambda a: np.ascontiguousarray(np.asarray(a, dtype=np.float32))
    x = f(inputs["x"]); c = f(inputs["c"])
    n = np.arange(NG) - 127
    oh = np.zeros((N_BUCKETS, NG), np.float32)
    oh[t5_bucket_np(n), np.arange(NG)] = 1.0
    conv_w = f(inputs["conv_w"])
    conv_wT = np.ascontiguousarray(conv_w.reshape(DEPTH, CONV_WIDTH, 4, 128).transpose(3, 0, 2, 1))
    rel_bias = f(inputs["rel_bias"])
    w_ada = f(inputs["w_ada"])
    w_adaP = np.ascontiguousarray(w_ada.reshape(DEPTH, 8, 128, 8, 384).transpose(0, 3, 2, 1, 4))
    shared = {
        "w_adaP": w_adaP, "b_adaT": _fm(f(inputs["b_ada"]), 24), "g_preT": _fm(f(inputs["g_pre"]), 8),
        "g_postT": _fm(f(inputs["g_post"]), 8), "wpack": pack_weights(inputs), "conv_wT": conv_wT,
        "conv_bT": _fm(f(inputs["conv_b"]), 4), "ln_gT": _fm(f(inputs["conv_ln_g"]), 4), "ln_bT": _fm(f(inputs["conv_ln_b"]), 4),
        "q_norm_gT": _fm(f(inputs["q_norm_g"]), 2), "kv_norm_gT": _fm(f(inputs["kv_norm_g"]), 1),
        "rel_bias": rel_bias, "rel_biasT": np.ascontiguousarray(rel_bias.T), "oh": oh,
    }
    maps = []
    for i in range(ncores):
        xb = x[i * nseq:(i + 1) * nseq]
        cb = c[i * nseq:(i + 1) * nseq]
        cTb = np.ascontiguousarray(cb.reshape(nseq, 8, 128).transpose(2, 1, 0))
        m = dict(shared)
        m["x"] = np.ascontiguousarray(xb)
        m["cT"] = cTb
        maps.append(m)
    return maps


def kernel(**inputs):
    nc = build_program(layers=(0, 1), nseq=SEQ_PER_CORE)
    in_maps = make_in_maps(inputs)
    res = run_bass_kernel_spmd(nc, in_maps, core_ids=list(range(NCORES)))
    outs = [np.asarray(r["out"], dtype=np.float32) for r in res.results]
    return np.concatenate(outs, axis=0)
```
